# Optimizing a Trainium2 kernel written in Bass

```python
import math
import jax, jax.numpy as jnp
from jax import lax
import numpy as np


D_MODEL = 1024
BATCH = 8
SEQ = 4096
DEPTH = 2

GRID_W = 64
CTX_LEN = 256
N_MIXERS = 2
N_MLSTM_LAYERS = (DEPTH + N_MIXERS - 1) // N_MIXERS
N_SSD_LAYERS = DEPTH // N_MIXERS
N_MOD = 9
D_FF = 2816
CONV_W = 5
CHUNK = 128
EPS = 1e-6
ML_INNER = 2 * D_MODEL
ML_HEADS = 4
ML_HEAD_DIM = ML_INNER // ML_HEADS
ML_BLOCK = 4
ML_NBLOCKS = ML_INNER // ML_BLOCK
SSD_INNER = 2 * D_MODEL
SSD_HEAD_DIM = 64
SSD_HEADS = SSD_INNER // SSD_HEAD_DIM
SSD_GROUPS = 8
SSD_HPG = SSD_HEADS // SSD_GROUPS
SSD_STATE = 128
SSD_CONV_DIM = SSD_INNER + 2 * SSD_GROUPS * SSD_STATE
SSD_IN_DIM = SSD_INNER + SSD_CONV_DIM + 2 * SSD_HEADS

kernel_name = 'hybrid_mlstm_ssd_prefix_dit'


def rmsnorm(x, g):
    xf = x.astype(jnp.float32)
    y = xf * lax.rsqrt(jnp.mean(xf * xf, axis=-1, keepdims=True) + EPS)
    return (y * g.astype(jnp.float32)).astype(x.dtype)


def head_layernorm(h):
    mu = jnp.mean(h, axis=-1, keepdims=True)
    var = jnp.mean(jnp.square(h - mu), axis=-1, keepdims=True)
    return (h - mu) * lax.rsqrt(var + EPS)


def modulate(h, shift, scale):
    return h * (1.0 + scale) + shift


def short_conv(u, w, b, rows):
    bsz, t, ch = u.shape
    row_len = t // rows
    k = w.shape[0]
    pad = k // 2
    up = jnp.pad(u.reshape(bsz, rows, row_len, ch), ((0, 0), (0, 0), (pad, pad), (0, 0)))
    out = b
    for j in range(k):
        out = out + up[:, :, j:j + row_len] * w[j]
    return out.reshape(bsz, t, ch)


def to_chunks(t):
    bsz, tl = t.shape[:2]
    return jnp.moveaxis(t.reshape(bsz, tl // CHUNK, CHUNK, *t.shape[2:]), 1, 0)


def from_chunks(t):
    t = jnp.moveaxis(t, 0, 1)
    return t.reshape(t.shape[0], t.shape[1] * t.shape[2], *t.shape[3:])


def swiglu(h, w_gate, w_up, w_down):
    return (jax.nn.silu(h @ w_gate) * (h @ w_up)) @ w_down


def ffn_sub(x, mod, j0, g_pre, g_post, w_gate, w_up, w_down):
    h = modulate(rmsnorm(x, g_pre), mod[:, :, j0], mod[:, :, j0 + 1])
    return x + 0.5 * mod[:, :, j0 + 2] * rmsnorm(swiglu(h, w_gate, w_up, w_down), g_post)


def mlstm_scan(state, q, k, v, ig, lf):
    tril = jnp.tril(jnp.ones((CHUNK, CHUNK), bool))[None, :, :, None]

    def step(carry, inp):
        cmat, nvec, m = carry
        qc, kc, vc, igc, lfc = inp
        b = jnp.cumsum(lfc, axis=1)
        dm = b[:, :, None, :] - b[:, None, :, :] + igc[:, None, :, :]
        dm = jnp.where(tril, dm, -jnp.inf)
        m_inter = b + m[:, None, :]
        m_t = jnp.maximum(m_inter, jnp.max(dm, axis=2))
        s = jnp.einsum('bthd,bshd->btsh', qc, kc) * jnp.exp(dm - m_t[:, :, None, :])
        sc = jnp.exp(m_inter - m_t)
        num = jnp.einsum('btsh,bshv->bthv', s, vc) + sc[..., None] * jnp.einsum('bthd,bhdv->bthv', qc, cmat)
        den = jnp.sum(s, axis=2) + sc * jnp.einsum('bthd,bhd->bth', qc, nvec)
        h = num / jnp.maximum(jnp.abs(den), jnp.exp(-m_t))[..., None]
        b_end = b[:, -1]
        gl = b_end[:, None] - b + igc
        m_new = jnp.maximum(b_end + m, jnp.max(gl, axis=1))
        wgt = jnp.exp(gl - m_new[:, None])
        dec = jnp.exp(b_end + m - m_new)
        c_new = dec[..., None, None] * cmat + jnp.einsum('bsh,bshd,bshv->bhdv', wgt, kc, vc)
        n_new = dec[..., None] * nvec + jnp.einsum('bsh,bshd->bhd', wgt, kc)
        return (c_new, n_new, m_new), h

    state, h = lax.scan(step, state, tuple(to_chunks(t) for t in (q, k, v, ig, lf)))
    return from_chunks(h), state


def ssd_scan(state, xs, dt, la, bm, cm):
    tril = jnp.tril(jnp.ones((CHUNK, CHUNK), bool))[None, :, :, None, None]

    def step(h, inp):
        xc, dtc, lac, bc, cc = inp
        cs = jnp.cumsum(lac, axis=1)
        seg = cs[:, :, None] - cs[:, None, :]
        decay = jnp.exp(jnp.where(tril, seg, -jnp.inf))
        cb = jnp.einsum('btgn,bsgn->btsg', cc, bc)
        mmat = cb[..., None] * decay * dtc[:, None]
        y = jnp.einsum('btsgr,bsgrp->btgrp', mmat, xc) \
            + jnp.exp(cs)[..., None] * jnp.einsum('btgn,bgrpn->btgrp', cc, h)
        wgt = jnp.exp(cs[:, -1:] - cs) * dtc
        h_new = jnp.exp(cs[:, -1])[..., None, None] * h + jnp.einsum('bsgr,bsgrp,bsgn->bgrpn', wgt, xc, bc)
        return h_new, y

    state, y = lax.scan(step, state, tuple(to_chunks(t) for t in (xs, dt, la, bm, cm)))
    return from_chunks(y), state


def scan_both_ways(scan_fn, init_state, ctx_in, lat_in, reverse):
    if reverse:
        ctx_in = tuple(jnp.flip(t, axis=1) for t in ctx_in)
        lat_in = tuple(jnp.flip(t, axis=1) for t in lat_in)
    h_c, st = scan_fn(init_state, *ctx_in)
    h_l, _ = scan_fn(st, *lat_in)
    if reverse:
        h_c, h_l = jnp.flip(h_c, axis=1), jnp.flip(h_l, axis=1)
    return h_c, h_l


def mlstm_mixer(hc, hl, rows, need_ctx, w_in, conv_w, conv_b, w_q, w_k, w_v,
                w_gates, b_gates, norm_g, skip, w_out):
    def prep(h, n_rows):
        bsz, t, _ = h.shape
        xm, z = jnp.split(h @ w_in, 2, axis=-1)
        xc = jax.nn.silu(short_conv(xm, conv_w, conv_b, n_rows))

        def blk(u, w):
            return jnp.einsum('btnc,ncd->btnd', u.reshape(bsz, t, ML_NBLOCKS, ML_BLOCK), w).reshape(bsz, t, ML_INNER)

        q, k, v = blk(xc, w_q), blk(xc, w_k), blk(xm, w_v)
        gates = (q @ w_gates[:ML_INNER] + k @ w_gates[ML_INNER:2 * ML_INNER]
                 + v @ w_gates[2 * ML_INNER:] + b_gates)
        gates = gates.astype(jnp.float32).reshape(bsz, t, 2, 2, ML_HEADS)

        def heads(u):
            return u.astype(jnp.float32).reshape(bsz, t, ML_HEADS, ML_HEAD_DIM)

        return xc, z, heads(q), heads(k) * (ML_HEAD_DIM ** -0.5), heads(v), gates

    xc_c, z_c, qc, kc, vc, gc = prep(hc, 1)
    xc_l, z_l, ql, kl, vl, gl = prep(hl, rows)
    bsz = hl.shape[0]
    init = (jnp.zeros((bsz, ML_HEADS, ML_HEAD_DIM, ML_HEAD_DIM), jnp.float32),
            jnp.zeros((bsz, ML_HEADS, ML_HEAD_DIM), jnp.float32),
            jnp.zeros((bsz, ML_HEADS), jnp.float32))
    h_c, h_l = None, None
    for d in range(2):
        ctx_in = (qc, kc, vc, gc[:, :, d, 0], jax.nn.log_sigmoid(gc[:, :, d, 1]))
        lat_in = (ql, kl, vl, gl[:, :, d, 0], jax.nn.log_sigmoid(gl[:, :, d, 1]))
        hcd, hld = scan_both_ways(mlstm_scan, init, ctx_in, lat_in, reverse=(d == 1))
        h_c = hcd if d == 0 else h_c + hcd
        h_l = hld if d == 0 else h_l + hld

    def readout(h, xc, z):
        b_, t_ = h.shape[:2]
        h = head_layernorm(h).reshape(b_, t_, ML_INNER).astype(xc.dtype) * norm_g + skip * xc
        return (h * jax.nn.silu(z)) @ w_out

    y_l = readout(h_l, xc_l, z_l)
    y_c = readout(h_c, xc_c, z_c) if need_ctx else None
    return y_c, y_l


def ssd_mixer(hc, hl, rows, need_ctx, w_in, conv_w, conv_b, dt_bias, a_log, d_skip, norm_g, w_out):
    gn = SSD_GROUPS * SSD_STATE

    def prep(h, n_rows):
        bsz, t, _ = h.shape
        proj = h @ w_in
        z = proj[..., :SSD_INNER]
        xbc = jax.nn.silu(short_conv(proj[..., SSD_INNER:SSD_INNER + SSD_CONV_DIM], conv_w, conv_b, n_rows))
        xbc = xbc.astype(jnp.float32)
        xs = xbc[..., :SSD_INNER].reshape(bsz, t, SSD_GROUPS, SSD_HPG, SSD_HEAD_DIM)
        bm = xbc[..., SSD_INNER:SSD_INNER + gn].reshape(bsz, t, SSD_GROUPS, SSD_STATE)
        cm = xbc[..., SSD_INNER + gn:].reshape(bsz, t, SSD_GROUPS, SSD_STATE)
        dt_raw = proj[..., SSD_INNER + SSD_CONV_DIM:].astype(jnp.float32).reshape(bsz, t, 2, SSD_GROUPS, SSD_HPG)
        return z, xs, bm, cm, dt_raw

    z_c, xs_c, bm_c, cm_c, dt_c = prep(hc, 1)
    z_l, xs_l, bm_l, cm_l, dt_l = prep(hl, rows)
    bsz = hl.shape[0]
    init = jnp.zeros((bsz, SSD_GROUPS, SSD_HPG, SSD_HEAD_DIM, SSD_STATE), jnp.float32)
    y_c, y_l = None, None
    for d in range(2):
        a = -jnp.exp(a_log[d].astype(jnp.float32)).reshape(SSD_GROUPS, SSD_HPG)
        bias = dt_bias[d].astype(jnp.float32).reshape(SSD_GROUPS, SSD_HPG)
        dtc = jax.nn.softplus(dt_c[:, :, d] + bias)
        dtl = jax.nn.softplus(dt_l[:, :, d] + bias)
        ctx_in = (xs_c, dtc, dtc * a, bm_c, cm_c)
        lat_in = (xs_l, dtl, dtl * a, bm_l, cm_l)
        ycd, yld = scan_both_ways(ssd_scan, init, ctx_in, lat_in, reverse=(d == 1))
        y_c = ycd if d == 0 else y_c + ycd
        y_l = yld if d == 0 else y_l + yld
    dsk = d_skip.astype(jnp.float32).reshape(SSD_GROUPS, SSD_HPG)[..., None]

    def readout(y, xs, z):
        b_, t_ = y.shape[:2]
        y = (y + dsk * xs).reshape(b_, t_, SSD_INNER).astype(z.dtype)
        return rmsnorm(y * jax.nn.silu(z), norm_g) @ w_out

    out_l = readout(y_l, xs_l, z_l)
    out_c = readout(y_c, xs_c, z_c) if need_ctx else None
    return out_c, out_l


def setup_inputs(seed: int = 0) -> dict:
    key = jax.random.key(seed)
    ks = iter(jax.random.split(key, 40))
    f32 = jnp.float32

    def nrm(shape, s):
        return s * jax.random.normal(next(ks), shape, f32)

    nm, ns = N_MLSTM_LAYERS, N_SSD_LAYERS
    x = nrm((BATCH, SEQ, D_MODEL), 1.0)
    c = nrm((BATCH, D_MODEL), 1.0)
    ctx = nrm((BATCH, CTX_LEN, D_MODEL), 1.0)
    c_ctx = nrm((D_MODEL,), 1.0)
    ada_w = nrm((DEPTH, D_MODEL, N_MOD * D_MODEL), 0.5 * D_MODEL ** -0.5)
    ada_b = nrm((DEPTH, N_MOD * D_MODEL), 0.02)
    norm_g = 1.0 + nrm((DEPTH, 6, D_MODEL), 0.02)
    ffn_w_gate = nrm((DEPTH, 2, D_MODEL, D_FF), D_MODEL ** -0.5)
    ffn_w_up = nrm((DEPTH, 2, D_MODEL, D_FF), D_MODEL ** -0.5)
    ffn_w_down = nrm((DEPTH, 2, D_FF, D_MODEL), D_FF ** -0.5)
    mlstm_w_in = nrm((nm, D_MODEL, 2 * ML_INNER), D_MODEL ** -0.5)
    mlstm_conv_w = nrm((nm, CONV_W, ML_INNER), CONV_W ** -0.5)
    mlstm_conv_b = nrm((nm, ML_INNER), 0.02)
    mlstm_w_q = nrm((nm, ML_NBLOCKS, ML_BLOCK, ML_BLOCK), ML_BLOCK ** -0.5)
    mlstm_w_k = nrm((nm, ML_NBLOCKS, ML_BLOCK, ML_BLOCK), ML_BLOCK ** -0.5)
    mlstm_w_v = nrm((nm, ML_NBLOCKS, ML_BLOCK, ML_BLOCK), ML_BLOCK ** -0.5)
    mlstm_w_gates = nrm((nm, 3 * ML_INNER, 4 * ML_HEADS), 0.01)
    i_bias = nrm((nm, 2, 1, ML_HEADS), 0.1)
    f_bias = jnp.linspace(3.0, 6.0, ML_HEADS, dtype=f32) + nrm((nm, 2, 1, ML_HEADS), 0.1)
    mlstm_b_gates = jnp.concatenate([i_bias, f_bias], axis=2).reshape(nm, 4 * ML_HEADS)
    mlstm_norm_g = 1.0 + nrm((nm, ML_INNER), 0.02)
    mlstm_skip = 1.0 + nrm((nm, ML_INNER), 0.02)
    mlstm_w_out = nrm((nm, ML_INNER, D_MODEL), ML_INNER ** -0.5)
    ssd_w_in = nrm((ns, D_MODEL, SSD_IN_DIM), D_MODEL ** -0.5)
    ssd_conv_w = nrm((ns, CONV_W, SSD_CONV_DIM), CONV_W ** -0.5)
    ssd_conv_b = nrm((ns, SSD_CONV_DIM), 0.02)
    dt0 = jnp.exp(jax.random.uniform(next(ks), (ns, 2, SSD_HEADS), f32, math.log(1e-3), math.log(1e-1)))
    ssd_dt_bias = dt0 + jnp.log(-jnp.expm1(-dt0))
    ssd_a_log = jnp.log(jax.random.uniform(next(ks), (ns, 2, SSD_HEADS), f32, 1.0, 16.0))
    ssd_d = 1.0 + nrm((ns, SSD_HEADS), 0.1)
    ssd_norm_g = 1.0 + nrm((ns, SSD_INNER), 0.02)
    ssd_w_out = nrm((ns, SSD_INNER, D_MODEL), SSD_INNER ** -0.5)
    return {'x': x, 'c': c, 'ctx': ctx, 'c_ctx': c_ctx,
            'ada_w': ada_w, 'ada_b': ada_b, 'norm_g': norm_g,
            'ffn_w_gate': ffn_w_gate, 'ffn_w_up': ffn_w_up, 'ffn_w_down': ffn_w_down,
            'mlstm_w_in': mlstm_w_in, 'mlstm_conv_w': mlstm_conv_w, 'mlstm_conv_b': mlstm_conv_b,
            'mlstm_w_q': mlstm_w_q, 'mlstm_w_k': mlstm_w_k, 'mlstm_w_v': mlstm_w_v,
            'mlstm_w_gates': mlstm_w_gates, 'mlstm_b_gates': mlstm_b_gates,
            'mlstm_norm_g': mlstm_norm_g, 'mlstm_skip': mlstm_skip, 'mlstm_w_out': mlstm_w_out,
            'ssd_w_in': ssd_w_in, 'ssd_conv_w': ssd_conv_w, 'ssd_conv_b': ssd_conv_b,
            'ssd_dt_bias': ssd_dt_bias, 'ssd_a_log': ssd_a_log, 'ssd_d': ssd_d,
            'ssd_norm_g': ssd_norm_g, 'ssd_w_out': ssd_w_out}


def reference(x, c, ctx, c_ctx, ada_w, ada_b, norm_g, ffn_w_gate, ffn_w_up, ffn_w_down,
              mlstm_w_in, mlstm_conv_w, mlstm_conv_b, mlstm_w_q, mlstm_w_k, mlstm_w_v,
              mlstm_w_gates, mlstm_b_gates, mlstm_norm_g, mlstm_skip, mlstm_w_out,
              ssd_w_in, ssd_conv_w, ssd_conv_b, ssd_dt_bias, ssd_a_log, ssd_d,
              ssd_norm_g, ssd_w_out):
    bsz = x.shape[0]
    rows = x.shape[1] // GRID_W
    sc = jax.nn.silu(c)
    scc = jax.nn.silu(c_ctx)
    for i in range(DEPTH):
        last = i == DEPTH - 1
        mod_l = (sc @ ada_w[i] + ada_b[i]).reshape(bsz, 1, N_MOD, D_MODEL)
        mod_c = (scc @ ada_w[i] + ada_b[i]).reshape(1, 1, N_MOD, D_MODEL)
        g = norm_g[i]
        x = ffn_sub(x, mod_l, 0, g[0], g[1], ffn_w_gate[i, 0], ffn_w_up[i, 0], ffn_w_down[i, 0])
        ctx = ffn_sub(ctx, mod_c, 0, g[0], g[1], ffn_w_gate[i, 0], ffn_w_up[i, 0], ffn_w_down[i, 0])
        hl = modulate(rmsnorm(x, g[2]), mod_l[:, :, 3], mod_l[:, :, 4])
        hc = modulate(rmsnorm(ctx, g[2]), mod_c[:, :, 3], mod_c[:, :, 4])
        j = i // N_MIXERS
        if i % N_MIXERS == 0:
            yc, yl = mlstm_mixer(hc, hl, rows, not last, mlstm_w_in[j], mlstm_conv_w[j], mlstm_conv_b[j],
                                 mlstm_w_q[j], mlstm_w_k[j], mlstm_w_v[j], mlstm_w_gates[j],
                                 mlstm_b_gates[j], mlstm_norm_g[j], mlstm_skip[j], mlstm_w_out[j])
        else:
            yc, yl = ssd_mixer(hc, hl, rows, not last, ssd_w_in[j], ssd_conv_w[j], ssd_conv_b[j],
                               ssd_dt_bias[j], ssd_a_log[j], ssd_d[j], ssd_norm_g[j], ssd_w_out[j])
        x = x + mod_l[:, :, 5] * rmsnorm(yl, g[3])
        x = ffn_sub(x, mod_l, 6, g[4], g[5], ffn_w_gate[i, 1], ffn_w_up[i, 1], ffn_w_down[i, 1])
        if not last:
            ctx = ctx + mod_c[:, :, 5] * rmsnorm(yc, g[3])
            ctx = ffn_sub(ctx, mod_c, 6, g[4], g[5], ffn_w_gate[i, 1], ffn_w_up[i, 1], ffn_w_down[i, 1])
    return x
```

```python
from contextlib import ExitStack
import numpy as np
import concourse.bass as bass
import concourse.mybir as mybir
from concourse.bass_utils import run_bass_kernel_spmd

F32 = mybir.dt.float32
BF16 = mybir.dt.bfloat16
ALU = mybir.AluOpType
AF = mybir.ActivationFunctionType
AX = mybir.AxisListType

D = 1024
KC = D // 128
DFF = 2816
FC = DFF // 128
EPS = 1e-6
NCORES = 8


class Buf:
    __slots__ = ("name", "w", "r", "dram", "dsem", "psum")

    def __init__(self, name, dram=False, psum=False):
        self.name = name
        self.w = {}
        self.r = {}
        self.dram = dram
        self.dsem = None
        self.psum = psum


class Tile:
    def __init__(self, h, buf):
        self.h = h
        self.b = buf

    def __getitem__(self, k):
        return self.h[k]


class Ctx:
    ENG = ("pe", "dve", "act", "pool", "sp")

    def __init__(self):
        nc = self.nc = bass.Bass("TRN2", target_bir_lowering=False)
        self.engs = dict(pe=nc.tensor, dve=nc.vector, act=nc.scalar, pool=nc.gpsimd, sp=nc.sync)
        self.semh = {}
        self.owner = {}
        self.cur = {}
        self.cnt = {}
        self.dcnt = {}
        self.seen = {e: {} for e in self.ENG}
        self.gen = 0
        for e in self.ENG:
            self._newsem(e)
        self.n_ins = 0
        self.n_wait = 0
        self.free_dsems = []

    def _newsem(self, e):
        self.gen += 1
        name = f"s_{e}_{self.gen}"
        self.semh[name] = self.nc.alloc_semaphore(name)
        self.owner[name] = e
        self.cur[e] = name
        self.cnt[e] = 0

    def sb(self, es, name, shape, dt):
        self.uid = getattr(self, "uid", 0) + 1
        name = f"{name}_{self.uid}"
        h = es.enter_context(self.nc.sbuf_tensor(name, list(shape), dt))
        t = Tile(h, Buf(name))
        es.callback(self._release, t.b)
        return t

    def _release(self, buf):
        if buf.dsem is not None:
            self.free_dsems.append(buf.dsem)
            buf.dsem = None

    def psum(self, es, name, shape, dt=F32):
        h = es.enter_context(self.nc.psum_tensor(name, list(shape), dt))
        return Tile(h, Buf(name, psum=True))

    def dram(self, name, shape, dt, kind="Internal"):
        h = self.nc.dram_tensor(name, list(shape), dt, kind=kind)
        return h

    def _val(self, name, val):
        return 16 * self.dcnt[name] if val is None else val

    def _gather(self, e, rd, wr):
        toks = {}

        def add(name, val, skip_same):
            if skip_same and self.owner[name] == e:
                return
            v = self._val(name, val)
            if toks.get(name, 0) < v:
                toks[name] = v

        pe = e == "pe"
        for b in rd:
            for n, v in b.w.items():
                add(n, v, pe)
            if b.psum:
                for n, v in b.r.items():
                    if self.owner[n] != e:
                        add(n, v, False)
        for b in wr:
            for n, v in b.w.items():
                add(n, v, pe)
            for n, v in b.r.items():
                add(n, v, pe)
        return toks

    def _wait(self, e, toks):
        for name, v in toks.items():
            if self.seen[e].get(name, 0) < v:
                self.engs[e].wait_ge(self.semh[name], v)
                self.seen[e][name] = v
                self.n_wait += 1

    def op(self, e, fn, rd=(), wr=()):
        rd = [t.b if isinstance(t, Tile) else t for t in rd]
        wr = [t.b if isinstance(t, Tile) else t for t in wr]
        self._wait(e, self._gather(e, rd, wr))
        ins = fn(self.engs[e])
        name = self.cur[e]
        self.cnt[e] += 1
        ins.then_inc(self.semh[name], 1)
        v = self.cnt[e]
        for b in rd:
            b.r[name] = v
        for b in wr:
            b.w = {name: v}
            b.r = {}
        if v >= 30000:
            self._newsem(e)
        self.n_ins += 1
        return ins

    def dma(self, q, out, in_, rd=(), wr=(), owner=None):
        rd = [t.b if isinstance(t, Tile) else t for t in rd]
        wr = [t.b if isinstance(t, Tile) else t for t in wr]
        owner = owner.b if isinstance(owner, Tile) else owner
        self._wait(q, self._gather("dma", rd, wr))
        ins = self.engs[q].dma_start(out=out, in_=in_)
        if owner.dsem is None:
            if self.free_dsems:
                name = self.free_dsems.pop()
            else:
                name = f"d_{owner.name}"
                self.semh[name] = self.nc.alloc_semaphore(name)
                self.owner[name] = None
                self.dcnt[name] = 0
            owner.dsem = name
        name = owner.dsem
        self.dcnt[name] += 1
        ins.then_inc(self.semh[name], 16)
        for b in rd:
            b.r[name] = None
        for b in wr:
            if b.dram:
                b.w[name] = None
            else:
                b.w = {name: None}
                b.r = {}
        self.n_ins += 1
        return ins

    def barrier(self):
        toks = {}
        for e in self.ENG:
            if self.cnt[e] > 0:
                toks[self.cur[e]] = self.cnt[e]
        for n, c in self.dcnt.items():
            if c > 0:
                toks[n] = 16 * c
        for e in self.ENG:
            t = {n: v for n, v in toks.items() if self.owner[n] != e}
            self._wait(e, t)


def load_weight_bf16(cx, q, Wd_ap, Wbf, stg, col0, ncol, kc):
    src = Wd_ap.rearrange("(kc p) m -> p kc m", p=128)[:, :, col0:col0 + ncol]
    cx.dma(q, stg[:, 0:kc, 0:ncol], src, wr=[stg], owner=stg)
    cx.op("pool", lambda g: g.tensor_copy(out=Wbf[:, :, col0:col0 + ncol], in_=stg[:, 0:kc, 0:ncol]),
          rd=[stg], wr=[Wbf.b if isinstance(Wbf, Tile) else Wbf])


class Net:
    def __init__(self, cx, T_lat=4096, T_ctx=256, depth=2, tile_n=512, debug_stop=None):
        self.cx = cx
        self.nc = cx.nc
        self.TL, self.TC = T_lat, T_ctx
        self.T = T_lat + T_ctx
        self.depth = depth
        self.debug_stop = debug_stop
        self.tiles = [(s, tile_n, 0) for s in range(0, T_lat, tile_n)] + [(T_lat, T_ctx, 1)]
        self.declare_io()

    def declare_io(self):
        nc, T = self.nc, self.T
        di = lambda n, s: nc.dram_tensor(n, list(s), F32, kind="ExternalInput")
        self.xin = di("xin", [D, T])
        self.cT = di("cT", [128, KC, 2])
        self.ada_w = di("ada_w", [self.depth, D, 9 * D])
        self.ada_bT = di("ada_bT", [self.depth, 128, 72])
        self.gT = di("gT", [self.depth, 128, 6, KC])
        self.w_gate = di("w_gate", [self.depth, 2, D, DFF])
        self.w_up = di("w_up", [self.depth, 2, D, DFF])
        self.w_down = di("w_down", [self.depth, 2, DFF, D])
        self.ml_w_in = di("ml_w_in", [D, 4096])
        self.ml_conv = di("ml_conv", [128, 16, 6])
        self.ml_bd = di("ml_bd", [3, 128, 16, 128])
        self.ml_wg = di("ml_wg", [128, 48, 16])
        self.ml_bg = di("ml_bg", [16, 1])
        self.ml_ns = di("ml_ns", [128, 2, 16])
        self.ml_w_out = di("ml_w_out", [2048, D])
        self.sd_w_in = di("sd_w_in", [D, 6208])
        self.sd_conv = di("sd_conv", [128, 32, 6])
        self.sd_smallT = di("sd_smallT", [128, 160])
        self.sd_ng = di("sd_ng", [128, 2048])
        self.sd_w_out = di("sd_w_out", [2048, D])
        self.out = nc.dram_tensor("out", [D, self.TL], F32, kind="ExternalOutput")
        NCH = T // 128
        self.NCH = NCH
        dm = lambda n, s, dt: nc.dram_tensor(n, list(s), dt, kind="ExternalOutput" if self.debug_stop else "Internal")
        self.szT = dm("szT", [2048, T], BF16)
        self.xcsT = dm("xcsT", [2048, T], BF16)
        self.qT = dm("qT", [2048, T], BF16)
        self.kT = dm("kT", [2048, T], BF16)
        self.ktm = dm("ktm", [T, 2048], BF16)
        self.vtm = dm("vtm", [T, 2048], BF16)
        self.hfwd = dm("hfwd", [T, 2048], F32)
        self.sztm = dm("sztm", [T, 2048], BF16)
        self.xtm = dm("xtm", [T, 2048], BF16)
        self.Btm = dm("Btm", [T, 1024], BF16)
        self.BT = dm("BT", [1024, T], BF16)
        self.CT = dm("CT", [1024, T], BF16)
        self.yT = nc.dram_tensor("yT", [2048, T], BF16,
                                 kind="ExternalOutput" if self.debug_stop else "Internal")
        self.yT_b = [Buf(f"yT{i}", dram=True) for i in range(len(self.tiles))]
        self.xres = nc.dram_tensor("xres", [D, T], F32,
                                   kind="ExternalOutput" if self.debug_stop else "Internal")
        self.uT = nc.dram_tensor("uT", [DFF, T], BF16, kind="Internal")
        nt = len(self.tiles)
        self.xres_b = [Buf(f"xres{i}", dram=True) for i in range(nt)]
        self.xin_b = [Buf(f"xin{i}", dram=True) for i in range(nt)]
        self.out_b = [Buf(f"out{i}", dram=True) for i in range(nt)]
        self.uT_b = [Buf(f"uT{i}", dram=True) for i in range(nt)]

    def build(self):
        cx = self.cx
        with ExitStack() as es:
            self.es0 = es
            self.ps = [cx.psum(es, f"ps{i}", [128, 512]) for i in range(8)]
            self.ones_bf = cx.sb(es, "ones_bf", [128, 128], BF16)
            cx.op("pool", lambda g: g.memset(self.ones_bf[:], 1.0 / D), wr=[self.ones_bf])
            self.eps_col = cx.sb(es, "eps_col", [128, 1], F32)
            cx.op("pool", lambda g: g.memset(self.eps_col[:], EPS), wr=[self.eps_col])
            self.one_col = cx.sb(es, "one_col", [128, 1], F32)
            cx.op("pool", lambda g: g.memset(self.one_col[:], 1.0), wr=[self.one_col])
            self.ones_f = cx.sb(es, "ones_f", [128, 128], F32)
            cx.op("pool", lambda g: g.memset(self.ones_f[:], 1.0), wr=[self.ones_f])
            self.ident = cx.sb(es, "ident", [128, 128], F32)
            self.maskU = cx.sb(es, "maskU", [128, 128], F32)
            self.maskL = cx.sb(es, "maskL", [128, 128], F32)
            self.identb = cx.sb(es, "identb", [128, 128], BF16)
            for t_, pat, cm, cmp_ in ((self.ident, [[1, 128]], -1, ALU.is_equal), (self.maskU, [[1, 128]], -1, ALU.is_ge),
                                      (self.maskL, [[-1, 128]], 1, ALU.is_ge)):
                cx.op("pool", lambda g, t_=t_, pat=pat, cm=cm, cmp_=cmp_: g.affine_select(
                    out=t_[:], in_=self.ones_f[:], pattern=pat, compare_op=cmp_, fill=0.0, base=0,
                    channel_multiplier=cm), rd=[self.ones_f], wr=[t_])
            cx.op("pool", lambda g: g.tensor_copy(out=self.identb[:], in_=self.ident[:]), rd=[self.ident],
                  wr=[self.identb])
            self.nmaskU = cx.sb(es, "nmaskU", [128, 128], F32)
            self.nmaskL = cx.sb(es, "nmaskL", [128, 128], F32)
            for nm_, mk_ in ((self.nmaskU, self.maskU), (self.nmaskL, self.maskL)):
                cx.op("pool", lambda g, nm_=nm_, mk_=mk_: g.tensor_scalar(
                    out=nm_[:], in0=mk_[:], scalar1=-1.0, scalar2=None, op0=ALU.mult), rd=[mk_], wr=[nm_])
            self.negU = cx.sb(es, "negU", [128, 128], BF16)
            self.negL = cx.sb(es, "negL", [128, 128], BF16)
            for ng_, mk_ in ((self.negU, self.maskU), (self.negL, self.maskL)):
                cx.op("pool", lambda g, ng_=ng_, mk_=mk_: g.tensor_scalar(
                    out=ng_[:], in0=mk_[:], scalar1=-1.0, scalar2=30000.0, op0=ALU.add, op1=ALU.mult),
                    rd=[mk_], wr=[ng_])
            self.mod = cx.sb(es, "mod", [128, 72, 2], F32)
            self.par = cx.sb(es, "par", [128, 9, KC, 2], F32)
            self.sc2 = cx.sb(es, "sc2", [128, KC, 2], F32)
            self.gsb = cx.sb(es, "gsb", [128, 6, KC], F32)
            self.adab = cx.sb(es, "adab", [128, 72], F32)
            cx.dma("sp", self.sc2[:], self.cT[:], wr=[self.sc2], owner=self.sc2)
            cx.op("act", lambda a: a.activation(out=self.sc2[:], in_=self.sc2[:], func=AF.Silu),
                  rd=[self.sc2], wr=[self.sc2])
            for li in range(self.depth):
                if self.layer(li):
                    break
            cx.barrier()

    def layer(self, li):
        last = li == self.depth - 1
        ds = self.debug_stop
        self.adaln(li)
        src = (self.xin, self.xin_b) if li == 0 else (self.xres, self.xres_b)
        self.ffn(li, 0, 0, src, (self.xres, self.xres_b), ctx_needed=True)
        if ds == (li, "ffn1"):
            return True
        if li % 2 == 0:
            self.mlstm(li, not last)
        else:
            self.ssd(li, not last)
        if ds and ds[0] == li and ds[1] in ("ml_in", "mixer", "ssd_tab", "ssd_d0"):
            return True
        dst = (self.out, self.out_b) if (last and not ds) else (self.xres, self.xres_b)
        self.ffn(li, 1, 2, (self.xres, self.xres_b), dst, ctx_needed=not last)
        return ds == (li, "layer")

    def adaln(self, li):
        cx = self.cx
        cx.barrier()
        with ExitStack() as es:
            NG = 1152
            stg = [cx.sb(es, f"adastg{i}", [128, KC, NG], F32) for i in range(2)]
            cx.dma("sp", self.adab[:], self.ada_bT[li], wr=[self.adab], owner=self.adab)
            cx.dma("sp", self.gsb[:], self.gT[li], wr=[self.gsb], owner=self.gsb)
            mps = self.ps[0]
            for gi in range(9 * D // NG):
                st = stg[gi % 2]
                src = self.ada_w[li].rearrange("(kc p) m -> p kc m", p=128)[:, :, gi * NG:(gi + 1) * NG]
                cx.dma("sp", st[:], src, wr=[st], owner=st)
                for j in range(NG // 128):
                    mc = gi * (NG // 128) + j
                    for k in range(KC):
                        cx.op("pe", lambda p, k=k, j=j, mc=mc, st=st: p.matmul(
                            mps[:, 2 * mc:2 * mc + 2], lhsT=st[:, k, j * 128:(j + 1) * 128],
                            rhs=self.sc2[:, k, :], start=(k == 0), stop=(k == KC - 1)),
                            rd=[st, self.sc2], wr=[mps])
            cx.op("dve", lambda v: v.tensor_tensor(
                out=self.mod[:], in0=mps[:, 0:144].rearrange("p (m c) -> p m c", c=2),
                in1=self.adab[:].unsqueeze(2).to_broadcast([128, 72, 2]), op=ALU.add),
                rd=[mps, self.adab], wr=[self.mod])
            md = lambda j: self.mod[:, j * KC:(j + 1) * KC, :]
            gb = lambda j: self.gsb[:, j, :].unsqueeze(2).to_broadcast([128, KC, 2])
            P = self.par
            spec = [
                (0, "A", 1, 0, 1.0), (1, "B", 0, None, 1.0), (2, "G", 2, 1, 0.5),
                (3, "A", 4, 2, 1.0), (4, "B", 3, None, 1.0), (5, "G", 5, 3, 1.0),
                (6, "A", 7, 4, 1.0), (7, "B", 6, None, 1.0), (8, "G", 8, 5, 0.5)]
            for idx, kind, mj, gj, fac in spec:
                if kind == "A":
                    cx.op("dve", lambda v, idx=idx, mj=mj, gj=gj: v.scalar_tensor_tensor(
                        out=P[:, idx], in0=md(mj), scalar=1.0, in1=gb(gj), op0=ALU.add, op1=ALU.mult),
                        rd=[self.mod, self.gsb], wr=[P])
                elif kind == "B":
                    cx.op("dve", lambda v, idx=idx, mj=mj: v.tensor_copy(out=P[:, idx], in_=md(mj)),
                          rd=[self.mod], wr=[P])
                else:
                    cx.op("dve", lambda v, idx=idx, mj=mj, gj=gj, fac=fac: v.scalar_tensor_tensor(
                        out=P[:, idx], in0=md(mj), scalar=fac, in1=gb(gj), op0=ALU.mult, op1=ALU.mult),
                        rd=[self.mod, self.gsb], wr=[P])

    def rsqrt_eps(self, rstd, ms_ps, n):
        cx = self.cx
        cx.op("act", lambda a: a.activation(out=rstd[:, 0:n], in_=ms_ps[:, 0:n], func=AF.Sqrt,
                                            bias=self.eps_col[:, 0:1], scale=1.0), rd=[ms_ps, self.eps_col],
              wr=[rstd])
        cx.op("dve", lambda v: v.reciprocal(out=rstd[:, 0:n], in_=rstd[:, 0:n]), rd=[rstd], wr=[rstd])

    def norm_mod(self, es_bufs, x, sq, hT, n, c, pidx, ssq_ps, rstd):
        cx = self.cx
        cx.op("act", lambda a: a.activation(out=sq[:, :, 0:n], in_=x[:, :, 0:n], func=AF.Square),
              rd=[x], wr=[sq])
        for k in range(KC):
            cx.op("pe", lambda p, k=k: p.matmul(ssq_ps[:, 0:n], lhsT=self.ones_bf[:], rhs=sq[:, k, 0:n],
                                                start=(k == 0), stop=(k == KC - 1)),
                  rd=[self.ones_bf, sq], wr=[ssq_ps])
        self.rsqrt_eps(rstd, ssq_ps, n)
        cx.op("dve", lambda v: v.tensor_tensor(
            out=x[:, :, 0:n], in0=x[:, :, 0:n],
            in1=rstd[:, 0:n].unsqueeze(1).to_broadcast([128, KC, n]), op=ALU.mult), rd=[x, rstd], wr=[x])
        for k in range(KC):
            cx.op("act", lambda a, k=k: a.activation(
                out=hT[:, k, 0:n], in_=x[:, k, 0:n], func=AF.Identity,
                scale=self.par[:, pidx, k, c:c + 1], bias=self.par[:, pidx + 1, k, c:c + 1]),
                rd=[x, self.par], wr=[hT])

    def ffn(self, li, half, pbase, src, dst, ctx_needed):
        cx = self.cx
        tiles = self.tiles if ctx_needed else self.tiles[:-1]
        pidx = pbase * 3
        srcT, srcB = src
        dstT, dstB = dst
        xv = lambda t: t.ap().rearrange("(kc p) t -> p kc t", p=128)
        cx.barrier()
        with ExitStack() as es:
            Wg = cx.sb(es, "Wg", [128, KC, DFF], BF16)
            Wu = cx.sb(es, "Wu", [128, KC, DFF], BF16)
            NGC = 352
            ngrp = DFF // NGC
            Wgb = [Buf(f"Wg{i}") for i in range(ngrp)]
            Wub = [Buf(f"Wu{i}") for i in range(ngrp)]
            stg = [cx.sb(es, f"wstg{i}", [128, KC, NGC], F32) for i in range(2)]
            xt = [cx.sb(es, f"xt{i}", [128, KC, 512], F32) for i in range(2)]
            hT = [cx.sb(es, f"hT{i}", [128, KC, 512], BF16) for i in range(2)]
            sq = cx.sb(es, "sq", [128, KC, 512], BF16)
            rstd = cx.sb(es, "rstd", [128, 512], F32)
            sg = [cx.sb(es, f"sg{i}", [128, 512], F32) for i in range(2)]
            uo = [cx.sb(es, f"uo{i}", [128, FC // 2, 512], BF16) for i in range(2)]
            ssq_ps = self.ps[0]
            gps = [self.ps[1], self.ps[2]]
            ups = [self.ps[3], self.ps[4]]
            def load_x(i):
                s, n, c = tiles[i]
                cx.dma("sp", xt[i % 2][:, :, 0:n], xv(srcT)[:, :, s:s + n], rd=[srcB[i]], wr=[xt[i % 2]],
                       owner=xt[i % 2])
            load_x(0)
            si = 0
            for g in range(ngrp):
                for (Wdram, Wsb, Wb) in ((self.w_gate, Wg, Wgb), (self.w_up, Wu, Wub)):
                    st = stg[si % 2]
                    si += 1
                    srcw = Wdram[li, half].rearrange("(kc p) m -> p kc m", p=128)[:, :, g * NGC:(g + 1) * NGC]
                    cx.dma("sp", st[:], srcw, wr=[st], owner=st)
                    cx.op("pool", lambda gp, Wsb=Wsb, st=st, g=g: gp.tensor_copy(
                        out=Wsb[:, :, g * NGC:(g + 1) * NGC], in_=st[:]), rd=[st], wr=[Wb[g]])
            for i, (s, n, c) in enumerate(tiles):
                if i + 1 < len(tiles):
                    load_x(i + 1)
                x = xt[i % 2]
                h = hT[i % 2]
                self.norm_mod(None, x, sq, h, n, c, pidx, ssq_ps, rstd)
                for f in range(FC):
                    gp_, up_ = gps[f % 2], ups[f % 2]
                    wb = [Wgb[(f * 128) // NGC], Wgb[(f * 128 + 127) // NGC]]
                    wb2 = [Wub[(f * 128) // NGC], Wub[(f * 128 + 127) // NGC]]
                    for k in range(KC):
                        cx.op("pe", lambda p, k=k, f=f, gp_=gp_: p.matmul(
                            gp_[:, 0:n], lhsT=Wg[:, k, f * 128:(f + 1) * 128], rhs=h[:, k, 0:n],
                            start=(k == 0), stop=(k == KC - 1)), rd=[h] + wb, wr=[gp_])
                    for k in range(KC):
                        cx.op("pe", lambda p, k=k, f=f, up_=up_: p.matmul(
                            up_[:, 0:n], lhsT=Wu[:, k, f * 128:(f + 1) * 128], rhs=h[:, k, 0:n],
                            start=(k == 0), stop=(k == KC - 1)), rd=[h] + wb2, wr=[up_])
                    sgt = sg[f % 2]
                    cx.op("act", lambda a, gp_=gp_, sgt=sgt: a.activation(out=sgt[:, 0:n], in_=gp_[:, 0:n],
                                                                          func=AF.Silu), rd=[gp_], wr=[sgt])
                    uh = uo[f // (FC // 2)]
                    fo = f % (FC // 2)
                    cx.op("dve", lambda v, up_=up_, sgt=sgt, uh=uh, fo=fo: v.tensor_tensor(
                        out=uh[:, fo, 0:n], in0=up_[:, 0:n], in1=sgt[:, 0:n], op=ALU.mult),
                        rd=[up_, sgt], wr=[uh])
                    if fo == FC // 2 - 1:
                        hh = f // (FC // 2)
                        dv = self.uT.ap().rearrange("(fc p) t -> p fc t", p=128)[
                            :, hh * (FC // 2):(hh + 1) * (FC // 2), s:s + n]
                        cx.dma("sp", dv, uh[:, :, 0:n], rd=[uh], wr=[self.uT_b[i]], owner=uh)
        self.proj_res(self.w_down[li, half], FC, self.uT, self.uT_b, pidx + 2, src, dst, tiles)

    def proj_res(self, W_ap, kc, inT, inB, gidx, src, dst, tiles):
        cx = self.cx
        srcT, srcB = src
        dstT, dstB = dst
        xv = lambda t: t.ap().rearrange("(kc p) t -> p kc t", p=128)
        cx.barrier()
        with ExitStack() as es:
            Wd = cx.sb(es, "Wd", [128, kc, D], BF16)
            NGC = 256
            ngrp = D // NGC
            Wdb = [Buf(f"Wd{i}") for i in range(ngrp)]
            stg = [cx.sb(es, f"wstg{i}", [128, kc, NGC], F32) for i in range(2)]
            ut = [cx.sb(es, f"ut{i}", [128, kc, 512], BF16) for i in range(2)]
            xt = [cx.sb(es, f"xt{i}", [128, KC, 512], F32) for i in range(2)]
            y = cx.sb(es, "y", [128, KC, 512], F32)
            sq = cx.sb(es, "sq", [128, KC, 512], BF16)
            rstd = cx.sb(es, "rstd", [128, 512], F32)
            ssq_ps = self.ps[0]
            yps = [self.ps[1], self.ps[2], self.ps[3]]

            def load_t(i):
                s, n, c = tiles[i]
                uv = inT.ap().rearrange("(fc p) t -> p fc t", p=128)[:, :, s:s + n]
                cx.dma("sp", ut[i % 2][:, :, 0:n], uv, rd=[inB[i]], wr=[ut[i % 2]], owner=ut[i % 2])
                cx.dma("sp", xt[i % 2][:, :, 0:n], xv(srcT)[:, :, s:s + n], rd=[srcB[i]], wr=[xt[i % 2]],
                       owner=xt[i % 2])
            load_t(0)
            for g in range(ngrp):
                st = stg[g % 2]
                srcw = W_ap.rearrange("(fc p) m -> p fc m", p=128)[:, :, g * NGC:(g + 1) * NGC]
                cx.dma("sp", st[:], srcw, wr=[st], owner=st)
                cx.op("pool", lambda gp, st=st, g=g: gp.tensor_copy(
                    out=Wd[:, :, g * NGC:(g + 1) * NGC], in_=st[:]), rd=[st], wr=[Wdb[g]])
            for i, (s, n, c) in enumerate(tiles):
                if i + 1 < len(tiles):
                    load_t(i + 1)
                u, x = ut[i % 2], xt[i % 2]
                for d in range(KC):
                    yp = yps[d % 3]
                    for f in range(kc):
                        cx.op("pe", lambda p, d=d, f=f, yp=yp: p.matmul(
                            yp[:, 0:n], lhsT=Wd[:, f, d * 128:(d + 1) * 128], rhs=u[:, f, 0:n],
                            start=(f == 0), stop=(f == kc - 1)), rd=[u, Wdb[(d * 128) // NGC]], wr=[yp])
                    cx.op("act", lambda a, d=d, yp=yp: a.activation(out=y[:, d, 0:n], in_=yp[:, 0:n],
                                                                    func=AF.Copy), rd=[yp], wr=[y])
                self.post_norm_res(y, sq, rstd, ssq_ps, x, n, c, gidx)
                sdst = dstT.ap().rearrange("(kc p) t -> p kc t", p=128)[:, :, s:s + n]
                cx.dma("sp", sdst, x[:, :, 0:n], rd=[x], wr=[dstB[i]], owner=x)

    def post_norm_res(self, y, sq, rstd, ssq_ps, x, n, c, gidx):
        cx = self.cx
        cx.op("act", lambda a: a.activation(out=sq[:, :, 0:n], in_=y[:, :, 0:n], func=AF.Square),
              rd=[y], wr=[sq])
        for k in range(KC):
            cx.op("pe", lambda p, k=k: p.matmul(ssq_ps[:, 0:n], lhsT=self.ones_bf[:], rhs=sq[:, k, 0:n],
                                                start=(k == 0), stop=(k == KC - 1)),
                  rd=[self.ones_bf, sq], wr=[ssq_ps])
        self.rsqrt_eps(rstd, ssq_ps, n)
        cx.op("dve", lambda v: v.tensor_tensor(
            out=y[:, :, 0:n], in0=y[:, :, 0:n],
            in1=rstd[:, 0:n].unsqueeze(1).to_broadcast([128, KC, n]), op=ALU.mult), rd=[y, rstd], wr=[y])
        for k in range(KC):
            cx.op("dve", lambda v, k=k: v.scalar_tensor_tensor(
                out=x[:, k, 0:n], in0=y[:, k, 0:n], scalar=self.par[:, gidx, k, c:c + 1], in1=x[:, k, 0:n],
                op0=ALU.mult, op1=ALU.add), rd=[y, x, self.par], wr=[x])


    def chunk_order(self, d):
        nl = self.TL // 128
        nc_ = self.TC // 128
        ctx = list(range(nl, nl + nc_))
        lat = list(range(nl))
        return ctx + lat if d == 0 else ctx[::-1] + lat[::-1]

    def mlstm(self, li, need_ctx):
        cx = self.cx
        T, NCH = self.T, self.NCH
        cx.barrier()
        with ExitStack() as es:
            self.gT16 = cx.sb(es, "gT16", [16, T], F32)
            self.ml_ns_sb = cx.sb(es, "ml_ns_sb", [128, 2, 16], F32)
            cx.dma("sp", self.ml_ns_sb[:], self.ml_ns.ap(), wr=[self.ml_ns_sb], owner=self.ml_ns_sb)
            self.Wtm = [cx.sb(es, f"Wtm{d}", [128, NCH, 4], F32) for d in range(2)]
            self.Wtmb = [cx.sb(es, f"Wtmb{d}", [128, NCH, 4], BF16) for d in range(2)]
            self.Ftm = [cx.sb(es, f"Ftm{d}", [128, NCH, 4], F32) for d in range(2)]
            self.decbc = [cx.sb(es, f"decbc{d}", [128, NCH, 4], F32) for d in range(2)]
            self.mlstm_in(li)
            if self.debug_stop == (li, "ml_in"):
                return
            self.mlstm_gates()
            for d in range(2):
                self.mlstm_scan(d)
        tiles = self.tiles if need_ctx else self.tiles[:-1]
        self.proj_res(self.ml_w_out.ap(), 16, self.yT, self.yT_b, 5, (self.xres, self.xres_b),
                      (self.xres, self.xres_b), tiles)

    def mlstm_in(self, li):
        cx = self.cx
        tiles = self.tiles
        cx.barrier()
        with ExitStack() as es:
            Win = cx.sb(es, "Win", [128, KC, 4096], BF16)
            Winb = [Buf(f"Win{g}") for g in range(8)]
            bd = cx.sb(es, "bd", [128, 3, 16, 128], BF16)
            wg = cx.sb(es, "wg", [128, 48, 16], BF16)
            conv = cx.sb(es, "conv", [128, 16, 6], F32)
            bg = cx.sb(es, "bg", [16, 1], F32)
            stg = [cx.sb(es, f"wstg{i}", [128, KC, 128], F32) for i in range(2)]
            sstg = Tile(stg[0][:, :, :].rearrange("p a b -> p (a b)"), stg[0].b)
            x = cx.sb(es, "x", [128, KC, 512], F32)
            hT = cx.sb(es, "hT", [128, KC, 512], BF16)
            sq = cx.sb(es, "sq", [128, KC, 512], BF16)
            rstd = cx.sb(es, "rstd", [128, 512], F32)
            xmf = [cx.sb(es, f"xmf{i}", [128, 512], F32) for i in range(2)]
            acc = [cx.sb(es, f"acc{i}", [128, 512], F32) for i in range(2)]
            xmb = [cx.sb(es, f"xmb{i}", [128, 512], BF16) for i in range(2)]
            vTs = [cx.sb(es, f"vTs{i}", [128, 512], BF16) for i in range(3)]
            xcb = [cx.sb(es, f"xcb{i}", [128, 4, 512], BF16) for i in range(2)]
            mk = lambda nm, k=2: [cx.sb(es, f"{nm}{i}", [128, 4, 512], BF16) for i in range(k)] * (2 // k)
            szo, xcso, qTo, kTo, ktmo, vtmo = mk("szo", 1), mk("xcso", 1), mk("qTo"), mk("kTo"), mk("ktmo"), mk("vtmo")
            cx.dma("sp", conv[:], self.ml_conv.ap(), wr=[conv], owner=conv)
            cx.dma("sp", bg[:], self.ml_bg.ap(), wr=[bg], owner=bg)
            for i in range(3):
                for hf_ in range(2):
                    cx.dma("sp", sstg[:, :].rearrange("p (a b) -> p a b", b=128),
                           self.ml_bd[i][:, hf_ * 8:(hf_ + 1) * 8, :], wr=[sstg], owner=sstg)
                    cx.op("pool", lambda g, i=i, hf_=hf_: g.tensor_copy(
                        out=bd[:, i, hf_ * 8:(hf_ + 1) * 8, :],
                        in_=sstg[:, :].rearrange("p (a b) -> p a b", b=128)), rd=[sstg], wr=[bd])
            cx.dma("sp", sstg[:, 0:768].rearrange("p (a b) -> p a b", b=16), self.ml_wg.ap(), wr=[sstg], owner=sstg)
            cx.op("pool", lambda g: g.tensor_copy(out=wg[:], in_=sstg[:, 0:768].rearrange(
                "p (a b) -> p a b", b=16)), rd=[sstg], wr=[wg])

            def load_x(i):
                s, n, c = tiles[i]
                cx.dma("sp", x[:, :, 0:n], self.xres.ap().rearrange("(kc p) t -> p kc t", p=128)[:, :, s:s + n],
                       rd=[self.xres_b[i]], wr=[x], owner=x)
            load_x(0)
            gorder = [g for q in range(16) for g in (q, 16 + q)]
            for gi, g in enumerate(gorder):
                st = stg[gi % 2]
                srcw = self.ml_w_in.ap().rearrange("(kc p) m -> p kc m", p=128)[:, :, g * 128:(g + 1) * 128]
                cx.dma("sp", st[:], srcw, wr=[st], owner=st)
                cx.op("pool", lambda gp, st=st, g=g: gp.tensor_copy(
                    out=Win[:, :, g * 128:(g + 1) * 128], in_=st[:]), rd=[st], wr=[Winb[g // 4]])
            g_ps = self.ps[0]
            prod = [self.ps[5], self.ps[6], self.ps[7]]
            pcount = [0]

            def nextp():
                pcount[0] += 1
                return prod[pcount[0] % 3]

            for i, (s, n, c) in enumerate(tiles):
                ntb = n // 128
                self.norm_mod(None, x, sq, hT, n, c, 3, self.ps[0], rstd)
                if i + 1 < len(tiles):
                    load_x(i + 1)
                R = n if c else 64
                v3 = lambda ap: ap.rearrange("p (r w) -> p r w", w=R)

                def stageA(cc):
                    gq, jj = cc // 4, cc % 4
                    ob = gq % 2
                    xm_ps = self.ps[1 + cc % 2]
                    z_ps = self.ps[3 + cc % 2]
                    for k in range(KC):
                        cx.op("pe", lambda p, k=k: p.matmul(
                            xm_ps[:, 0:n], lhsT=Win[:, k, cc * 128:(cc + 1) * 128], rhs=hT[:, k, 0:n],
                            start=(k == 0), stop=(k == KC - 1)), rd=[hT, Winb[cc // 4]], wr=[xm_ps])
                    for k in range(KC):
                        cx.op("pe", lambda p, k=k: p.matmul(
                            z_ps[:, 0:n], lhsT=Win[:, k, 2048 + cc * 128:2048 + (cc + 1) * 128], rhs=hT[:, k, 0:n],
                            start=(k == 0), stop=(k == KC - 1)), rd=[hT, Winb[4 + cc // 4]], wr=[z_ps])
                    xf, ac, xb = xmf[cc % 2], acc[cc % 2], xmb[cc % 2]
                    cx.op("act", lambda a: a.activation(out=xf[:, 0:n], in_=xm_ps[:, 0:n], func=AF.Copy),
                          rd=[xm_ps], wr=[xf])
                    cx.op("act", lambda a: a.activation(out=xb[:, 0:n], in_=xm_ps[:, 0:n], func=AF.Copy),
                          rd=[xm_ps], wr=[xb])
                    cx.op("act", lambda a: a.activation(out=szo[ob][:, jj, 0:n], in_=z_ps[:, 0:n], func=AF.Silu),
                          rd=[z_ps], wr=[szo[ob]])
                    cx.op("dve", lambda v: v.tensor_scalar(
                        out=ac[:, 0:n], in0=xf[:, 0:n], scalar1=conv[:, cc, 2:3], scalar2=conv[:, cc, 5:6],
                        op0=ALU.mult, op1=ALU.add), rd=[xf, conv], wr=[ac])
                    for j in (0, 1, 3, 4):
                        sh = j - 2
                        o0, o1 = max(0, -sh), R - max(0, sh)
                        i0, i1 = max(0, sh), R - max(0, -sh)
                        cx.op("dve", lambda v, j=j, o0=o0, o1=o1, i0=i0, i1=i1: v.scalar_tensor_tensor(
                            out=v3(ac[:, 0:n])[:, :, o0:o1], in0=v3(xf[:, 0:n])[:, :, i0:i1],
                            scalar=conv[:, cc, j:j + 1], in1=v3(ac[:, 0:n])[:, :, o0:o1],
                            op0=ALU.mult, op1=ALU.add), rd=[xf, ac, conv], wr=[ac])
                    cx.op("act", lambda a: a.activation(out=xcb[ob][:, jj, 0:n], in_=ac[:, 0:n], func=AF.Silu),
                          rd=[ac], wr=[xcb[ob]])
                    cx.op("pool", lambda g: g.tensor_scalar(
                        out=xcso[ob][:, jj, 0:n], in0=xcb[ob][:, jj, 0:n], scalar1=self.ml_ns_sb[:, 1, cc:cc + 1],
                        scalar2=None, op0=ALU.mult), rd=[xcb[ob], self.ml_ns_sb], wr=[xcso[ob]])
                    if jj == 3:
                        fm = lambda t: t.ap().rearrange("(cc p) t -> p cc t", p=128)[:, gq * 4:(gq + 1) * 4, s:s + n]
                        for dt_, ot in ((self.szT, szo[ob]), (self.xcsT, xcso[ob])):
                            cx.dma("sp", fm(dt_), ot[:, :, 0:n], rd=[ot], owner=ot)

                def stageB(cc):
                    gq, jj = cc // 4, cc % 4
                    ob = gq % 2
                    xb = xmb[cc % 2]
                    vt = vTs[cc % 3]
                    for wi, (rhs_t, rhs_ap, dst_t, dst_ap, eng) in enumerate((
                            (xcb[ob], xcb[ob][:, jj, 0:n], qTo[ob], qTo[ob][:, jj, 0:n], "act"),
                            (xcb[ob], xcb[ob][:, jj, 0:n], kTo[ob], kTo[ob][:, jj, 0:n], "dve"),
                            (xb, xb[:, 0:n], vt, vt[:, 0:n], "act"))):
                        pp = nextp()
                        cx.op("pe", lambda p, wi=wi, rhs_ap=rhs_ap, pp=pp: p.matmul(
                            pp[:, 0:n], lhsT=bd[:, wi, cc, :], rhs=rhs_ap, start=True, stop=True),
                            rd=[bd, rhs_t], wr=[pp])
                        if eng == "act":
                            cx.op("act", lambda a, pp=pp, dst_ap=dst_ap: a.activation(
                                out=dst_ap, in_=pp[:, 0:n], func=AF.Copy), rd=[pp], wr=[dst_t])
                        else:
                            cx.op("dve", lambda v, pp=pp, dst_ap=dst_ap: v.tensor_copy(out=dst_ap, in_=pp[:, 0:n]),
                                  rd=[pp], wr=[dst_t])
                    for wi, lt, lap, dst_t in ((1, xcb[ob], lambda tb: xcb[ob][:, jj, tb * 128:(tb + 1) * 128], ktmo[ob]),
                                               (2, xb, lambda tb: xb[:, tb * 128:(tb + 1) * 128], vtmo[ob])):
                        pp = nextp()
                        for tb in range(ntb):
                            cx.op("pe", lambda p, wi=wi, lap=lap, tb=tb, pp=pp: p.matmul(
                                pp[:, tb * 128:(tb + 1) * 128], lhsT=lap(tb), rhs=bd[:, wi, cc, :],
                                start=True, stop=True), rd=[bd, lt], wr=[pp])
                        cx.op("dve", lambda v, pp=pp, dst_t=dst_t: v.tensor_copy(
                            out=dst_t[:, 0:ntb, jj * 128:(jj + 1) * 128],
                            in_=pp[:, 0:ntb * 128].rearrange("p (a b) -> p a b", b=128)), rd=[pp], wr=[dst_t])

                def stageC(cc):
                    gq, jj = cc // 4, cc % 4
                    ob = gq % 2
                    vt = vTs[cc % 3]
                    for wi, (rt, rap) in enumerate(((qTo[ob], qTo[ob][:, jj, 0:n]), (kTo[ob], kTo[ob][:, jj, 0:n]),
                                                    (vt, vt[:, 0:n]))):
                        cx.op("pe", lambda p, wi=wi, rap=rap: p.matmul(
                            g_ps[0:16, 0:n], lhsT=wg[:, wi * 16 + cc, :], rhs=rap,
                            start=(cc == 0 and wi == 0), stop=(cc == 15 and wi == 2)), rd=[wg, rt], wr=[g_ps])
                    if jj == 3:
                        fm = lambda t: t.ap().rearrange("(cc p) t -> p cc t", p=128)[:, gq * 4:(gq + 1) * 4, s:s + n]
                        for dt_, ot in ((self.qT, qTo[ob]), (self.kT, kTo[ob])):
                            cx.dma("sp", fm(dt_), ot[:, :, 0:n], rd=[ot], owner=ot)
                        tmv = lambda t: t.ap()[s:s + n, gq * 512:(gq + 1) * 512].rearrange("(tb p) ch -> p tb ch", p=128)
                        for dt_, ot in ((self.ktm, ktmo[ob]), (self.vtm, vtmo[ob])):
                            cx.dma("sp", tmv(dt_), ot[:, 0:ntb, :], rd=[ot], owner=ot)

                for step in range(16 + 2):
                    if step < 16:
                        stageA(step)
                    if 0 <= step - 1 < 16:
                        stageB(step - 1)
                    if 0 <= step - 2 < 16:
                        stageC(step - 2)
                cx.op("act", lambda a: a.activation(out=self.gT16[:, s:s + n], in_=g_ps[0:16, 0:n], func=AF.Identity,
                                                    bias=bg[:, 0:1], scale=1.0), rd=[g_ps, bg], wr=[self.gT16])

    def mlstm_gates(self):
        cx = self.cx
        T, NCH = self.T, self.NCH
        cx.barrier()
        with ExitStack() as es:
            mk = lambda nm: cx.sb(es, nm, [4, T], F32)
            IG, FG, CS, A, P, TM = mk("IG"), mk("FG"), mk("CS"), mk("A"), mk("P"), mk("TM")
            mk2 = lambda nm: cx.sb(es, nm, [4, NCH], F32)
            Mb, Mend, dec = mk2("Mb"), mk2("Mend"), mk2("dec")
            ones4 = cx.sb(es, "ones4", [4, 128], F32)
            R = cx.sb(es, "R", [4, NCH, 4], F32)
            cx.op("pool", lambda g: g.memset(ones4[:], 1.0), wr=[ones4])
            c3 = lambda t: t[:, :].rearrange("p (c t) -> p c t", t=128)
            for d in range(2):
                order = self.chunk_order(d)
                rv = (lambda ap: ap[:, ::-1]) if d == 1 else (lambda ap: ap)
                for s0 in range(0, T, 512):
                    n = min(512, T - s0)
                    for dst, c0 in ((IG, d * 4), (FG, 8 + d * 4)):
                        pp = self.ps[1 + (s0 // 512) % 2] if dst is IG else self.ps[3 + (s0 // 512) % 2]
                        cx.op("pe", lambda p, pp=pp, c0=c0, s0=s0, n=n: p.matmul(
                            pp[0:4, 0:n], lhsT=self.ident[0:16, c0:c0 + 4], rhs=self.gT16[:, s0:s0 + n],
                            start=True, stop=True), rd=[self.ident, self.gT16], wr=[pp])
                        cx.op("act", lambda a, pp=pp, dst=dst, s0=s0, n=n: a.activation(
                            out=dst[:, s0:s0 + n], in_=pp[0:4, 0:n], func=AF.Copy), rd=[pp], wr=[dst])
                cx.op("act", lambda a: a.activation(out=FG[:, :], in_=FG[:, :], func=AF.Exp, scale=-1.0),
                      rd=[FG], wr=[FG])
                cx.op("act", lambda a: a.activation(out=FG[:, :], in_=FG[:, :], func=AF.Ln, bias=self.one_col[0:4, 0:1],
                                                    scale=1.0), rd=[FG, self.one_col], wr=[FG])
                for c in range(NCH):
                    sl = slice(c * 128, (c + 1) * 128)
                    cx.op("dve", lambda v, sl=sl: v.tensor_tensor_scan(
                        out=rv(CS[:, sl]), data0=ones4[:, :], data1=rv(FG[:, sl]), initial=0.0,
                        op0=ALU.mult, op1=ALU.add), rd=[FG, ones4], wr=[CS])
                cx.op("dve", lambda v: v.tensor_tensor(out=A[:, :], in0=IG[:, :], in1=CS[:, :], op=ALU.add),
                      rd=[IG, CS], wr=[A])
                for c in range(NCH):
                    sl = slice(c * 128, (c + 1) * 128)
                    cx.op("dve", lambda v, sl=sl: v.tensor_tensor_scan(
                        out=rv(P[:, sl]), data0=rv(A[:, sl]), data1=rv(A[:, sl]), initial=-1e30,
                        op0=ALU.max, op1=ALU.max), rd=[A], wr=[P])
                last = 127 if d == 0 else 0
                csl = c3(CS)[:, :, last]
                pend = c3(P)[:, :, last]
                cx.op("pool", lambda g: g.memset(Mb[:, :], 0.0), wr=[Mb])
                for i, c in enumerate(order):
                    cx.op("dve", lambda v, c=c: v.tensor_tensor(out=Mend[:, c:c + 1], in0=Mb[:, c:c + 1],
                                                                in1=pend[:, c:c + 1], op=ALU.max),
                          rd=[Mb, P], wr=[Mend])
                    if i + 1 < len(order):
                        cn = order[i + 1]
                        cx.op("dve", lambda v, c=c, cn=cn: v.tensor_tensor(
                            out=Mb[:, cn:cn + 1], in0=Mend[:, c:c + 1], in1=csl[:, c:c + 1], op=ALU.subtract),
                            rd=[Mend, CS], wr=[Mb])
                cx.op("dve", lambda v: v.tensor_tensor(out=dec[:, :], in0=Mb[:, :], in1=Mend[:, :], op=ALU.subtract),
                      rd=[Mb, Mend], wr=[dec])
                cx.op("act", lambda a: a.activation(out=dec[:, :], in_=dec[:, :], func=AF.Exp), rd=[dec], wr=[dec])
                mbc = Mend[:, :].unsqueeze(2).to_broadcast([4, NCH, 128])
                cx.op("dve", lambda v: v.tensor_tensor(out=c3(TM), in0=c3(A), in1=mbc, op=ALU.subtract),
                      rd=[A, Mend], wr=[TM])
                cx.op("act", lambda a: a.activation(out=TM[:, :], in_=TM[:, :], func=AF.Exp), rd=[TM], wr=[TM])
                cx.op("dve", lambda v: v.scalar_tensor_tensor(
                    out=c3(A), in0=c3(CS), scalar=float(0.5 * np.log(512.0)), in1=mbc, op0=ALU.add, op1=ALU.subtract),
                    rd=[CS, Mend], wr=[A])
                cx.op("act", lambda a: a.activation(out=A[:, :], in_=A[:, :], func=AF.Exp), rd=[A], wr=[A])
                for src, pp, dsts in ((TM, self.ps[5], (self.Wtm[d], self.Wtmb[d])), (A, self.ps[6], (self.Ftm[d],))):
                    for c in range(NCH):
                        cx.op("pe", lambda p, c=c, src=src, pp=pp: p.transpose(
                            pp[:, c * 4:(c + 1) * 4], src[:, c * 128:(c + 1) * 128], self.ident[0:4, 0:4]),
                            rd=[src, self.ident], wr=[pp])
                    for dst in dsts:
                        cx.op("dve", lambda v, pp=pp, dst=dst: v.tensor_copy(
                            out=dst[:, :, :], in_=pp[:, 0:NCH * 4].rearrange("p (c h) -> p c h", h=4)),
                            rd=[pp], wr=[dst])
                cx.op("dve", lambda v: v.tensor_tensor(
                    out=R[:, :, :], in0=dec[:, :].unsqueeze(2).to_broadcast([4, NCH, 4]),
                    in1=self.ident[0:4, 0:4].unsqueeze(1).to_broadcast([4, NCH, 4]), op=ALU.mult),
                    rd=[dec, self.ident], wr=[R])
                pp = self.ps[7]
                cx.op("pe", lambda p, pp=pp: p.matmul(pp[:, 0:NCH * 4], lhsT=self.ones_f[0:4, :],
                                                      rhs=R[:, :, :].rearrange("p c h -> p (c h)"),
                                                      start=True, stop=True), rd=[self.ones_f, R], wr=[pp])
                cx.op("dve", lambda v, pp=pp: v.tensor_copy(
                    out=self.decbc[d][:, :, :], in_=pp[:, 0:NCH * 4].rearrange("p (c h) -> p c h", h=4)),
                    rd=[pp], wr=[self.decbc[d]])

    def mlstm_scan(self, d):
        cx = self.cx
        T, NCH = self.T, self.NCH
        order = self.chunk_order(d)
        mask = self.maskU if d == 0 else self.maskL
        Wtm, Wtmb, Ftm, decbc = self.Wtm[d], self.Wtmb[d], self.Ftm[d], self.decbc[d]
        cx.barrier()
        with ExitStack() as es:
            C = [cx.sb(es, f"C{h}", [128, 4, 512], F32) for h in range(4)]
            Cd = [cx.sb(es, f"Cd{h}", [128, 4, 512], BF16) for h in range(4)]
            nv = cx.sb(es, "nv", [128, 4, 4], F32)
            nd = cx.sb(es, "nd", [128, 4, 4], BF16)
            for h in range(4):
                cx.op("pool", lambda g, h=h: g.memset(C[h][:, :, :], 0.0), wr=[C[h]])
                cx.op("pool", lambda g, h=h: g.memset(Cd[h][:, :, :], 0.0), wr=[Cd[h]])
            cx.op("pool", lambda g: g.memset(nv[:, :, :], 0.0), wr=[nv])
            cx.op("pool", lambda g: g.memset(nd[:, :, :], 0.0), wr=[nd])
            mk = lambda nm, shp, dt, k=2: [cx.sb(es, f"{nm}{i}", shp, dt) for i in range(k)]
            qTc, kTc = mk("qTc", [128, 16, 128], BF16), mk("kTc", [128, 16, 128], BF16)
            ktc, vtc = mk("ktc", [128, 2048], BF16), mk("vtc", [128, 2048], BF16)
            SmT = mk("SmT", [128, 128], BF16)
            vw = mk("vw", [128, 512], BF16)
            den = mk("den", [128, 1], F32, 4)
            hb = mk("hb", [128, 2048], F32)
            if d == 1:
                hf = mk("hf", [128, 2048], F32)
                hn = cx.sb(es, "hn", [128, 2048], BF16)
                st = cx.sb(es, "st", [128, 4, 6], F32)
                mv = cx.sb(es, "mv", [128, 4, 2], F32)
                rs = cx.sb(es, "rs", [128, 4], F32)
                xcsc, szc = mk("xcsc", [128, 16, 128], BF16), mk("szc", [128, 16, 128], BF16)
                t1 = mk("t1", [128, 8, 128], F32)
                yTo = mk("yTo", [128, 16, 128], BF16)
            fmv = lambda t, c: t.ap().rearrange("(cc p) t -> p cc t", p=128)[:, :, c * 128:(c + 1) * 128]

            def loads(ci):
                c = order[ci]
                b = ci % 2
                tb = self.yT_b[0]
                cx.dma("sp", qTc[b][:, :, :], fmv(self.qT, c), wr=[qTc[b]], owner=qTc[b])
                cx.dma("sp", kTc[b][:, :, :], fmv(self.kT, c), wr=[kTc[b]], owner=kTc[b])
                cx.dma("sp", ktc[b][:, :], self.ktm.ap()[c * 128:(c + 1) * 128, :], wr=[ktc[b]], owner=ktc[b])
                cx.dma("sp", vtc[b][:, :], self.vtm.ap()[c * 128:(c + 1) * 128, :], wr=[vtc[b]], owner=vtc[b])
                if d == 1:
                    cx.dma("sp", hf[b][:, :], self.hfwd.ap()[c * 128:(c + 1) * 128, :], wr=[hf[b]], owner=hf[b])
                    cx.dma("sp", xcsc[b][:, :, :], fmv(self.xcsT, c), wr=[xcsc[b]], owner=xcsc[b])
                    cx.dma("sp", szc[b][:, :, :], fmv(self.szT, c), wr=[szc[b]], owner=szc[b])
            loads(0)
            items = [(ci, c, h) for ci, c in enumerate(order) for h in range(4)]

            def stage1(it):
                ci, c, h = items[it]
                b = ci % 2
                S_ps = self.ps[it % 2]
                sm, vwt = SmT[it % 2], vw[it % 2]
                for j in range(4):
                    cx.op("pe", lambda p, j=j: p.matmul(
                        S_ps[:, 0:128], lhsT=kTc[b][:, h * 4 + j, :], rhs=qTc[b][:, h * 4 + j, :],
                        start=(j == 0), stop=(j == 3)), rd=[kTc[b], qTc[b]], wr=[S_ps])
                cx.op("act", lambda a: a.activation(
                    out=vwt[:, :], in_=vtc[b][:, h * 512:(h + 1) * 512], func=AF.Copy, scale=Wtm[:, c, h:h + 1]),
                    rd=[vtc[b], Wtm], wr=[vwt])
                cx.op("dve", lambda v: v.tensor_tensor(out=sm[:, :], in0=S_ps[:, 0:128], in1=mask[:, :],
                                                       op=ALU.mult), rd=[S_ps, mask], wr=[sm])

            def stage2(it):
                ci, c, h = items[it]
                b = ci % 2
                cn = order[ci + 1] if ci + 1 < len(order) else None
                N_ps = self.ps[2 + it % 2]
                D_ps = self.ps[4]
                dcol = (it % 8) * 8
                sm, vwt, dn = SmT[it % 2], vw[it % 2], den[it % 4]
                cx.op("pe", lambda p: p.matmul(N_ps[:, :], lhsT=sm[:, :], rhs=vwt[:, :], start=True, stop=False),
                      rd=[sm, vwt], wr=[N_ps])
                for j in range(4):
                    cx.op("pe", lambda p, j=j: p.matmul(
                        N_ps[:, :], lhsT=qTc[b][:, h * 4 + j, :], rhs=Cd[h][:, j, :], start=False, stop=(j == 3)),
                        rd=[qTc[b], Cd[h]], wr=[N_ps])
                cx.op("pe", lambda p: p.matmul(D_ps[:, dcol:dcol + 1], lhsT=sm[:, :], rhs=Wtmb[:, c, h:h + 1],
                                               start=True, stop=False), rd=[sm, Wtmb], wr=[D_ps])
                for j in range(4):
                    cx.op("pe", lambda p, j=j: p.matmul(
                        D_ps[:, dcol:dcol + 1], lhsT=qTc[b][:, h * 4 + j, :], rhs=nd[:, h, j:j + 1],
                        start=False, stop=(j == 3)), rd=[qTc[b], nd], wr=[D_ps])
                ucol = dcol + 4
                for j in range(4):
                    cx.op("pe", lambda p, j=j: p.matmul(
                        D_ps[:, ucol + j:ucol + j + 1],
                        lhsT=ktc[b][:, h * 512 + j * 128:h * 512 + (j + 1) * 128], rhs=Wtmb[:, c, h:h + 1],
                        start=True, stop=True), rd=[ktc[b], Wtmb], wr=[D_ps])
                cx.op("act", lambda a: a.activation(out=dn[:, :], in_=D_ps[:, dcol:dcol + 1], func=AF.Abs),
                      rd=[D_ps], wr=[dn])
                cx.op("dve", lambda v: v.tensor_scalar(
                    out=dn[:, :], in0=dn[:, :], scalar1=Ftm[:, c, h:h + 1], scalar2=None,
                    op0=ALU.max), rd=[dn, Ftm], wr=[dn])
                cx.op("dve", lambda v: v.reciprocal(out=dn[:, :], in_=dn[:, :]), rd=[dn], wr=[dn])
                cx.op("act", lambda a: a.activation(out=hb[b][:, h * 512:(h + 1) * 512], in_=N_ps[:, :],
                                                    func=AF.Copy, scale=dn[:, 0:1]), rd=[N_ps, dn], wr=[hb[b]])
                cx.op("dve", lambda v: v.scalar_tensor_tensor(
                    out=nv[:, h, :], in0=nv[:, h, :], scalar=decbc[:, c, h:h + 1], in1=D_ps[:, ucol:ucol + 4],
                    op0=ALU.mult, op1=ALU.add), rd=[nv, decbc, D_ps], wr=[nv])
                if cn is not None:
                    cx.op("dve", lambda v: v.tensor_scalar(
                        out=nd[:, h, :], in0=nv[:, h, :], scalar1=decbc[:, cn, h:h + 1], scalar2=None,
                        op0=ALU.mult), rd=[nv, decbc], wr=[nd])
                for j in range(4):
                    U_ps = self.ps[5 + j % 2]
                    cx.op("pe", lambda p, j=j, U_ps=U_ps: p.matmul(
                        U_ps[:, :], lhsT=ktc[b][:, h * 512 + j * 128:h * 512 + (j + 1) * 128], rhs=vwt[:, :],
                        start=True, stop=True), rd=[ktc[b], vwt], wr=[U_ps])
                    cx.op("dve", lambda v, j=j, U_ps=U_ps: v.scalar_tensor_tensor(
                        out=C[h][:, j, :], in0=C[h][:, j, :], scalar=decbc[:, c, h:h + 1], in1=U_ps[:, :],
                        op0=ALU.mult, op1=ALU.add), rd=[C[h], decbc, U_ps], wr=[C[h]])
                    if cn is not None:
                        cx.op("act", lambda a, j=j: a.activation(
                            out=Cd[h][:, j, :], in_=C[h][:, j, :], func=AF.Copy, scale=decbc[:, cn, h:h + 1]),
                            rd=[C[h], decbc], wr=[Cd[h]])

            stage1(0)
            for it, (ci, c, h) in enumerate(items):
                b = ci % 2
                if h == 0 and ci + 1 < len(order):
                    loads(ci + 1)
                if it + 1 < len(items):
                    stage1(it + 1)
                stage2(it)
                if h != 3:
                    continue
                hrow = self.hfwd.ap()[c * 128:(c + 1) * 128, :]
                if d == 0:
                    cx.dma("sp", hrow, hb[b][:, :], rd=[hb[b]], owner=hb[b])
                    continue
                cx.op("pool", lambda g: g.tensor_tensor(out=hb[b][:, :], in0=hb[b][:, :], in1=hf[b][:, :], op=ALU.add),
                      rd=[hb[b], hf[b]], wr=[hb[b]])
                for h in range(4):
                    cx.op("dve", lambda v, h=h: v.bn_stats(out=st[:, h, :], in_=hb[b][:, h * 512:(h + 1) * 512]),
                          rd=[hb[b]], wr=[st])
                    cx.op("dve", lambda v, h=h: v.bn_aggr(out=mv[:, h, :], in_=st[:, h, :]), rd=[st], wr=[mv])
                cx.op("act", lambda a: a.activation(out=rs[:, :], in_=mv[:, :, 1], func=AF.Sqrt,
                                                    bias=self.eps_col[:, 0:1], scale=1.0), rd=[mv, self.eps_col], wr=[rs])
                cx.op("dve", lambda v: v.reciprocal(out=rs[:, :], in_=rs[:, :]), rd=[rs], wr=[rs])
                for h in range(4):
                    cx.op("dve", lambda v, h=h: v.tensor_scalar(
                        out=hn[:, h * 512:(h + 1) * 512], in0=hb[b][:, h * 512:(h + 1) * 512],
                        scalar1=mv[:, h, 0:1], scalar2=rs[:, h:h + 1], op0=ALU.subtract, op1=ALU.mult),
                        rd=[hb[b], mv, rs], wr=[hn])
                tp = self.ps[7]
                tpv = tp[:, :].bitcast(BF16)
                for r in range(2):
                    for q in range(8):
                        cc = r * 8 + q
                        cx.op("pe", lambda p, cc=cc, q=q: p.transpose(
                            tpv[:, q * 128:(q + 1) * 128], hn[:, cc * 128:(cc + 1) * 128], self.identb[:, :]),
                            rd=[hn, self.identb], wr=[tp])
                    tt = t1[r]
                    cx.op("dve", lambda v, r=r, tt=tt: v.tensor_tensor(
                        out=tt[:, :, :], in0=tpv.rearrange("p (a b) -> p a b", b=128),
                        in1=self.ml_ns_sb[:, 0, r * 8:(r + 1) * 8].unsqueeze(2).to_broadcast([128, 8, 128]),
                        op=ALU.mult), rd=[tp, self.ml_ns_sb], wr=[tt])
                    cx.op("pool", lambda g, r=r, tt=tt: g.tensor_tensor(
                        out=tt[:, :, :], in0=tt[:, :, :], in1=xcsc[b][:, r * 8:(r + 1) * 8, :], op=ALU.add),
                        rd=[tt, xcsc[b]], wr=[tt])
                    cx.op("pool", lambda g, r=r, tt=tt: g.tensor_tensor(
                        out=yTo[b][:, r * 8:(r + 1) * 8, :], in0=tt[:, :, :], in1=szc[b][:, r * 8:(r + 1) * 8, :],
                        op=ALU.mult), rd=[tt, szc[b]], wr=[yTo[b]])
                cx.dma("sp", fmv(self.yT, c), yTo[b][:, :, :], rd=[yTo[b]], owner=yTo[b])


    def ssd(self, li, need_ctx):
        cx = self.cx
        NCH = self.NCH
        cx.barrier()
        with ExitStack() as es:
            self.dtr_sb = cx.sb(es, "dtr_sb", [128, NCH, 64], F32)
            self.sd_small = cx.sb(es, "sd_small", [128, 160], F32)
            cx.dma("sp", self.sd_small[:, :], self.sd_smallT.ap(), wr=[self.sd_small], owner=self.sd_small)
            cx.op("act", lambda a: a.activation(out=self.sd_small[:, 64:128], in_=self.sd_small[:, 64:128],
                                                func=AF.Exp), rd=[self.sd_small], wr=[self.sd_small])
            cx.op("dve", lambda v: v.tensor_scalar(out=self.sd_small[:, 64:128], in0=self.sd_small[:, 64:128],
                                                   scalar1=-1.0, scalar2=None, op0=ALU.mult),
                  rd=[self.sd_small], wr=[self.sd_small])
            self.ssd_in_z(li)
            self.ssd_in_x(li)
            if self.debug_stop == (li, "ml_in"):
                return
            for d in range(2):
                self.ssd_scan(d, need_ctx)
                if self.debug_stop and self.debug_stop[1] in ("ssd_tab", "ssd_d0"):
                    return
        tiles = self.tiles if need_ctx else self.tiles[:-1]
        self.proj_res(self.sd_w_out.ap(), 16, self.yT, self.yT_b, 5, (self.xres, self.xres_b),
                      (self.xres, self.xres_b), tiles)

    def _load_x(self, x, i):
        s, n, c = self.tiles[i]
        self.cx.dma("sp", x[:, :, 0:n], self.xres.ap().rearrange("(kc p) t -> p kc t", p=128)[:, :, s:s + n],
                    rd=[self.xres_b[i]], wr=[x], owner=x)

    def ssd_in_z(self, li):
        cx = self.cx
        tiles = self.tiles
        cx.barrier()
        with ExitStack() as es:
            NZ = 2048
            Wz = cx.sb(es, "Wz", [128, KC, NZ + 64], BF16)
            Wzb = [Buf(f"Wz{g}") for g in range(9)]
            stg = [cx.sb(es, f"wstg{i}", [128, KC, 256], F32) for i in range(2)]
            x = cx.sb(es, "x", [128, KC, 512], F32)
            hT = cx.sb(es, "hT", [128, KC, 512], BF16)
            sq = cx.sb(es, "sq", [128, KC, 512], BF16)
            rstd = cx.sb(es, "rstd", [128, 512], F32)
            szo = [cx.sb(es, f"szo{i}", [128, 4, NZ], BF16) for i in range(2)]
            self._load_x(x, 0)
            wv = self.sd_w_in.ap().rearrange("(kc p) m -> p kc m", p=128)
            for g in range(9):
                st = stg[g % 2]
                if g < 8:
                    cx.dma("sp", st[:], wv[:, :, g * 256:(g + 1) * 256], wr=[st], owner=st)
                    cx.op("pool", lambda gp, st=st, g=g: gp.tensor_copy(
                        out=Wz[:, :, g * 256:(g + 1) * 256], in_=st[:]), rd=[st], wr=[Wzb[g]])
                else:
                    cx.dma("sp", st[:, :, 0:64], wv[:, :, 6144:6208], wr=[st], owner=st)
                    cx.op("pool", lambda gp, st=st: gp.tensor_copy(
                        out=Wz[:, :, NZ:NZ + 64], in_=st[:, :, 0:64]), rd=[st], wr=[Wzb[8]])
            cnt = 0
            for i, (s, n, c) in enumerate(tiles):
                ntb = n // 128
                self.norm_mod(None, x, sq, hT, n, c, 3, self.ps[0], rstd)
                if i + 1 < len(tiles):
                    self._load_x(x, i + 1)
                so = szo[i % 2]
                for tb in range(ntb):
                    for zb in range(4):
                        cnt += 1
                        zp = self.ps[1 + cnt % 4]
                        for k in range(KC):
                            cx.op("pe", lambda p, k=k, zp=zp: p.matmul(
                                zp[:, :], lhsT=hT[:, k, tb * 128:(tb + 1) * 128], rhs=Wz[:, k, zb * 512:(zb + 1) * 512],
                                start=(k == 0), stop=(k == KC - 1)), rd=[hT, Wzb[2 * zb], Wzb[2 * zb + 1]], wr=[zp])
                        cx.op("act", lambda a, zp=zp: a.activation(out=so[:, tb, zb * 512:(zb + 1) * 512], in_=zp[:, :],
                                                                   func=AF.Silu), rd=[zp], wr=[so])
                    dp = self.ps[5 + tb % 2]
                    for k in range(KC):
                        cx.op("pe", lambda p, k=k, dp=dp: p.matmul(
                            dp[:, 0:64], lhsT=hT[:, k, tb * 128:(tb + 1) * 128], rhs=Wz[:, k, NZ:NZ + 64],
                            start=(k == 0), stop=(k == KC - 1)), rd=[hT, Wzb[8]], wr=[dp])
                    ch = s // 128 + tb
                    cx.op("dve", lambda v, dp=dp, ch=ch: v.tensor_copy(out=self.dtr_sb[:, ch, :], in_=dp[:, 0:64]),
                          rd=[dp], wr=[self.dtr_sb])
                cx.dma("sp", self.sztm.ap()[s:s + n, :].rearrange("(tb p) ch -> p tb ch", p=128), so[:, 0:ntb, :],
                       rd=[so], owner=so)

    def ssd_in_x(self, li):
        cx = self.cx
        tiles = self.tiles
        cx.barrier()
        with ExitStack() as es:
            Wx = cx.sb(es, "Wx", [128, KC, 4096], BF16)
            Wxb = [Buf(f"Wx{g}") for g in range(16)]
            conv = cx.sb(es, "conv", [128, 32, 6], F32)
            stg = [cx.sb(es, f"wstg{i}", [128, KC, 256], F32) for i in range(2)]
            x = cx.sb(es, "x", [128, KC, 512], F32)
            hT = cx.sb(es, "hT", [128, KC, 512], BF16)
            sq = cx.sb(es, "sq", [128, KC, 512], BF16)
            rstd = cx.sb(es, "rstd", [128, 512], F32)
            xf = [cx.sb(es, f"xf{i}", [128, 512], F32) for i in range(2)]
            acc = [cx.sb(es, f"acc{i}", [128, 512], F32) for i in range(2)]
            xc = [cx.sb(es, f"xc{i}", [128, 512], BF16) for i in range(2)]
            xtmo = cx.sb(es, "xtmo", [128, 4, 2048], BF16)
            Btmo = cx.sb(es, "Btmo", [128, 4, 1024], BF16)
            BTo = cx.sb(es, "BTo", [128, 8, 512], BF16)
            CTo = cx.sb(es, "CTo", [128, 8, 512], BF16)
            cx.dma("sp", conv[:], self.sd_conv.ap(), wr=[conv], owner=conv)
            self._load_x(x, 0)
            wv = self.sd_w_in.ap().rearrange("(kc p) m -> p kc m", p=128)
            for g in range(16):
                st = stg[g % 2]
                cx.dma("sp", st[:], wv[:, :, 2048 + g * 256:2048 + (g + 1) * 256], wr=[st], owner=st)
                cx.op("pool", lambda gp, st=st, g=g: gp.tensor_copy(
                    out=Wx[:, :, g * 256:(g + 1) * 256], in_=st[:]), rd=[st], wr=[Wxb[g]])
            for i, (s, n, c) in enumerate(tiles):
                ntb = n // 128
                self.norm_mod(None, x, sq, hT, n, c, 3, self.ps[0], rstd)
                if i + 1 < len(tiles):
                    self._load_x(x, i + 1)
                R = n if c else 64
                v3 = lambda ap: ap.rearrange("p (r w) -> p r w", w=R)

                def dst_of(cc):
                    if cc < 16:
                        return xc[cc % 2], xc[cc % 2][:, 0:n]
                    if cc < 24:
                        return BTo, BTo[:, cc - 16, 0:n]
                    return CTo, CTo[:, cc - 24, 0:n]

                def stageA(cc):
                    xm_ps = self.ps[1 + cc % 2]
                    for k in range(KC):
                        cx.op("pe", lambda p, k=k: p.matmul(
                            xm_ps[:, 0:n], lhsT=Wx[:, k, cc * 128:(cc + 1) * 128], rhs=hT[:, k, 0:n],
                            start=(k == 0), stop=(k == KC - 1)), rd=[hT, Wxb[cc // 2]], wr=[xm_ps])
                    f, ac = xf[cc % 2], acc[cc % 2]
                    cx.op("act", lambda a: a.activation(out=f[:, 0:n], in_=xm_ps[:, 0:n], func=AF.Copy),
                          rd=[xm_ps], wr=[f])
                    cx.op("dve", lambda v: v.tensor_scalar(
                        out=ac[:, 0:n], in0=f[:, 0:n], scalar1=conv[:, cc, 2:3], scalar2=conv[:, cc, 5:6],
                        op0=ALU.mult, op1=ALU.add), rd=[f, conv], wr=[ac])
                    for j in (0, 1, 3, 4):
                        sh = j - 2
                        o0, o1 = max(0, -sh), R - max(0, sh)
                        i0, i1 = max(0, sh), R - max(0, -sh)
                        cx.op("dve", lambda v, j=j, o0=o0, o1=o1, i0=i0, i1=i1: v.scalar_tensor_tensor(
                            out=v3(ac[:, 0:n])[:, :, o0:o1], in0=v3(f[:, 0:n])[:, :, i0:i1],
                            scalar=conv[:, cc, j:j + 1], in1=v3(ac[:, 0:n])[:, :, o0:o1],
                            op0=ALU.mult, op1=ALU.add), rd=[f, ac, conv], wr=[ac])
                    dt_, dap = dst_of(cc)
                    cx.op("act", lambda a: a.activation(out=dap, in_=ac[:, 0:n], func=AF.Silu), rd=[ac], wr=[dt_])

                def stageB(cc):
                    if cc >= 24:
                        return
                    st_, sap = dst_of(cc)
                    tp = self.ps[3 + cc % 2]
                    tpv = tp[:, :].bitcast(BF16)
                    for tb in range(ntb):
                        cx.op("pe", lambda p, tb=tb: p.transpose(
                            tpv[:, tb * 128:(tb + 1) * 128], sap[:, tb * 128:(tb + 1) * 128], self.identb[:, :]),
                            rd=[st_, self.identb], wr=[tp])
                    if cc < 16:
                        ot, oap = xtmo, xtmo[:, 0:ntb, cc * 128:(cc + 1) * 128]
                    else:
                        ot, oap = Btmo, Btmo[:, 0:ntb, (cc - 16) * 128:(cc - 15) * 128]
                    cx.op("dve", lambda v: v.tensor_copy(
                        out=oap, in_=tpv[:, 0:ntb * 128].rearrange("p (a b) -> p a b", b=128)), rd=[tp], wr=[ot])

                for step in range(33):
                    if step < 32:
                        stageA(step)
                    if step >= 1:
                        stageB(step - 1)
                tmv = lambda t: t.ap()[s:s + n, :].rearrange("(tb p) ch -> p tb ch", p=128)
                cx.dma("sp", tmv(self.xtm), xtmo[:, 0:ntb, :], rd=[xtmo], owner=xtmo)
                cx.dma("sp", tmv(self.Btm), Btmo[:, 0:ntb, :], rd=[Btmo], owner=Btmo)
                fm = lambda t: t.ap().rearrange("(g p) t -> p g t", p=128)[:, :, s:s + n]
                cx.dma("sp", fm(self.BT), BTo[:, :, 0:n], rd=[BTo], owner=BTo)
                cx.dma("sp", fm(self.CT), CTo[:, :, 0:n], rd=[CTo], owner=CTo)

    def ssd_scan(self, d, need_ctx):
        cx = self.cx
        T, NCH = self.T, self.NCH
        order = self.chunk_order(d)
        tri = self.maskU if d == 0 else self.maskL
        neg = self.negU if d == 0 else self.negL
        nlat = self.TL // 128
        cx.barrier()
        with ExitStack() as es:
            mkt = lambda nm: cx.sb(es, nm, [128, NCH, 32], F32)
            dt_t, la_t, ncs_t, ecs_t, w2_t, dec_t = mkt("dt_t"), mkt("la_t"), mkt("ncs_t"), mkt("ecs_t"), mkt("w2_t"), mkt("dec_t")
            fl = lambda t: t[:, :, :].rearrange("p c h -> p (c h)")
            sm = self.sd_small
            cx.op("dve", lambda v: v.tensor_tensor(
                out=dt_t[:, :, :], in0=self.dtr_sb[:, :, d * 32:(d + 1) * 32],
                in1=sm[:, d * 32:(d + 1) * 32].unsqueeze(1).to_broadcast([128, NCH, 32]), op=ALU.add),
                rd=[self.dtr_sb, sm], wr=[dt_t])
            cx.op("act", lambda a: a.activation(out=fl(dt_t), in_=fl(dt_t), func=AF.Exp), rd=[dt_t], wr=[dt_t])
            cx.op("act", lambda a: a.activation(out=fl(dt_t), in_=fl(dt_t), func=AF.Ln, bias=self.one_col[:, 0:1],
                                                scale=1.0), rd=[dt_t, self.one_col], wr=[dt_t])
            cx.op("dve", lambda v: v.tensor_tensor(
                out=la_t[:, :, :], in0=dt_t[:, :, :],
                in1=sm[:, 64 + d * 32:64 + (d + 1) * 32].unsqueeze(1).to_broadcast([128, NCH, 32]), op=ALU.mult),
                rd=[dt_t, sm], wr=[la_t])
            NF = NCH * 32
            for c0 in range(0, NF, 512):
                w = min(512, NF - c0)
                csp, cep = self.ps[1], self.ps[2]
                cx.op("pe", lambda p, c0=c0, w=w: p.matmul(csp[:, 0:w], lhsT=tri[:, :], rhs=fl(la_t)[:, c0:c0 + w],
                                                           start=True, stop=True), rd=[tri, la_t], wr=[csp])
                cx.op("pe", lambda p, c0=c0, w=w: p.matmul(cep[:, 0:w], lhsT=self.ones_f[:, :], rhs=fl(la_t)[:, c0:c0 + w],
                                                           start=True, stop=True), rd=[self.ones_f, la_t], wr=[cep])
                cx.op("dve", lambda v, c0=c0, w=w: v.tensor_scalar(
                    out=fl(ncs_t)[:, c0:c0 + w], in0=csp[:, 0:w], scalar1=-1.0, scalar2=None, op0=ALU.mult),
                    rd=[csp], wr=[ncs_t])
                cx.op("act", lambda a, c0=c0, w=w: a.activation(out=fl(ecs_t)[:, c0:c0 + w], in_=csp[:, 0:w],
                                                                func=AF.Exp), rd=[csp], wr=[ecs_t])
                cx.op("act", lambda a, c0=c0, w=w: a.activation(out=fl(dec_t)[:, c0:c0 + w], in_=cep[:, 0:w],
                                                                func=AF.Exp), rd=[cep], wr=[dec_t])
                cx.op("dve", lambda v, c0=c0, w=w: v.tensor_tensor(
                    out=fl(w2_t)[:, c0:c0 + w], in0=cep[:, 0:w], in1=fl(ncs_t)[:, c0:c0 + w], op=ALU.add),
                    rd=[cep, ncs_t], wr=[w2_t])
                cx.op("act", lambda a, c0=c0, w=w: a.activation(out=fl(w2_t)[:, c0:c0 + w], in_=fl(w2_t)[:, c0:c0 + w],
                                                                func=AF.Exp), rd=[w2_t], wr=[w2_t])
            if self.debug_stop and self.debug_stop[1] == "ssd_tab":
                return
            hs = cx.sb(es, "hs", [128, 8, 256], F32)
            hsb = cx.sb(es, "hsb", [128, 8, 256], BF16)
            cx.op("pool", lambda g: g.memset(hs[:, :, :], 0.0), wr=[hs])
            cx.op("pool", lambda g: g.memset(hsb[:, :, :], 0.0), wr=[hsb])
            mk = lambda nm, shp, dt, k=2: [cx.sb(es, f"{nm}{i}", shp, dt) for i in range(k)]
            xt, Bt = mk("xt", [128, 2048], BF16), mk("Bt", [128, 1024], BF16)
            BTc, CTc = mk("BTc", [128, 8, 128], BF16), mk("CTc", [128, 8, 128], BF16)
            xdt, xw = mk("xdt", [128, 2048], BF16), mk("xw", [128, 2048], BF16)
            Eb = mk("Eb", [128, 128], BF16, 4)
            mT = mk("mT", [128, 128], BF16, 4)
            tz = mk("tz", [128, 256], F32)
            yb = mk("yb", [128, 2048], F32)
            if d == 1:
                yf = mk("yf", [128, 2048], F32)
                szc = mk("szc", [128, 2048], BF16)
                ngb = cx.sb(es, "ngb", [128, 2048], F32)
                cx.dma("sp", ngb[:, :], self.sd_ng.ap(), wr=[ngb], owner=ngb)
                yg = cx.sb(es, "yg", [128, 2048], F32)
                junk = cx.sb(es, "junk", [128, 2048], BF16)
                yn = cx.sb(es, "yn", [128, 2048], BF16)
                ss = cx.sb(es, "ss", [128, 1], F32)
                yTo = mk("yTo", [128, 16, 128], BF16)
            fmv = lambda t, c: t.ap().rearrange("(g p) t -> p g t", p=128)[:, :, c * 128:(c + 1) * 128]
            rows = lambda t, c: t.ap()[c * 128:(c + 1) * 128, :]

            def loads(ci):
                c = order[ci]
                b = ci % 2
                cx.dma("sp", xt[b][:, :], rows(self.xtm, c), wr=[xt[b]], owner=xt[b])
                cx.dma("sp", Bt[b][:, :], rows(self.Btm, c), wr=[Bt[b]], owner=Bt[b])
                cx.dma("sp", BTc[b][:, :, :], fmv(self.BT, c), wr=[BTc[b]], owner=BTc[b])
                cx.dma("sp", CTc[b][:, :, :], fmv(self.CT, c), wr=[CTc[b]], owner=CTc[b])
                if d == 1 and (need_ctx or c < nlat):
                    cx.dma("sp", yf[b][:, :], rows(self.hfwd, c), wr=[yf[b]], owner=yf[b])
                    cx.dma("sp", szc[b][:, :], rows(self.sztm, c), wr=[szc[b]], owner=szc[b])
            loads(0)
            hc = 0
            for ci, c in enumerate(order):
                b = ci % 2
                if ci + 1 < len(order):
                    loads(ci + 1)
                bc64 = lambda ap: ap.unsqueeze(2).to_broadcast([128, 32, 64])
                v64 = lambda ap: ap.rearrange("p (h q) -> p h q", q=64)
                cx.op("pool", lambda g: g.tensor_tensor(out=v64(xdt[b][:, :]), in0=v64(xt[b][:, :]),
                                                        in1=bc64(dt_t[:, c, :]), op=ALU.mult),
                      rd=[xt[b], dt_t], wr=[xdt[b]])
                cx.op("pool", lambda g: g.tensor_tensor(out=v64(xw[b][:, :]), in0=v64(xdt[b][:, :]),
                                                        in1=bc64(w2_t[:, c, :]), op=ALU.mult),
                      rd=[xdt[b], w2_t], wr=[xw[b]])
                for g in range(8):
                    cb = self.ps[g % 2]
                    cx.op("pe", lambda p, g=g, cb=cb: p.matmul(cb[:, 0:128], lhsT=BTc[b][:, g, :], rhs=CTc[b][:, g, :],
                                                               start=True, stop=True), rd=[BTc[b], CTc[b]], wr=[cb])
                    Y = self.ps[4 + g % 2]
                    for r in range(4):
                        hc += 1
                        hd = g * 4 + r
                        ep = self.ps[2 + hc % 2]
                        E, m_ = Eb[hc % 4], mT[hc % 4]
                        cx.op("pe", lambda p, hd=hd, ep=ep: p.matmul(
                            ep[:, 0:128], lhsT=la_t[:, c, hd:hd + 1].to_broadcast([128, 128]), rhs=tri[:, :],
                            start=True, stop=False), rd=[la_t, tri], wr=[ep])
                        cx.op("pe", lambda p, ep=ep: p.matmul(ep[:, 0:128], lhsT=self.identb[:, :], rhs=neg[:, :],
                                                              start=False, stop=True), rd=[self.identb, neg], wr=[ep])
                        cx.op("act", lambda a, hd=hd, ep=ep, E=E: a.activation(
                            out=E[:, :], in_=ep[:, 0:128], func=AF.Exp, bias=ncs_t[:, c, hd:hd + 1], scale=1.0),
                            rd=[ep, ncs_t], wr=[E])
                        cx.op("dve", lambda v, cb=cb, E=E, m_=m_: v.tensor_tensor(
                            out=m_[:, :], in0=cb[:, 0:128], in1=E[:, :], op=ALU.mult), rd=[cb, E], wr=[m_])
                        cx.op("pe", lambda p, hd=hd, r=r, m_=m_, Y=Y: p.matmul(
                            Y[:, r * 64:(r + 1) * 64], lhsT=m_[:, :], rhs=xdt[b][:, hd * 64:(hd + 1) * 64],
                            start=True, stop=True), rd=[m_, xdt[b]], wr=[Y])
                    Z = self.ps[6]
                    zc = (g % 2) * 256
                    cx.op("pe", lambda p, g=g, zc=zc: p.matmul(Z[:, zc:zc + 256], lhsT=CTc[b][:, g, :], rhs=hsb[:, g, :],
                                                               start=True, stop=True), rd=[CTc[b], hsb], wr=[Z])
                    t_ = tz[g % 2]
                    v4 = lambda ap: ap.rearrange("p (r q) -> p r q", q=64)
                    cx.op("dve", lambda v, g=g, zc=zc, t_=t_: v.tensor_tensor(
                        out=v4(t_[:, :]), in0=v4(Z[:, zc:zc + 256]),
                        in1=ecs_t[:, c, g * 4:(g + 1) * 4].unsqueeze(2).to_broadcast([128, 4, 64]), op=ALU.mult),
                        rd=[Z, ecs_t], wr=[t_])
                    cx.op("dve", lambda v, g=g, Y=Y, t_=t_: v.tensor_tensor(
                        out=yb[b][:, g * 256:(g + 1) * 256], in0=Y[:, 0:256], in1=t_[:, :], op=ALU.add),
                        rd=[Y, t_], wr=[yb[b]])
                    U = self.ps[7]
                    cx.op("pe", lambda p, g=g, zc=zc: p.matmul(
                        U[:, zc:zc + 256], lhsT=Bt[b][:, g * 128:(g + 1) * 128], rhs=xw[b][:, g * 256:(g + 1) * 256],
                        start=True, stop=True), rd=[Bt[b], xw[b]], wr=[U])
                    cx.op("pool", lambda gp, g=g: gp.tensor_tensor(
                        out=v4(hs[:, g, :]), in0=v4(hs[:, g, :]),
                        in1=dec_t[:, c, g * 4:(g + 1) * 4].unsqueeze(2).to_broadcast([128, 4, 64]), op=ALU.mult),
                        rd=[hs, dec_t], wr=[hs])
                    cx.op("dve", lambda v, g=g, zc=zc: v.tensor_tensor(
                        out=hs[:, g, :], in0=U[:, zc:zc + 256], in1=hs[:, g, :], op=ALU.add), rd=[U, hs], wr=[hs])
                    cx.op("act", lambda a, g=g: a.activation(out=hsb[:, g, :], in_=hs[:, g, :], func=AF.Copy),
                          rd=[hs], wr=[hsb])
                if d == 0:
                    cx.dma("sp", rows(self.hfwd, c), yb[b][:, :], rd=[yb[b]], owner=yb[b])
                    continue
                if not (need_ctx or c < nlat):
                    continue
                cx.op("pool", lambda g: g.tensor_tensor(out=yb[b][:, :], in0=yb[b][:, :], in1=yf[b][:, :], op=ALU.add),
                      rd=[yb[b], yf[b]], wr=[yb[b]])
                cx.op("pool", lambda g: g.tensor_tensor(out=v64(yg[:, :]), in0=v64(xt[b][:, :]),
                                                        in1=bc64(sm[:, 128:160]), op=ALU.mult),
                      rd=[xt[b], sm], wr=[yg])
                cx.op("pool", lambda g: g.tensor_tensor(out=yg[:, :], in0=yg[:, :], in1=yb[b][:, :], op=ALU.add),
                      rd=[yg, yb[b]], wr=[yg])
                cx.op("dve", lambda v: v.tensor_tensor(out=yg[:, :], in0=yg[:, :], in1=szc[b][:, :], op=ALU.mult),
                      rd=[yg, szc[b]], wr=[yg])
                cx.op("act", lambda a: a.activation(out=junk[:, :], in_=yg[:, :], func=AF.Square, accum_out=ss[:, 0:1]),
                      rd=[yg], wr=[junk, ss])
                cx.op("act", lambda a: a.activation(out=ss[:, :], in_=ss[:, :], func=AF.Sqrt, bias=self.eps_col[:, 0:1],
                                                    scale=1.0 / 2048), rd=[ss, self.eps_col], wr=[ss])
                cx.op("dve", lambda v: v.reciprocal(out=ss[:, :], in_=ss[:, :]), rd=[ss], wr=[ss])
                cx.op("dve", lambda v: v.scalar_tensor_tensor(
                    out=yn[:, :], in0=yg[:, :], scalar=ss[:, 0:1], in1=ngb[:, :], op0=ALU.mult, op1=ALU.mult),
                    rd=[yg, ss, ngb], wr=[yn])
                for r in range(2):
                    tp = self.ps[r]
                    tpv = tp[:, :].bitcast(BF16)
                    for q in range(8):
                        cc = r * 8 + q
                        cx.op("pe", lambda p, cc=cc, q=q, tpv=tpv: p.transpose(
                            tpv[:, q * 128:(q + 1) * 128], yn[:, cc * 128:(cc + 1) * 128], self.identb[:, :]),
                            rd=[yn, self.identb], wr=[tp])
                    cx.op("act", lambda a, r=r, tpv=tpv: a.activation(
                        out=yTo[b][:, r * 8:(r + 1) * 8, :], in_=tpv.rearrange("p (a b) -> p a b", b=128),
                        func=AF.Copy), rd=[tp], wr=[yTo[b]])
                cx.dma("sp", self.yT.ap().rearrange("(cc p) t -> p cc t", p=128)[:, :, c * 128:(c + 1) * 128],
                       yTo[b][:, :, :], rd=[yTo[b]], owner=yTo[b])


def prep_core_inputs(inp, b):
    f = np.float32
    xin = np.ascontiguousarray(np.concatenate([inp["x"][b].T, inp["ctx"][b].T], axis=1), dtype=f)
    cT = np.stack([inp["c"][b].reshape(KC, 128).T, inp["c_ctx"].reshape(KC, 128).T], axis=-1)
    return {"xin": xin, "cT": np.ascontiguousarray(cT, dtype=f)}


def prep_shared_inputs(inp):
    f = np.float32
    depth = inp["ada_w"].shape[0]
    sh = {}
    sh["ada_w"] = np.ascontiguousarray(inp["ada_w"], dtype=f)
    sh["ada_bT"] = np.ascontiguousarray(inp["ada_b"].reshape(depth, 72, 128).transpose(0, 2, 1), dtype=f)
    sh["gT"] = np.ascontiguousarray(inp["norm_g"].reshape(depth, 6, KC, 128).transpose(0, 3, 1, 2), dtype=f)
    sh["w_gate"] = np.ascontiguousarray(inp["ffn_w_gate"], dtype=f)
    sh["w_up"] = np.ascontiguousarray(inp["ffn_w_up"], dtype=f)
    sh["w_down"] = np.ascontiguousarray(inp["ffn_w_down"], dtype=f)
    sh["ml_w_in"] = np.ascontiguousarray(inp["mlstm_w_in"][0], dtype=f)
    cw = np.concatenate([inp["mlstm_conv_w"][0], inp["mlstm_conv_b"][0][None]], 0)
    sh["ml_conv"] = np.ascontiguousarray(cw.reshape(6, 16, 128).transpose(2, 1, 0), dtype=f)
    bd = np.zeros((3, 16, 128, 128), f)
    for i, nm in enumerate(("mlstm_w_q", "mlstm_w_k", "mlstm_w_v")):
        w = inp[nm][0].reshape(16, 32, 4, 4)
        for n in range(32):
            bd[i, :, n * 4:(n + 1) * 4, n * 4:(n + 1) * 4] = w[:, n]
    sh["ml_bd"] = np.ascontiguousarray(bd.transpose(0, 2, 1, 3))
    perm = np.array([d * 8 + io * 4 + h for io in range(2) for d in range(2) for h in range(4)])
    wg = inp["mlstm_w_gates"][0][:, perm]
    sh["ml_wg"] = np.ascontiguousarray(wg.reshape(48, 128, 16).transpose(1, 0, 2), dtype=f)
    sh["ml_bg"] = np.ascontiguousarray(inp["mlstm_b_gates"][0][perm].reshape(16, 1), dtype=f)
    ns = np.stack([inp["mlstm_norm_g"][0], inp["mlstm_skip"][0]], 0)
    sh["ml_ns"] = np.ascontiguousarray(ns.reshape(2, 16, 128).transpose(2, 0, 1), dtype=f)
    sh["ml_w_out"] = np.ascontiguousarray(inp["mlstm_w_out"][0], dtype=f)
    sh["sd_w_in"] = np.ascontiguousarray(inp["ssd_w_in"][0], dtype=f)
    cw = np.concatenate([inp["ssd_conv_w"][0], inp["ssd_conv_b"][0][None]], 0)
    sh["sd_conv"] = np.ascontiguousarray(cw.reshape(6, 32, 128).transpose(2, 1, 0), dtype=f)
    small = np.concatenate([inp["ssd_dt_bias"][0].reshape(64), inp["ssd_a_log"][0].reshape(64), inp["ssd_d"][0]])
    sh["sd_smallT"] = np.ascontiguousarray(np.broadcast_to(small[None], (128, 160)), dtype=f)
    sh["sd_ng"] = np.ascontiguousarray(np.broadcast_to(inp["ssd_norm_g"][0][None], (128, 2048)), dtype=f)
    sh["sd_w_out"] = np.ascontiguousarray(inp["ssd_w_out"][0], dtype=f)
    return sh


def kernel(**inp):
    inp = {k: np.asarray(v) for k, v in inp.items()}
    B = inp["x"].shape[0]
    cx = Ctx()
    net = Net(cx)
    net.build()
    sh = prep_shared_inputs(inp)
    in_maps = []
    for b in range(B):
        m = dict(sh)
        m.update(prep_core_inputs(inp, b))
        in_maps.append(m)
    res = run_bass_kernel_spmd(cx.nc, in_maps, core_ids=list(range(B)))
    out = np.stack([np.ascontiguousarray(r["out"].T) for r in res.results], axis=0)
    return out.astype(np.float32)
```

```python
from contextlib import ExitStack
import numpy as np
import concourse.bass as bass
import concourse.mybir as mybir
from concourse.bass_utils import run_bass_kernel_spmd

F32 = mybir.dt.float32
BF16 = mybir.dt.bfloat16
ALU = mybir.AluOpType
AF = mybir.ActivationFunctionType
AX = mybir.AxisListType

D = 1024
KC = D // 128
DFF = 2816
FC = DFF // 128
EPS = 1e-6
NCORES = 8


class Buf:
    __slots__ = ("name", "w", "r", "dram", "dsem", "psum")

    def __init__(self, name, dram=False, psum=False):
        self.name = name
        self.w = {}
        self.r = {}
        self.dram = dram
        self.dsem = None
        self.psum = psum


class Tile:
    def __init__(self, h, buf):
        self.h = h
        self.b = buf

    def __getitem__(self, k):
        return self.h[k]


class Ctx:
    ENG = ("pe", "dve", "act", "pool", "sp")

    def __init__(self):
        nc = self.nc = bass.Bass("TRN2", target_bir_lowering=False)
        self.engs = dict(pe=nc.tensor, dve=nc.vector, act=nc.scalar, pool=nc.gpsimd, sp=nc.sync)
        self.semh = {}
        self.owner = {}
        self.cur = {}
        self.cnt = {}
        self.dcnt = {}
        self.seen = {e: {} for e in self.ENG}
        self.gen = 0
        for e in self.ENG:
            self._newsem(e)
        self.n_ins = 0
        self.n_wait = 0
        self.free_dsems = []

    def _newsem(self, e):
        self.gen += 1
        name = f"s_{e}_{self.gen}"
        self.semh[name] = self.nc.alloc_semaphore(name)
        self.owner[name] = e
        self.cur[e] = name
        self.cnt[e] = 0

    def sb(self, es, name, shape, dt):
        self.uid = getattr(self, "uid", 0) + 1
        name = f"{name}_{self.uid}"
        h = es.enter_context(self.nc.sbuf_tensor(name, list(shape), dt))
        t = Tile(h, Buf(name))
        es.callback(self._release, t.b)
        return t

    def _release(self, buf):
        if buf.dsem is not None:
            self.free_dsems.append(buf.dsem)
            buf.dsem = None

    def psum(self, es, name, shape, dt=F32):
        h = es.enter_context(self.nc.psum_tensor(name, list(shape), dt))
        return Tile(h, Buf(name, psum=True))

    def dram(self, name, shape, dt, kind="Internal"):
        h = self.nc.dram_tensor(name, list(shape), dt, kind=kind)
        return h

    def _val(self, name, val):
        return 16 * self.dcnt[name] if val is None else val

    def _gather(self, e, rd, wr):
        toks = {}

        def add(name, val, skip_same):
            if skip_same and self.owner[name] == e:
                return
            v = self._val(name, val)
            if toks.get(name, 0) < v:
                toks[name] = v

        pe = e == "pe"
        for b in rd:
            for n, v in b.w.items():
                add(n, v, pe)
            if b.psum:
                for n, v in b.r.items():
                    if self.owner[n] != e:
                        add(n, v, False)
        for b in wr:
            for n, v in b.w.items():
                add(n, v, pe)
            for n, v in b.r.items():
                add(n, v, pe)
        return toks

    def _wait(self, e, toks):
        for name, v in toks.items():
            if self.seen[e].get(name, 0) < v:
                self.engs[e].wait_ge(self.semh[name], v)
                self.seen[e][name] = v
                self.n_wait += 1

    def op(self, e, fn, rd=(), wr=()):
        rd = [t.b if isinstance(t, Tile) else t for t in rd]
        wr = [t.b if isinstance(t, Tile) else t for t in wr]
        self._wait(e, self._gather(e, rd, wr))
        ins = fn(self.engs[e])
        name = self.cur[e]
        self.cnt[e] += 1
        ins.then_inc(self.semh[name], 1)
        v = self.cnt[e]
        for b in rd:
            b.r[name] = v
        for b in wr:
            b.w = {name: v}
            b.r = {}
        if v >= 30000:
            self._newsem(e)
        self.n_ins += 1
        return ins

    def dma(self, q, out, in_, rd=(), wr=(), owner=None):
        rd = [t.b if isinstance(t, Tile) else t for t in rd]
        wr = [t.b if isinstance(t, Tile) else t for t in wr]
        owner = owner.b if isinstance(owner, Tile) else owner
        self._wait(q, self._gather("dma", rd, wr))
        ins = self.engs[q].dma_start(out=out, in_=in_)
        if owner.dsem is None:
            if self.free_dsems:
                name = self.free_dsems.pop()
            else:
                name = f"d_{owner.name}"
                self.semh[name] = self.nc.alloc_semaphore(name)
                self.owner[name] = None
                self.dcnt[name] = 0
            owner.dsem = name
        name = owner.dsem
        self.dcnt[name] += 1
        ins.then_inc(self.semh[name], 16)
        for b in rd:
            b.r[name] = None
        for b in wr:
            if b.dram:
                b.w[name] = None
            else:
                b.w = {name: None}
                b.r = {}
        self.n_ins += 1
        return ins

    def barrier(self):
        toks = {}
        for e in self.ENG:
            if self.cnt[e] > 0:
                toks[self.cur[e]] = self.cnt[e]
        for n, c in self.dcnt.items():
            if c > 0:
                toks[n] = 16 * c
        for e in self.ENG:
            t = {n: v for n, v in toks.items() if self.owner[n] != e}
            self._wait(e, t)


def load_weight_bf16(cx, q, Wd_ap, Wbf, stg, col0, ncol, kc):
    src = Wd_ap.rearrange("(kc p) m -> p kc m", p=128)[:, :, col0:col0 + ncol]
    cx.dma(q, stg[:, 0:kc, 0:ncol], src, wr=[stg], owner=stg)
    cx.op("pool", lambda g: g.tensor_copy(out=Wbf[:, :, col0:col0 + ncol], in_=stg[:, 0:kc, 0:ncol]),
          rd=[stg], wr=[Wbf.b if isinstance(Wbf, Tile) else Wbf])


class Net:
    def __init__(self, cx, T_lat=4096, T_ctx=256, depth=2, tile_n=512, debug_stop=None):
        self.cx = cx
        self.nc = cx.nc
        self.TL, self.TC = T_lat, T_ctx
        self.T = T_lat + T_ctx
        self.depth = depth
        self.debug_stop = debug_stop
        self.tiles = [(s, tile_n, 0) for s in range(0, T_lat, tile_n)] + [(T_lat, T_ctx, 1)]
        self.declare_io()

    def declare_io(self):
        nc, T = self.nc, self.T
        di = lambda n, s: nc.dram_tensor(n, list(s), F32, kind="ExternalInput")
        self.xin = di("xin", [D, T])
        self.cT = di("cT", [128, KC, 2])
        self.ada_w = di("ada_w", [self.depth, D, 9 * D])
        self.ada_bT = di("ada_bT", [self.depth, 128, 72])
        self.gT = di("gT", [self.depth, 128, 6, KC])
        self.w_gate = di("w_gate", [self.depth, 2, D, DFF])
        self.w_up = di("w_up", [self.depth, 2, D, DFF])
        self.w_down = di("w_down", [self.depth, 2, DFF, D])
        self.ml_w_in = di("ml_w_in", [D, 4096])
        self.ml_conv = di("ml_conv", [128, 16, 6])
        self.ml_bd = di("ml_bd", [3, 128, 16, 128])
        self.ml_wg = di("ml_wg", [128, 48, 16])
        self.ml_bg = di("ml_bg", [16, 1])
        self.ml_ns = di("ml_ns", [128, 2, 16])
        self.ml_w_out = di("ml_w_out", [2048, D])
        self.sd_w_in = di("sd_w_in", [D, 6208])
        self.sd_conv = di("sd_conv", [128, 32, 6])
        self.sd_smallT = di("sd_smallT", [128, 160])
        self.sd_ng = di("sd_ng", [128, 2048])
        self.sd_w_out = di("sd_w_out", [2048, D])
        self.out = nc.dram_tensor("out", [D, self.TL], F32, kind="ExternalOutput")
        NCH = T // 128
        self.NCH = NCH
        dm = lambda n, s, dt: nc.dram_tensor(n, list(s), dt, kind="ExternalOutput" if self.debug_stop else "Internal")
        self.szT = dm("szT", [2048, T], BF16)
        self.xcsT = dm("xcsT", [2048, T], BF16)
        self.qT = dm("qT", [2048, T], BF16)
        self.kT = dm("kT", [2048, T], BF16)
        self.ktm = dm("ktm", [T, 2048], BF16)
        self.vtm = dm("vtm", [T, 2048], BF16)
        self.hfwd = dm("hfwd", [T, 2048], F32)
        self.sztm = dm("sztm", [T, 2048], BF16)
        self.xtm = dm("xtm", [T, 2048], BF16)
        self.Btm = dm("Btm", [T, 1024], BF16)
        self.BT = dm("BT", [1024, T], BF16)
        self.CT = dm("CT", [1024, T], BF16)
        self.yT = nc.dram_tensor("yT", [2048, T], BF16,
                                 kind="ExternalOutput" if self.debug_stop else "Internal")
        self.yT_b = [Buf(f"yT{i}", dram=True) for i in range(len(self.tiles))]
        self.xres = nc.dram_tensor("xres", [D, T], F32,
                                   kind="ExternalOutput" if self.debug_stop else "Internal")
        self.uT = nc.dram_tensor("uT", [DFF, T], BF16, kind="Internal")
        nt = len(self.tiles)
        self.xres_b = [Buf(f"xres{i}", dram=True) for i in range(nt)]
        self.xin_b = [Buf(f"xin{i}", dram=True) for i in range(nt)]
        self.out_b = [Buf(f"out{i}", dram=True) for i in range(nt)]
        self.uT_b = [Buf(f"uT{i}", dram=True) for i in range(nt)]

    def build(self):
        cx = self.cx
        with ExitStack() as es:
            self.es0 = es
            self.ps = [cx.psum(es, f"ps{i}", [128, 512]) for i in range(8)]
            self.ones_bf = cx.sb(es, "ones_bf", [128, 128], BF16)
            cx.op("pool", lambda g: g.memset(self.ones_bf[:], 1.0 / D), wr=[self.ones_bf])
            self.eps_col = cx.sb(es, "eps_col", [128, 1], F32)
            cx.op("pool", lambda g: g.memset(self.eps_col[:], EPS), wr=[self.eps_col])
            self.one_col = cx.sb(es, "one_col", [128, 1], F32)
            cx.op("pool", lambda g: g.memset(self.one_col[:], 1.0), wr=[self.one_col])
            self.ones_f = cx.sb(es, "ones_f", [128, 128], F32)
            cx.op("pool", lambda g: g.memset(self.ones_f[:], 1.0), wr=[self.ones_f])
            self.ident = cx.sb(es, "ident", [128, 128], F32)
            self.maskU = cx.sb(es, "maskU", [128, 128], F32)
            self.maskL = cx.sb(es, "maskL", [128, 128], F32)
            self.identb = cx.sb(es, "identb", [128, 128], BF16)
            for t_, pat, cm, cmp_ in ((self.ident, [[1, 128]], -1, ALU.is_equal), (self.maskU, [[1, 128]], -1, ALU.is_ge),
                                      (self.maskL, [[-1, 128]], 1, ALU.is_ge)):
                cx.op("pool", lambda g, t_=t_, pat=pat, cm=cm, cmp_=cmp_: g.affine_select(
                    out=t_[:], in_=self.ones_f[:], pattern=pat, compare_op=cmp_, fill=0.0, base=0,
                    channel_multiplier=cm), rd=[self.ones_f], wr=[t_])
            cx.op("pool", lambda g: g.tensor_copy(out=self.identb[:], in_=self.ident[:]), rd=[self.ident],
                  wr=[self.identb])
            self.nmaskU = cx.sb(es, "nmaskU", [128, 128], F32)
            self.nmaskL = cx.sb(es, "nmaskL", [128, 128], F32)
            for nm_, mk_ in ((self.nmaskU, self.maskU), (self.nmaskL, self.maskL)):
                cx.op("pool", lambda g, nm_=nm_, mk_=mk_: g.tensor_scalar(
                    out=nm_[:], in0=mk_[:], scalar1=-1.0, scalar2=None, op0=ALU.mult), rd=[mk_], wr=[nm_])
            self.negU = cx.sb(es, "negU", [128, 128], BF16)
            self.negL = cx.sb(es, "negL", [128, 128], BF16)
            for ng_, mk_ in ((self.negU, self.maskU), (self.negL, self.maskL)):
                cx.op("pool", lambda g, ng_=ng_, mk_=mk_: g.tensor_scalar(
                    out=ng_[:], in0=mk_[:], scalar1=-1.0, scalar2=30000.0, op0=ALU.add, op1=ALU.mult),
                    rd=[mk_], wr=[ng_])
            self.mod = cx.sb(es, "mod", [128, 72, 2], F32)
            self.par = cx.sb(es, "par", [128, 9, KC, 2], F32)
            self.sc2 = cx.sb(es, "sc2", [128, KC, 2], F32)
            self.gsb = cx.sb(es, "gsb", [128, 6, KC], F32)
            self.adab = cx.sb(es, "adab", [128, 72], F32)
            cx.dma("sp", self.sc2[:], self.cT[:], wr=[self.sc2], owner=self.sc2)
            cx.op("act", lambda a: a.activation(out=self.sc2[:], in_=self.sc2[:], func=AF.Silu),
                  rd=[self.sc2], wr=[self.sc2])
            for li in range(self.depth):
                if self.layer(li):
                    break
            cx.barrier()

    def layer(self, li):
        last = li == self.depth - 1
        ds = self.debug_stop
        self.adaln(li)
        src = (self.xin, self.xin_b) if li == 0 else (self.xres, self.xres_b)
        self.ffn(li, 0, 0, src, (self.xres, self.xres_b), ctx_needed=True)
        if ds == (li, "ffn1"):
            return True
        if li % 2 == 0:
            self.mlstm(li, not last)
        else:
            self.ssd(li, not last)
        if ds and ds[0] == li and ds[1] in ("ml_in", "mixer", "ssd_tab", "ssd_d0"):
            return True
        dst = (self.out, self.out_b) if (last and not ds) else (self.xres, self.xres_b)
        self.ffn(li, 1, 2, (self.xres, self.xres_b), dst, ctx_needed=not last)
        return ds == (li, "layer")

    def adaln(self, li):
        cx = self.cx
        cx.barrier()
        with ExitStack() as es:
            NG = 1152
            stg = [cx.sb(es, f"adastg{i}", [128, KC, NG], F32) for i in range(2)]
            cx.dma("sp", self.adab[:], self.ada_bT[li], wr=[self.adab], owner=self.adab)
            cx.dma("sp", self.gsb[:], self.gT[li], wr=[self.gsb], owner=self.gsb)
            mps = self.ps[0]
            for gi in range(9 * D // NG):
                st = stg[gi % 2]
                src = self.ada_w[li].rearrange("(kc p) m -> p kc m", p=128)[:, :, gi * NG:(gi + 1) * NG]
                cx.dma("sp", st[:], src, wr=[st], owner=st)
                for j in range(NG // 128):
                    mc = gi * (NG // 128) + j
                    for k in range(KC):
                        cx.op("pe", lambda p, k=k, j=j, mc=mc, st=st: p.matmul(
                            mps[:, 2 * mc:2 * mc + 2], lhsT=st[:, k, j * 128:(j + 1) * 128],
                            rhs=self.sc2[:, k, :], start=(k == 0), stop=(k == KC - 1)),
                            rd=[st, self.sc2], wr=[mps])
            cx.op("dve", lambda v: v.tensor_tensor(
                out=self.mod[:], in0=mps[:, 0:144].rearrange("p (m c) -> p m c", c=2),
                in1=self.adab[:].unsqueeze(2).to_broadcast([128, 72, 2]), op=ALU.add),
                rd=[mps, self.adab], wr=[self.mod])
            md = lambda j: self.mod[:, j * KC:(j + 1) * KC, :]
            gb = lambda j: self.gsb[:, j, :].unsqueeze(2).to_broadcast([128, KC, 2])
            P = self.par
            spec = [
                (0, "A", 1, 0, 1.0), (1, "B", 0, None, 1.0), (2, "G", 2, 1, 0.5),
                (3, "A", 4, 2, 1.0), (4, "B", 3, None, 1.0), (5, "G", 5, 3, 1.0),
                (6, "A", 7, 4, 1.0), (7, "B", 6, None, 1.0), (8, "G", 8, 5, 0.5)]
            for idx, kind, mj, gj, fac in spec:
                if kind == "A":
                    cx.op("dve", lambda v, idx=idx, mj=mj, gj=gj: v.scalar_tensor_tensor(
                        out=P[:, idx], in0=md(mj), scalar=1.0, in1=gb(gj), op0=ALU.add, op1=ALU.mult),
                        rd=[self.mod, self.gsb], wr=[P])
                elif kind == "B":
                    cx.op("dve", lambda v, idx=idx, mj=mj: v.tensor_copy(out=P[:, idx], in_=md(mj)),
                          rd=[self.mod], wr=[P])
                else:
                    cx.op("dve", lambda v, idx=idx, mj=mj, gj=gj, fac=fac: v.scalar_tensor_tensor(
                        out=P[:, idx], in0=md(mj), scalar=fac, in1=gb(gj), op0=ALU.mult, op1=ALU.mult),
                        rd=[self.mod, self.gsb], wr=[P])

    def rsqrt_eps(self, rstd, ms_ps, n):
        cx = self.cx
        cx.op("act", lambda a: a.activation(out=rstd[:, 0:n], in_=ms_ps[:, 0:n], func=AF.Sqrt,
                                            bias=self.eps_col[:, 0:1], scale=1.0), rd=[ms_ps, self.eps_col],
              wr=[rstd])
        cx.op("dve", lambda v: v.reciprocal(out=rstd[:, 0:n], in_=rstd[:, 0:n]), rd=[rstd], wr=[rstd])

    def norm_mod(self, es_bufs, x, sq, hT, n, c, pidx, ssq_ps, rstd):
        cx = self.cx
        cx.op("act", lambda a: a.activation(out=sq[:, :, 0:n], in_=x[:, :, 0:n], func=AF.Square),
              rd=[x], wr=[sq])
        for k in range(KC):
            cx.op("pe", lambda p, k=k: p.matmul(ssq_ps[:, 0:n], lhsT=self.ones_bf[:], rhs=sq[:, k, 0:n],
                                                start=(k == 0), stop=(k == KC - 1)),
                  rd=[self.ones_bf, sq], wr=[ssq_ps])
        self.rsqrt_eps(rstd, ssq_ps, n)
        cx.op("dve", lambda v: v.tensor_tensor(
            out=x[:, :, 0:n], in0=x[:, :, 0:n],
            in1=rstd[:, 0:n].unsqueeze(1).to_broadcast([128, KC, n]), op=ALU.mult), rd=[x, rstd], wr=[x])
        for k in range(KC):
            cx.op("act", lambda a, k=k: a.activation(
                out=hT[:, k, 0:n], in_=x[:, k, 0:n], func=AF.Identity,
                scale=self.par[:, pidx, k, c:c + 1], bias=self.par[:, pidx + 1, k, c:c + 1]),
                rd=[x, self.par], wr=[hT])

    def ffn(self, li, half, pbase, src, dst, ctx_needed):
        cx = self.cx
        tiles = self.tiles if ctx_needed else self.tiles[:-1]
        pidx = pbase * 3
        srcT, srcB = src
        dstT, dstB = dst
        xv = lambda t: t.ap().rearrange("(kc p) t -> p kc t", p=128)
        cx.barrier()
        with ExitStack() as es:
            Wg = cx.sb(es, "Wg", [128, KC, DFF], BF16)
            Wu = cx.sb(es, "Wu", [128, KC, DFF], BF16)
            NGC = 352
            ngrp = DFF // NGC
            Wgb = [Buf(f"Wg{i}") for i in range(ngrp)]
            Wub = [Buf(f"Wu{i}") for i in range(ngrp)]
            stg = [cx.sb(es, f"wstg{i}", [128, KC, NGC], F32) for i in range(2)]
            xt = [cx.sb(es, f"xt{i}", [128, KC, 512], F32) for i in range(2)]
            hT = [cx.sb(es, f"hT{i}", [128, KC, 512], BF16) for i in range(2)]
            sq = cx.sb(es, "sq", [128, KC, 512], BF16)
            rstd = cx.sb(es, "rstd", [128, 512], F32)
            sg = [cx.sb(es, f"sg{i}", [128, 512], F32) for i in range(2)]
            uo = [cx.sb(es, f"uo{i}", [128, FC // 2, 512], BF16) for i in range(2)]
            ssq_ps = self.ps[0]
            gps = [self.ps[1], self.ps[2]]
            ups = [self.ps[3], self.ps[4]]
            def load_x(i):
                s, n, c = tiles[i]
                cx.dma("sp", xt[i % 2][:, :, 0:n], xv(srcT)[:, :, s:s + n], rd=[srcB[i]], wr=[xt[i % 2]],
                       owner=xt[i % 2])
            load_x(0)
            si = 0
            for g in range(ngrp):
                for (Wdram, Wsb, Wb) in ((self.w_gate, Wg, Wgb), (self.w_up, Wu, Wub)):
                    st = stg[si % 2]
                    si += 1
                    srcw = Wdram[li, half].rearrange("(kc p) m -> p kc m", p=128)[:, :, g * NGC:(g + 1) * NGC]
                    cx.dma("sp", st[:], srcw, wr=[st], owner=st)
                    cx.op("pool", lambda gp, Wsb=Wsb, st=st, g=g: gp.tensor_copy(
                        out=Wsb[:, :, g * NGC:(g + 1) * NGC], in_=st[:]), rd=[st], wr=[Wb[g]])
            for i, (s, n, c) in enumerate(tiles):
                if i + 1 < len(tiles):
                    load_x(i + 1)
                x = xt[i % 2]
                h = hT[i % 2]
                self.norm_mod(None, x, sq, h, n, c, pidx, ssq_ps, rstd)
                for f in range(FC):
                    gp_, up_ = gps[f % 2], ups[f % 2]
                    wb = [Wgb[(f * 128) // NGC], Wgb[(f * 128 + 127) // NGC]]
                    wb2 = [Wub[(f * 128) // NGC], Wub[(f * 128 + 127) // NGC]]
                    for k in range(KC):
                        cx.op("pe", lambda p, k=k, f=f, gp_=gp_: p.matmul(
                            gp_[:, 0:n], lhsT=Wg[:, k, f * 128:(f + 1) * 128], rhs=h[:, k, 0:n],
                            start=(k == 0), stop=(k == KC - 1)), rd=[h] + wb, wr=[gp_])
                    for k in range(KC):
                        cx.op("pe", lambda p, k=k, f=f, up_=up_: p.matmul(
                            up_[:, 0:n], lhsT=Wu[:, k, f * 128:(f + 1) * 128], rhs=h[:, k, 0:n],
                            start=(k == 0), stop=(k == KC - 1)), rd=[h] + wb2, wr=[up_])
                    sgt = sg[f % 2]
                    cx.op("act", lambda a, gp_=gp_, sgt=sgt: a.activation(out=sgt[:, 0:n], in_=gp_[:, 0:n],
                                                                          func=AF.Silu), rd=[gp_], wr=[sgt])
                    uh = uo[f // (FC // 2)]
                    fo = f % (FC // 2)
                    cx.op("dve", lambda v, up_=up_, sgt=sgt, uh=uh, fo=fo: v.tensor_tensor(
                        out=uh[:, fo, 0:n], in0=up_[:, 0:n], in1=sgt[:, 0:n], op=ALU.mult),
                        rd=[up_, sgt], wr=[uh])
                    if fo == FC // 2 - 1:
                        hh = f // (FC // 2)
                        dv = self.uT.ap().rearrange("(fc p) t -> p fc t", p=128)[
                            :, hh * (FC // 2):(hh + 1) * (FC // 2), s:s + n]
                        cx.dma("sp", dv, uh[:, :, 0:n], rd=[uh], wr=[self.uT_b[i]], owner=uh)
        self.proj_res(self.w_down[li, half], FC, self.uT, self.uT_b, pidx + 2, src, dst, tiles)

    def proj_res(self, W_ap, kc, inT, inB, gidx, src, dst, tiles):
        cx = self.cx
        srcT, srcB = src
        dstT, dstB = dst
        xv = lambda t: t.ap().rearrange("(kc p) t -> p kc t", p=128)
        cx.barrier()
        with ExitStack() as es:
            Wd = cx.sb(es, "Wd", [128, kc, D], BF16)
            NGC = 256
            ngrp = D // NGC
            Wdb = [Buf(f"Wd{i}") for i in range(ngrp)]
            stg = [cx.sb(es, f"wstg{i}", [128, kc, NGC], F32) for i in range(2)]
            ut = [cx.sb(es, f"ut{i}", [128, kc, 512], BF16) for i in range(2)]
            xt = [cx.sb(es, f"xt{i}", [128, KC, 512], F32) for i in range(2)]
            y = cx.sb(es, "y", [128, KC, 512], F32)
            sq = cx.sb(es, "sq", [128, KC, 512], BF16)
            rstd = cx.sb(es, "rstd", [128, 512], F32)
            ssq_ps = self.ps[0]
            yps = [self.ps[1], self.ps[2], self.ps[3]]

            def load_t(i):
                s, n, c = tiles[i]
                uv = inT.ap().rearrange("(fc p) t -> p fc t", p=128)[:, :, s:s + n]
                cx.dma("sp", ut[i % 2][:, :, 0:n], uv, rd=[inB[i]], wr=[ut[i % 2]], owner=ut[i % 2])
                cx.dma("sp", xt[i % 2][:, :, 0:n], xv(srcT)[:, :, s:s + n], rd=[srcB[i]], wr=[xt[i % 2]],
                       owner=xt[i % 2])
            load_t(0)
            for g in range(ngrp):
                st = stg[g % 2]
                srcw = W_ap.rearrange("(fc p) m -> p fc m", p=128)[:, :, g * NGC:(g + 1) * NGC]
                cx.dma("sp", st[:], srcw, wr=[st], owner=st)
                cx.op("pool", lambda gp, st=st, g=g: gp.tensor_copy(
                    out=Wd[:, :, g * NGC:(g + 1) * NGC], in_=st[:]), rd=[st], wr=[Wdb[g]])
            for i, (s, n, c) in enumerate(tiles):
                if i + 1 < len(tiles):
                    load_t(i + 1)
                u, x = ut[i % 2], xt[i % 2]
                for d in range(KC):
                    yp = yps[d % 3]
                    for f in range(kc):
                        cx.op("pe", lambda p, d=d, f=f, yp=yp: p.matmul(
                            yp[:, 0:n], lhsT=Wd[:, f, d * 128:(d + 1) * 128], rhs=u[:, f, 0:n],
                            start=(f == 0), stop=(f == kc - 1)), rd=[u, Wdb[(d * 128) // NGC]], wr=[yp])
                    cx.op("act", lambda a, d=d, yp=yp: a.activation(out=y[:, d, 0:n], in_=yp[:, 0:n],
                                                                    func=AF.Copy), rd=[yp], wr=[y])
                self.post_norm_res(y, sq, rstd, ssq_ps, x, n, c, gidx)
                sdst = dstT.ap().rearrange("(kc p) t -> p kc t", p=128)[:, :, s:s + n]
                cx.dma("sp", sdst, x[:, :, 0:n], rd=[x], wr=[dstB[i]], owner=x)

    def post_norm_res(self, y, sq, rstd, ssq_ps, x, n, c, gidx):
        cx = self.cx
        cx.op("act", lambda a: a.activation(out=sq[:, :, 0:n], in_=y[:, :, 0:n], func=AF.Square),
              rd=[y], wr=[sq])
        for k in range(KC):
            cx.op("pe", lambda p, k=k: p.matmul(ssq_ps[:, 0:n], lhsT=self.ones_bf[:], rhs=sq[:, k, 0:n],
                                                start=(k == 0), stop=(k == KC - 1)),
                  rd=[self.ones_bf, sq], wr=[ssq_ps])
        self.rsqrt_eps(rstd, ssq_ps, n)
        cx.op("dve", lambda v: v.tensor_tensor(
            out=y[:, :, 0:n], in0=y[:, :, 0:n],
            in1=rstd[:, 0:n].unsqueeze(1).to_broadcast([128, KC, n]), op=ALU.mult), rd=[y, rstd], wr=[y])
        for k in range(KC):
            cx.op("dve", lambda v, k=k: v.scalar_tensor_tensor(
                out=x[:, k, 0:n], in0=y[:, k, 0:n], scalar=self.par[:, gidx, k, c:c + 1], in1=x[:, k, 0:n],
                op0=ALU.mult, op1=ALU.add), rd=[y, x, self.par], wr=[x])


    def chunk_order(self, d):
        nl = self.TL // 128
        nc_ = self.TC // 128
        ctx = list(range(nl, nl + nc_))
        lat = list(range(nl))
        return ctx + lat if d == 0 else ctx[::-1] + lat[::-1]

    def mlstm(self, li, need_ctx):
        cx = self.cx
        T, NCH = self.T, self.NCH
        cx.barrier()
        with ExitStack() as es:
            self.gT16 = cx.sb(es, "gT16", [16, T], F32)
            self.ml_ns_sb = cx.sb(es, "ml_ns_sb", [128, 2, 16], F32)
            cx.dma("sp", self.ml_ns_sb[:], self.ml_ns.ap(), wr=[self.ml_ns_sb], owner=self.ml_ns_sb)
            self.Wtm = [cx.sb(es, f"Wtm{d}", [128, NCH, 4], F32) for d in range(2)]
            self.Wtmb = [cx.sb(es, f"Wtmb{d}", [128, NCH, 4], BF16) for d in range(2)]
            self.Ftm = [cx.sb(es, f"Ftm{d}", [128, NCH, 4], F32) for d in range(2)]
            self.decbc = [cx.sb(es, f"decbc{d}", [128, NCH, 4], F32) for d in range(2)]
            self.mlstm_in(li)
            if self.debug_stop == (li, "ml_in"):
                return
            self.mlstm_gates()
            for d in range(2):
                self.mlstm_scan(d)
        tiles = self.tiles if need_ctx else self.tiles[:-1]
        self.proj_res(self.ml_w_out.ap(), 16, self.yT, self.yT_b, 5, (self.xres, self.xres_b),
                      (self.xres, self.xres_b), tiles)

    def mlstm_in(self, li):
        cx = self.cx
        tiles = self.tiles
        cx.barrier()
        with ExitStack() as es:
            Win = cx.sb(es, "Win", [128, KC, 4096], BF16)
            Winb = [Buf(f"Win{g}") for g in range(8)]
            bd = cx.sb(es, "bd", [128, 3, 16, 128], BF16)
            wg = cx.sb(es, "wg", [128, 48, 16], BF16)
            conv = cx.sb(es, "conv", [128, 16, 6], F32)
            bg = cx.sb(es, "bg", [16, 1], F32)
            stg = [cx.sb(es, f"wstg{i}", [128, KC, 128], F32) for i in range(2)]
            sstg = Tile(stg[0][:, :, :].rearrange("p a b -> p (a b)"), stg[0].b)
            x = cx.sb(es, "x", [128, KC, 512], F32)
            hT = cx.sb(es, "hT", [128, KC, 512], BF16)
            sq = cx.sb(es, "sq", [128, KC, 512], BF16)
            rstd = cx.sb(es, "rstd", [128, 512], F32)
            xmf = [cx.sb(es, f"xmf{i}", [128, 512], F32) for i in range(2)]
            acc = [cx.sb(es, f"acc{i}", [128, 512], F32) for i in range(2)]
            xmb = [cx.sb(es, f"xmb{i}", [128, 512], BF16) for i in range(2)]
            vTs = [cx.sb(es, f"vTs{i}", [128, 512], BF16) for i in range(3)]
            xcb = [cx.sb(es, f"xcb{i}", [128, 4, 512], BF16) for i in range(2)]
            mk = lambda nm, k=2: [cx.sb(es, f"{nm}{i}", [128, 4, 512], BF16) for i in range(k)] * (2 // k)
            szo, xcso, qTo, kTo, ktmo, vtmo = mk("szo", 1), mk("xcso", 1), mk("qTo"), mk("kTo"), mk("ktmo"), mk("vtmo")
            cx.dma("sp", conv[:], self.ml_conv.ap(), wr=[conv], owner=conv)
            cx.dma("sp", bg[:], self.ml_bg.ap(), wr=[bg], owner=bg)
            for i in range(3):
                for hf_ in range(2):
                    cx.dma("sp", sstg[:, :].rearrange("p (a b) -> p a b", b=128),
                           self.ml_bd[i][:, hf_ * 8:(hf_ + 1) * 8, :], wr=[sstg], owner=sstg)
                    cx.op("pool", lambda g, i=i, hf_=hf_: g.tensor_copy(
                        out=bd[:, i, hf_ * 8:(hf_ + 1) * 8, :],
                        in_=sstg[:, :].rearrange("p (a b) -> p a b", b=128)), rd=[sstg], wr=[bd])
            cx.dma("sp", sstg[:, 0:768].rearrange("p (a b) -> p a b", b=16), self.ml_wg.ap(), wr=[sstg], owner=sstg)
            cx.op("pool", lambda g: g.tensor_copy(out=wg[:], in_=sstg[:, 0:768].rearrange(
                "p (a b) -> p a b", b=16)), rd=[sstg], wr=[wg])

            def load_x(i):
                s, n, c = tiles[i]
                cx.dma("sp", x[:, :, 0:n], self.xres.ap().rearrange("(kc p) t -> p kc t", p=128)[:, :, s:s + n],
                       rd=[self.xres_b[i]], wr=[x], owner=x)
            load_x(0)
            gorder = [g for q in range(16) for g in (q, 16 + q)]
            for gi, g in enumerate(gorder):
                st = stg[gi % 2]
                srcw = self.ml_w_in.ap().rearrange("(kc p) m -> p kc m", p=128)[:, :, g * 128:(g + 1) * 128]
                cx.dma("sp", st[:], srcw, wr=[st], owner=st)
                cx.op("pool", lambda gp, st=st, g=g: gp.tensor_copy(
                    out=Win[:, :, g * 128:(g + 1) * 128], in_=st[:]), rd=[st], wr=[Winb[g // 4]])
            g_ps = self.ps[0]
            prod = [self.ps[5], self.ps[6], self.ps[7]]
            pcount = [0]

            def nextp():
                pcount[0] += 1
                return prod[pcount[0] % 3]

            for i, (s, n, c) in enumerate(tiles):
                ntb = n // 128
                self.norm_mod(None, x, sq, hT, n, c, 3, self.ps[0], rstd)
                if i + 1 < len(tiles):
                    load_x(i + 1)
                R = n if c else 64
                v3 = lambda ap: ap.rearrange("p (r w) -> p r w", w=R)

                def stageA(cc):
                    gq, jj = cc // 4, cc % 4
                    ob = gq % 2
                    xm_ps = self.ps[1 + cc % 2]
                    z_ps = self.ps[3 + cc % 2]
                    for k in range(KC):
                        cx.op("pe", lambda p, k=k: p.matmul(
                            xm_ps[:, 0:n], lhsT=Win[:, k, cc * 128:(cc + 1) * 128], rhs=hT[:, k, 0:n],
                            start=(k == 0), stop=(k == KC - 1)), rd=[hT, Winb[cc // 4]], wr=[xm_ps])
                    for k in range(KC):
                        cx.op("pe", lambda p, k=k: p.matmul(
                            z_ps[:, 0:n], lhsT=Win[:, k, 2048 + cc * 128:2048 + (cc + 1) * 128], rhs=hT[:, k, 0:n],
                            start=(k == 0), stop=(k == KC - 1)), rd=[hT, Winb[4 + cc // 4]], wr=[z_ps])
                    xf, ac, xb = xmf[cc % 2], acc[cc % 2], xmb[cc % 2]
                    cx.op("act", lambda a: a.activation(out=xf[:, 0:n], in_=xm_ps[:, 0:n], func=AF.Copy),
                          rd=[xm_ps], wr=[xf])
                    cx.op("act", lambda a: a.activation(out=xb[:, 0:n], in_=xm_ps[:, 0:n], func=AF.Copy),
                          rd=[xm_ps], wr=[xb])
                    cx.op("act", lambda a: a.activation(out=szo[ob][:, jj, 0:n], in_=z_ps[:, 0:n], func=AF.Silu),
                          rd=[z_ps], wr=[szo[ob]])
                    cx.op("dve", lambda v: v.tensor_scalar(
                        out=ac[:, 0:n], in0=xf[:, 0:n], scalar1=conv[:, cc, 2:3], scalar2=conv[:, cc, 5:6],
                        op0=ALU.mult, op1=ALU.add), rd=[xf, conv], wr=[ac])
                    for j in (0, 1, 3, 4):
                        sh = j - 2
                        o0, o1 = max(0, -sh), R - max(0, sh)
                        i0, i1 = max(0, sh), R - max(0, -sh)
                        cx.op("dve", lambda v, j=j, o0=o0, o1=o1, i0=i0, i1=i1: v.scalar_tensor_tensor(
                            out=v3(ac[:, 0:n])[:, :, o0:o1], in0=v3(xf[:, 0:n])[:, :, i0:i1],
                            scalar=conv[:, cc, j:j + 1], in1=v3(ac[:, 0:n])[:, :, o0:o1],
                            op0=ALU.mult, op1=ALU.add), rd=[xf, ac, conv], wr=[ac])
                    cx.op("act", lambda a: a.activation(out=xcb[ob][:, jj, 0:n], in_=ac[:, 0:n], func=AF.Silu),
                          rd=[ac], wr=[xcb[ob]])
                    cx.op("pool", lambda g: g.tensor_scalar(
                        out=xcso[ob][:, jj, 0:n], in0=xcb[ob][:, jj, 0:n], scalar1=self.ml_ns_sb[:, 1, cc:cc + 1],
                        scalar2=None, op0=ALU.mult), rd=[xcb[ob], self.ml_ns_sb], wr=[xcso[ob]])
                    if jj == 3:
                        fm = lambda t: t.ap().rearrange("(cc p) t -> p cc t", p=128)[:, gq * 4:(gq + 1) * 4, s:s + n]
                        for dt_, ot in ((self.szT, szo[ob]), (self.xcsT, xcso[ob])):
                            cx.dma("sp", fm(dt_), ot[:, :, 0:n], rd=[ot], owner=ot)

                def stageB(cc):
                    gq, jj = cc // 4, cc % 4
                    ob = gq % 2
                    xb = xmb[cc % 2]
                    vt = vTs[cc % 3]
                    for wi, (rhs_t, rhs_ap, dst_t, dst_ap, eng) in enumerate((
                            (xcb[ob], xcb[ob][:, jj, 0:n], qTo[ob], qTo[ob][:, jj, 0:n], "act"),
                            (xcb[ob], xcb[ob][:, jj, 0:n], kTo[ob], kTo[ob][:, jj, 0:n], "dve"),
                            (xb, xb[:, 0:n], vt, vt[:, 0:n], "act"))):
                        pp = nextp()
                        cx.op("pe", lambda p, wi=wi, rhs_ap=rhs_ap, pp=pp: p.matmul(
                            pp[:, 0:n], lhsT=bd[:, wi, cc, :], rhs=rhs_ap, start=True, stop=True),
                            rd=[bd, rhs_t], wr=[pp])
                        if eng == "act":
                            cx.op("act", lambda a, pp=pp, dst_ap=dst_ap: a.activation(
                                out=dst_ap, in_=pp[:, 0:n], func=AF.Copy), rd=[pp], wr=[dst_t])
                        else:
                            cx.op("dve", lambda v, pp=pp, dst_ap=dst_ap: v.tensor_copy(out=dst_ap, in_=pp[:, 0:n]),
                                  rd=[pp], wr=[dst_t])
                    for wi, lt, lap, dst_t in ((1, xcb[ob], lambda tb: xcb[ob][:, jj, tb * 128:(tb + 1) * 128], ktmo[ob]),
                                               (2, xb, lambda tb: xb[:, tb * 128:(tb + 1) * 128], vtmo[ob])):
                        pp = nextp()
                        for tb in range(ntb):
                            cx.op("pe", lambda p, wi=wi, lap=lap, tb=tb, pp=pp: p.matmul(
                                pp[:, tb * 128:(tb + 1) * 128], lhsT=lap(tb), rhs=bd[:, wi, cc, :],
                                start=True, stop=True), rd=[bd, lt], wr=[pp])
                        cx.op("dve", lambda v, pp=pp, dst_t=dst_t: v.tensor_copy(
                            out=dst_t[:, 0:ntb, jj * 128:(jj + 1) * 128],
                            in_=pp[:, 0:ntb * 128].rearrange("p (a b) -> p a b", b=128)), rd=[pp], wr=[dst_t])

                def stageC(cc):
                    gq, jj = cc // 4, cc % 4
                    ob = gq % 2
                    vt = vTs[cc % 3]
                    for wi, (rt, rap) in enumerate(((qTo[ob], qTo[ob][:, jj, 0:n]), (kTo[ob], kTo[ob][:, jj, 0:n]),
                                                    (vt, vt[:, 0:n]))):
                        cx.op("pe", lambda p, wi=wi, rap=rap: p.matmul(
                            g_ps[0:16, 0:n], lhsT=wg[:, wi * 16 + cc, :], rhs=rap,
                            start=(cc == 0 and wi == 0), stop=(cc == 15 and wi == 2)), rd=[wg, rt], wr=[g_ps])
                    if jj == 3:
                        fm = lambda t: t.ap().rearrange("(cc p) t -> p cc t", p=128)[:, gq * 4:(gq + 1) * 4, s:s + n]
                        for dt_, ot in ((self.qT, qTo[ob]), (self.kT, kTo[ob])):
                            cx.dma("sp", fm(dt_), ot[:, :, 0:n], rd=[ot], owner=ot)
                        tmv = lambda t: t.ap()[s:s + n, gq * 512:(gq + 1) * 512].rearrange("(tb p) ch -> p tb ch", p=128)
                        for dt_, ot in ((self.ktm, ktmo[ob]), (self.vtm, vtmo[ob])):
                            cx.dma("sp", tmv(dt_), ot[:, 0:ntb, :], rd=[ot], owner=ot)

                for step in range(16 + 2):
                    if step < 16:
                        stageA(step)
                    if 0 <= step - 1 < 16:
                        stageB(step - 1)
                    if 0 <= step - 2 < 16:
                        stageC(step - 2)
                cx.op("act", lambda a: a.activation(out=self.gT16[:, s:s + n], in_=g_ps[0:16, 0:n], func=AF.Identity,
                                                    bias=bg[:, 0:1], scale=1.0), rd=[g_ps, bg], wr=[self.gT16])

    def mlstm_gates(self):
        cx = self.cx
        T, NCH = self.T, self.NCH
        cx.barrier()
        with ExitStack() as es:
            mk = lambda nm: cx.sb(es, nm, [4, T], F32)
            IG, FG, CS, A, P, TM = mk("IG"), mk("FG"), mk("CS"), mk("A"), mk("P"), mk("TM")
            mk2 = lambda nm: cx.sb(es, nm, [4, NCH], F32)
            Mb, Mend, dec = mk2("Mb"), mk2("Mend"), mk2("dec")
            ones4 = cx.sb(es, "ones4", [4, 128], F32)
            R = cx.sb(es, "R", [4, NCH, 4], F32)
            cx.op("pool", lambda g: g.memset(ones4[:], 1.0), wr=[ones4])
            c3 = lambda t: t[:, :].rearrange("p (c t) -> p c t", t=128)
            for d in range(2):
                order = self.chunk_order(d)
                rv = (lambda ap: ap[:, ::-1]) if d == 1 else (lambda ap: ap)
                for s0 in range(0, T, 512):
                    n = min(512, T - s0)
                    for dst, c0 in ((IG, d * 4), (FG, 8 + d * 4)):
                        pp = self.ps[1 + (s0 // 512) % 2] if dst is IG else self.ps[3 + (s0 // 512) % 2]
                        cx.op("pe", lambda p, pp=pp, c0=c0, s0=s0, n=n: p.matmul(
                            pp[0:4, 0:n], lhsT=self.ident[0:16, c0:c0 + 4], rhs=self.gT16[:, s0:s0 + n],
                            start=True, stop=True), rd=[self.ident, self.gT16], wr=[pp])
                        cx.op("act", lambda a, pp=pp, dst=dst, s0=s0, n=n: a.activation(
                            out=dst[:, s0:s0 + n], in_=pp[0:4, 0:n], func=AF.Copy), rd=[pp], wr=[dst])
                cx.op("act", lambda a: a.activation(out=FG[:, :], in_=FG[:, :], func=AF.Exp, scale=-1.0),
                      rd=[FG], wr=[FG])
                cx.op("act", lambda a: a.activation(out=FG[:, :], in_=FG[:, :], func=AF.Ln, bias=self.one_col[0:4, 0:1],
                                                    scale=1.0), rd=[FG, self.one_col], wr=[FG])
                for c in range(NCH):
                    sl = slice(c * 128, (c + 1) * 128)
                    cx.op("dve", lambda v, sl=sl: v.tensor_tensor_scan(
                        out=rv(CS[:, sl]), data0=ones4[:, :], data1=rv(FG[:, sl]), initial=0.0,
                        op0=ALU.mult, op1=ALU.add), rd=[FG, ones4], wr=[CS])
                cx.op("dve", lambda v: v.tensor_tensor(out=A[:, :], in0=IG[:, :], in1=CS[:, :], op=ALU.add),
                      rd=[IG, CS], wr=[A])
                for c in range(NCH):
                    sl = slice(c * 128, (c + 1) * 128)
                    cx.op("dve", lambda v, sl=sl: v.tensor_tensor_scan(
                        out=rv(P[:, sl]), data0=rv(A[:, sl]), data1=rv(A[:, sl]), initial=-1e30,
                        op0=ALU.max, op1=ALU.max), rd=[A], wr=[P])
                last = 127 if d == 0 else 0
                csl = c3(CS)[:, :, last]
                pend = c3(P)[:, :, last]
                cx.op("pool", lambda g: g.memset(Mb[:, :], 0.0), wr=[Mb])
                for i, c in enumerate(order):
                    cx.op("dve", lambda v, c=c: v.tensor_tensor(out=Mend[:, c:c + 1], in0=Mb[:, c:c + 1],
                                                                in1=pend[:, c:c + 1], op=ALU.max),
                          rd=[Mb, P], wr=[Mend])
                    if i + 1 < len(order):
                        cn = order[i + 1]
                        cx.op("dve", lambda v, c=c, cn=cn: v.tensor_tensor(
                            out=Mb[:, cn:cn + 1], in0=Mend[:, c:c + 1], in1=csl[:, c:c + 1], op=ALU.subtract),
                            rd=[Mend, CS], wr=[Mb])
                cx.op("dve", lambda v: v.tensor_tensor(out=dec[:, :], in0=Mb[:, :], in1=Mend[:, :], op=ALU.subtract),
                      rd=[Mb, Mend], wr=[dec])
                cx.op("act", lambda a: a.activation(out=dec[:, :], in_=dec[:, :], func=AF.Exp), rd=[dec], wr=[dec])
                mbc = Mend[:, :].unsqueeze(2).to_broadcast([4, NCH, 128])
                cx.op("dve", lambda v: v.tensor_tensor(out=c3(TM), in0=c3(A), in1=mbc, op=ALU.subtract),
                      rd=[A, Mend], wr=[TM])
                cx.op("act", lambda a: a.activation(out=TM[:, :], in_=TM[:, :], func=AF.Exp), rd=[TM], wr=[TM])
                cx.op("dve", lambda v: v.scalar_tensor_tensor(
                    out=c3(A), in0=c3(CS), scalar=float(0.5 * np.log(512.0)), in1=mbc, op0=ALU.add, op1=ALU.subtract),
                    rd=[CS, Mend], wr=[A])
                cx.op("act", lambda a: a.activation(out=A[:, :], in_=A[:, :], func=AF.Exp), rd=[A], wr=[A])
                for src, pp, dsts in ((TM, self.ps[5], (self.Wtm[d], self.Wtmb[d])), (A, self.ps[6], (self.Ftm[d],))):
                    for c in range(NCH):
                        cx.op("pe", lambda p, c=c, src=src, pp=pp: p.transpose(
                            pp[:, c * 4:(c + 1) * 4], src[:, c * 128:(c + 1) * 128], self.ident[0:4, 0:4]),
                            rd=[src, self.ident], wr=[pp])
                    for dst in dsts:
                        cx.op("dve", lambda v, pp=pp, dst=dst: v.tensor_copy(
                            out=dst[:, :, :], in_=pp[:, 0:NCH * 4].rearrange("p (c h) -> p c h", h=4)),
                            rd=[pp], wr=[dst])
                cx.op("dve", lambda v: v.tensor_tensor(
                    out=R[:, :, :], in0=dec[:, :].unsqueeze(2).to_broadcast([4, NCH, 4]),
                    in1=self.ident[0:4, 0:4].unsqueeze(1).to_broadcast([4, NCH, 4]), op=ALU.mult),
                    rd=[dec, self.ident], wr=[R])
                pp = self.ps[7]
                cx.op("pe", lambda p, pp=pp: p.matmul(pp[:, 0:NCH * 4], lhsT=self.ones_f[0:4, :],
                                                      rhs=R[:, :, :].rearrange("p c h -> p (c h)"),
                                                      start=True, stop=True), rd=[self.ones_f, R], wr=[pp])
                cx.op("dve", lambda v, pp=pp: v.tensor_copy(
                    out=self.decbc[d][:, :, :], in_=pp[:, 0:NCH * 4].rearrange("p (c h) -> p c h", h=4)),
                    rd=[pp], wr=[self.decbc[d]])

    def mlstm_scan(self, d):
        cx = self.cx
        T, NCH = self.T, self.NCH
        order = self.chunk_order(d)
        mask = self.maskU if d == 0 else self.maskL
        Wtm, Wtmb, Ftm, decbc = self.Wtm[d], self.Wtmb[d], self.Ftm[d], self.decbc[d]
        cx.barrier()
        with ExitStack() as es:
            C = [cx.sb(es, f"C{h}", [128, 4, 512], F32) for h in range(4)]
            Cd = [cx.sb(es, f"Cd{h}", [128, 4, 512], BF16) for h in range(4)]
            nv = cx.sb(es, "nv", [128, 4, 4], F32)
            nd = cx.sb(es, "nd", [128, 4, 4], BF16)
            for h in range(4):
                cx.op("pool", lambda g, h=h: g.memset(C[h][:, :, :], 0.0), wr=[C[h]])
                cx.op("pool", lambda g, h=h: g.memset(Cd[h][:, :, :], 0.0), wr=[Cd[h]])
            cx.op("pool", lambda g: g.memset(nv[:, :, :], 0.0), wr=[nv])
            cx.op("pool", lambda g: g.memset(nd[:, :, :], 0.0), wr=[nd])
            mk = lambda nm, shp, dt, k=2: [cx.sb(es, f"{nm}{i}", shp, dt) for i in range(k)]
            qTc, kTc = mk("qTc", [128, 16, 128], BF16), mk("kTc", [128, 16, 128], BF16)
            ktc, vtc = mk("ktc", [128, 2048], BF16), mk("vtc", [128, 2048], BF16)
            SmT = mk("SmT", [128, 128], BF16)
            vw = mk("vw", [128, 512], BF16)
            den = mk("den", [128, 1], F32, 4)
            hb = mk("hb", [128, 2048], F32)
            if d == 1:
                hf = mk("hf", [128, 2048], F32)
                hn = cx.sb(es, "hn", [128, 2048], BF16)
                st = cx.sb(es, "st", [128, 4, 6], F32)
                mv = cx.sb(es, "mv", [128, 4, 2], F32)
                rs = cx.sb(es, "rs", [128, 4], F32)
                xcsc, szc = mk("xcsc", [128, 16, 128], BF16), mk("szc", [128, 16, 128], BF16)
                t1 = mk("t1", [128, 8, 128], F32)
                yTo = mk("yTo", [128, 16, 128], BF16)
            fmv = lambda t, c: t.ap().rearrange("(cc p) t -> p cc t", p=128)[:, :, c * 128:(c + 1) * 128]

            def loads(ci):
                c = order[ci]
                b = ci % 2
                tb = self.yT_b[0]
                cx.dma("sp", qTc[b][:, :, :], fmv(self.qT, c), wr=[qTc[b]], owner=qTc[b])
                cx.dma("sp", kTc[b][:, :, :], fmv(self.kT, c), wr=[kTc[b]], owner=kTc[b])
                cx.dma("sp", ktc[b][:, :], self.ktm.ap()[c * 128:(c + 1) * 128, :], wr=[ktc[b]], owner=ktc[b])
                cx.dma("sp", vtc[b][:, :], self.vtm.ap()[c * 128:(c + 1) * 128, :], wr=[vtc[b]], owner=vtc[b])
                if d == 1:
                    cx.dma("sp", hf[b][:, :], self.hfwd.ap()[c * 128:(c + 1) * 128, :], wr=[hf[b]], owner=hf[b])
                    cx.dma("sp", xcsc[b][:, :, :], fmv(self.xcsT, c), wr=[xcsc[b]], owner=xcsc[b])
                    cx.dma("sp", szc[b][:, :, :], fmv(self.szT, c), wr=[szc[b]], owner=szc[b])
            loads(0)
            items = [(ci, c, h) for ci, c in enumerate(order) for h in range(4)]

            def stage1(it):
                ci, c, h = items[it]
                b = ci % 2
                S_ps = self.ps[it % 2]
                sm, vwt = SmT[it % 2], vw[it % 2]
                for j in range(4):
                    cx.op("pe", lambda p, j=j: p.matmul(
                        S_ps[:, 0:128], lhsT=kTc[b][:, h * 4 + j, :], rhs=qTc[b][:, h * 4 + j, :],
                        start=(j == 0), stop=(j == 3)), rd=[kTc[b], qTc[b]], wr=[S_ps])
                cx.op("act", lambda a: a.activation(
                    out=vwt[:, :], in_=vtc[b][:, h * 512:(h + 1) * 512], func=AF.Copy, scale=Wtm[:, c, h:h + 1]),
                    rd=[vtc[b], Wtm], wr=[vwt])
                cx.op("dve", lambda v: v.tensor_tensor(out=sm[:, :], in0=S_ps[:, 0:128], in1=mask[:, :],
                                                       op=ALU.mult), rd=[S_ps, mask], wr=[sm])

            def stage2(it):
                ci, c, h = items[it]
                b = ci % 2
                cn = order[ci + 1] if ci + 1 < len(order) else None
                N_ps = self.ps[2 + it % 2]
                D_ps = self.ps[4]
                dcol = (it % 8) * 8
                sm, vwt, dn = SmT[it % 2], vw[it % 2], den[it % 4]
                cx.op("pe", lambda p: p.matmul(N_ps[:, :], lhsT=sm[:, :], rhs=vwt[:, :], start=True, stop=False),
                      rd=[sm, vwt], wr=[N_ps])
                for j in range(4):
                    cx.op("pe", lambda p, j=j: p.matmul(
                        N_ps[:, :], lhsT=qTc[b][:, h * 4 + j, :], rhs=Cd[h][:, j, :], start=False, stop=(j == 3)),
                        rd=[qTc[b], Cd[h]], wr=[N_ps])
                cx.op("pe", lambda p: p.matmul(D_ps[:, dcol:dcol + 1], lhsT=sm[:, :], rhs=Wtmb[:, c, h:h + 1],
                                               start=True, stop=False), rd=[sm, Wtmb], wr=[D_ps])
                for j in range(4):
                    cx.op("pe", lambda p, j=j: p.matmul(
                        D_ps[:, dcol:dcol + 1], lhsT=qTc[b][:, h * 4 + j, :], rhs=nd[:, h, j:j + 1],
                        start=False, stop=(j == 3)), rd=[qTc[b], nd], wr=[D_ps])
                ucol = dcol + 4
                for j in range(4):
                    cx.op("pe", lambda p, j=j: p.matmul(
                        D_ps[:, ucol + j:ucol + j + 1],
                        lhsT=ktc[b][:, h * 512 + j * 128:h * 512 + (j + 1) * 128], rhs=Wtmb[:, c, h:h + 1],
                        start=True, stop=True), rd=[ktc[b], Wtmb], wr=[D_ps])
                cx.op("act", lambda a: a.activation(out=dn[:, :], in_=D_ps[:, dcol:dcol + 1], func=AF.Abs),
                      rd=[D_ps], wr=[dn])
                cx.op("dve", lambda v: v.tensor_scalar(
                    out=dn[:, :], in0=dn[:, :], scalar1=Ftm[:, c, h:h + 1], scalar2=None,
                    op0=ALU.max), rd=[dn, Ftm], wr=[dn])
                cx.op("dve", lambda v: v.reciprocal(out=dn[:, :], in_=dn[:, :]), rd=[dn], wr=[dn])
                cx.op("act", lambda a: a.activation(out=hb[b][:, h * 512:(h + 1) * 512], in_=N_ps[:, :],
                                                    func=AF.Copy, scale=dn[:, 0:1]), rd=[N_ps, dn], wr=[hb[b]])
                cx.op("dve", lambda v: v.scalar_tensor_tensor(
                    out=nv[:, h, :], in0=nv[:, h, :], scalar=decbc[:, c, h:h + 1], in1=D_ps[:, ucol:ucol + 4],
                    op0=ALU.mult, op1=ALU.add), rd=[nv, decbc, D_ps], wr=[nv])
                if cn is not None:
                    cx.op("dve", lambda v: v.tensor_scalar(
                        out=nd[:, h, :], in0=nv[:, h, :], scalar1=decbc[:, cn, h:h + 1], scalar2=None,
                        op0=ALU.mult), rd=[nv, decbc], wr=[nd])
                for j in range(4):
                    U_ps = self.ps[5 + j % 2]
                    cx.op("pe", lambda p, j=j, U_ps=U_ps: p.matmul(
                        U_ps[:, :], lhsT=ktc[b][:, h * 512 + j * 128:h * 512 + (j + 1) * 128], rhs=vwt[:, :],
                        start=True, stop=True), rd=[ktc[b], vwt], wr=[U_ps])
                    cx.op("dve", lambda v, j=j, U_ps=U_ps: v.scalar_tensor_tensor(
                        out=C[h][:, j, :], in0=C[h][:, j, :], scalar=decbc[:, c, h:h + 1], in1=U_ps[:, :],
                        op0=ALU.mult, op1=ALU.add), rd=[C[h], decbc, U_ps], wr=[C[h]])
                    if cn is not None:
                        cx.op("act", lambda a, j=j: a.activation(
                            out=Cd[h][:, j, :], in_=C[h][:, j, :], func=AF.Copy, scale=decbc[:, cn, h:h + 1]),
                            rd=[C[h], decbc], wr=[Cd[h]])

            stage1(0)
            for it, (ci, c, h) in enumerate(items):
                b = ci % 2
                if h == 0 and ci + 1 < len(order):
                    loads(ci + 1)
                if it + 1 < len(items):
                    stage1(it + 1)
                stage2(it)
                if h != 3:
                    continue
                hrow = self.hfwd.ap()[c * 128:(c + 1) * 128, :]
                if d == 0:
                    cx.dma("sp", hrow, hb[b][:, :], rd=[hb[b]], owner=hb[b])
                    continue
                cx.op("pool", lambda g: g.tensor_tensor(out=hb[b][:, :], in0=hb[b][:, :], in1=hf[b][:, :], op=ALU.add),
                      rd=[hb[b], hf[b]], wr=[hb[b]])
                for h in range(4):
                    cx.op("dve", lambda v, h=h: v.bn_stats(out=st[:, h, :], in_=hb[b][:, h * 512:(h + 1) * 512]),
                          rd=[hb[b]], wr=[st])
                    cx.op("dve", lambda v, h=h: v.bn_aggr(out=mv[:, h, :], in_=st[:, h, :]), rd=[st], wr=[mv])
                cx.op("act", lambda a: a.activation(out=rs[:, :], in_=mv[:, :, 1], func=AF.Sqrt,
                                                    bias=self.eps_col[:, 0:1], scale=1.0), rd=[mv, self.eps_col], wr=[rs])
                cx.op("dve", lambda v: v.reciprocal(out=rs[:, :], in_=rs[:, :]), rd=[rs], wr=[rs])
                for h in range(4):
                    cx.op("dve", lambda v, h=h: v.tensor_scalar(
                        out=hn[:, h * 512:(h + 1) * 512], in0=hb[b][:, h * 512:(h + 1) * 512],
                        scalar1=mv[:, h, 0:1], scalar2=rs[:, h:h + 1], op0=ALU.subtract, op1=ALU.mult),
                        rd=[hb[b], mv, rs], wr=[hn])
                tp = self.ps[7]
                tpv = tp[:, :].bitcast(BF16)
                for r in range(2):
                    for q in range(8):
                        cc = r * 8 + q
                        cx.op("pe", lambda p, cc=cc, q=q: p.transpose(
                            tpv[:, q * 128:(q + 1) * 128], hn[:, cc * 128:(cc + 1) * 128], self.identb[:, :]),
                            rd=[hn, self.identb], wr=[tp])
                    tt = t1[r]
                    cx.op("dve", lambda v, r=r, tt=tt: v.tensor_tensor(
                        out=tt[:, :, :], in0=tpv.rearrange("p (a b) -> p a b", b=128),
                        in1=self.ml_ns_sb[:, 0, r * 8:(r + 1) * 8].unsqueeze(2).to_broadcast([128, 8, 128]),
                        op=ALU.mult), rd=[tp, self.ml_ns_sb], wr=[tt])
                    cx.op("pool", lambda g, r=r, tt=tt: g.tensor_tensor(
                        out=tt[:, :, :], in0=tt[:, :, :], in1=xcsc[b][:, r * 8:(r + 1) * 8, :], op=ALU.add),
                        rd=[tt, xcsc[b]], wr=[tt])
                    cx.op("pool", lambda g, r=r, tt=tt: g.tensor_tensor(
                        out=yTo[b][:, r * 8:(r + 1) * 8, :], in0=tt[:, :, :], in1=szc[b][:, r * 8:(r + 1) * 8, :],
                        op=ALU.mult), rd=[tt, szc[b]], wr=[yTo[b]])
                cx.dma("sp", fmv(self.yT, c), yTo[b][:, :, :], rd=[yTo[b]], owner=yTo[b])


    def ssd(self, li, need_ctx):
        cx = self.cx
        NCH = self.NCH
        cx.barrier()
        with ExitStack() as es:
            self.dtr_sb = cx.sb(es, "dtr_sb", [128, NCH, 64], F32)
            self.sd_small = cx.sb(es, "sd_small", [128, 160], F32)
            cx.dma("sp", self.sd_small[:, :], self.sd_smallT.ap(), wr=[self.sd_small], owner=self.sd_small)
            cx.op("act", lambda a: a.activation(out=self.sd_small[:, 64:128], in_=self.sd_small[:, 64:128],
                                                func=AF.Exp), rd=[self.sd_small], wr=[self.sd_small])
            cx.op("dve", lambda v: v.tensor_scalar(out=self.sd_small[:, 64:128], in0=self.sd_small[:, 64:128],
                                                   scalar1=-1.0, scalar2=None, op0=ALU.mult),
                  rd=[self.sd_small], wr=[self.sd_small])
            self.ssd_in_z(li)
            self.ssd_in_x(li)
            if self.debug_stop == (li, "ml_in"):
                return
            for d in range(2):
                self.ssd_scan(d, need_ctx)
                if self.debug_stop and self.debug_stop[1] in ("ssd_tab", "ssd_d0"):
                    return
        tiles = self.tiles if need_ctx else self.tiles[:-1]
        self.proj_res(self.sd_w_out.ap(), 16, self.yT, self.yT_b, 5, (self.xres, self.xres_b),
                      (self.xres, self.xres_b), tiles)

    def _load_x(self, x, i):
        s, n, c = self.tiles[i]
        self.cx.dma("sp", x[:, :, 0:n], self.xres.ap().rearrange("(kc p) t -> p kc t", p=128)[:, :, s:s + n],
                    rd=[self.xres_b[i]], wr=[x], owner=x)

    def ssd_in_z(self, li):
        cx = self.cx
        tiles = self.tiles
        cx.barrier()
        with ExitStack() as es:
            NZ = 2048
            Wz = cx.sb(es, "Wz", [128, KC, NZ + 64], BF16)
            Wzb = [Buf(f"Wz{g}") for g in range(9)]
            stg = [cx.sb(es, f"wstg{i}", [128, KC, 256], F32) for i in range(2)]
            x = cx.sb(es, "x", [128, KC, 512], F32)
            hT = cx.sb(es, "hT", [128, KC, 512], BF16)
            sq = cx.sb(es, "sq", [128, KC, 512], BF16)
            rstd = cx.sb(es, "rstd", [128, 512], F32)
            szo = [cx.sb(es, f"szo{i}", [128, 4, NZ], BF16) for i in range(2)]
            self._load_x(x, 0)
            wv = self.sd_w_in.ap().rearrange("(kc p) m -> p kc m", p=128)
            for g in range(9):
                st = stg[g % 2]
                if g < 8:
                    cx.dma("sp", st[:], wv[:, :, g * 256:(g + 1) * 256], wr=[st], owner=st)
                    cx.op("pool", lambda gp, st=st, g=g: gp.tensor_copy(
                        out=Wz[:, :, g * 256:(g + 1) * 256], in_=st[:]), rd=[st], wr=[Wzb[g]])
                else:
                    cx.dma("sp", st[:, :, 0:64], wv[:, :, 6144:6208], wr=[st], owner=st)
                    cx.op("pool", lambda gp, st=st: gp.tensor_copy(
                        out=Wz[:, :, NZ:NZ + 64], in_=st[:, :, 0:64]), rd=[st], wr=[Wzb[8]])
            cnt = 0
            for i, (s, n, c) in enumerate(tiles):
                ntb = n // 128
                self.norm_mod(None, x, sq, hT, n, c, 3, self.ps[0], rstd)
                if i + 1 < len(tiles):
                    self._load_x(x, i + 1)
                so = szo[i % 2]
                for tb in range(ntb):
                    for zb in range(4):
                        cnt += 1
                        zp = self.ps[1 + cnt % 4]
                        for k in range(KC):
                            cx.op("pe", lambda p, k=k, zp=zp: p.matmul(
                                zp[:, :], lhsT=hT[:, k, tb * 128:(tb + 1) * 128], rhs=Wz[:, k, zb * 512:(zb + 1) * 512],
                                start=(k == 0), stop=(k == KC - 1)), rd=[hT, Wzb[2 * zb], Wzb[2 * zb + 1]], wr=[zp])
                        cx.op("act", lambda a, zp=zp: a.activation(out=so[:, tb, zb * 512:(zb + 1) * 512], in_=zp[:, :],
                                                                   func=AF.Silu), rd=[zp], wr=[so])
                    dp = self.ps[5 + tb % 2]
                    for k in range(KC):
                        cx.op("pe", lambda p, k=k, dp=dp: p.matmul(
                            dp[:, 0:64], lhsT=hT[:, k, tb * 128:(tb + 1) * 128], rhs=Wz[:, k, NZ:NZ + 64],
                            start=(k == 0), stop=(k == KC - 1)), rd=[hT, Wzb[8]], wr=[dp])
                    ch = s // 128 + tb
                    cx.op("dve", lambda v, dp=dp, ch=ch: v.tensor_copy(out=self.dtr_sb[:, ch, :], in_=dp[:, 0:64]),
                          rd=[dp], wr=[self.dtr_sb])
                cx.dma("sp", self.sztm.ap()[s:s + n, :].rearrange("(tb p) ch -> p tb ch", p=128), so[:, 0:ntb, :],
                       rd=[so], owner=so)

    def ssd_in_x(self, li):
        cx = self.cx
        tiles = self.tiles
        cx.barrier()
        with ExitStack() as es:
            Wx = cx.sb(es, "Wx", [128, KC, 4096], BF16)
            Wxb = [Buf(f"Wx{g}") for g in range(16)]
            conv = cx.sb(es, "conv", [128, 32, 6], F32)
            stg = [cx.sb(es, f"wstg{i}", [128, KC, 256], F32) for i in range(2)]
            x = cx.sb(es, "x", [128, KC, 512], F32)
            hT = cx.sb(es, "hT", [128, KC, 512], BF16)
            sq = cx.sb(es, "sq", [128, KC, 512], BF16)
            rstd = cx.sb(es, "rstd", [128, 512], F32)
            xf = [cx.sb(es, f"xf{i}", [128, 512], F32) for i in range(2)]
            acc = [cx.sb(es, f"acc{i}", [128, 512], F32) for i in range(2)]
            xc = [cx.sb(es, f"xc{i}", [128, 512], BF16) for i in range(2)]
            xtmo = cx.sb(es, "xtmo", [128, 4, 2048], BF16)
            Btmo = cx.sb(es, "Btmo", [128, 4, 1024], BF16)
            BTo = cx.sb(es, "BTo", [128, 8, 512], BF16)
            CTo = cx.sb(es, "CTo", [128, 8, 512], BF16)
            cx.dma("sp", conv[:], self.sd_conv.ap(), wr=[conv], owner=conv)
            self._load_x(x, 0)
            wv = self.sd_w_in.ap().rearrange("(kc p) m -> p kc m", p=128)
            for g in range(16):
                st = stg[g % 2]
                cx.dma("sp", st[:], wv[:, :, 2048 + g * 256:2048 + (g + 1) * 256], wr=[st], owner=st)
                cx.op("pool", lambda gp, st=st, g=g: gp.tensor_copy(
                    out=Wx[:, :, g * 256:(g + 1) * 256], in_=st[:]), rd=[st], wr=[Wxb[g]])
            for i, (s, n, c) in enumerate(tiles):
                ntb = n // 128
                self.norm_mod(None, x, sq, hT, n, c, 3, self.ps[0], rstd)
                if i + 1 < len(tiles):
                    self._load_x(x, i + 1)
                R = n if c else 64
                v3 = lambda ap: ap.rearrange("p (r w) -> p r w", w=R)

                def dst_of(cc):
                    if cc < 16:
                        return xc[cc % 2], xc[cc % 2][:, 0:n]
                    if cc < 24:
                        return BTo, BTo[:, cc - 16, 0:n]
                    return CTo, CTo[:, cc - 24, 0:n]

                def stageA(cc):
                    xm_ps = self.ps[1 + cc % 2]
                    for k in range(KC):
                        cx.op("pe", lambda p, k=k: p.matmul(
                            xm_ps[:, 0:n], lhsT=Wx[:, k, cc * 128:(cc + 1) * 128], rhs=hT[:, k, 0:n],
                            start=(k == 0), stop=(k == KC - 1)), rd=[hT, Wxb[cc // 2]], wr=[xm_ps])
                    f, ac = xf[cc % 2], acc[cc % 2]
                    cx.op("act", lambda a: a.activation(out=f[:, 0:n], in_=xm_ps[:, 0:n], func=AF.Copy),
                          rd=[xm_ps], wr=[f])
                    cx.op("dve", lambda v: v.tensor_scalar(
                        out=ac[:, 0:n], in0=f[:, 0:n], scalar1=conv[:, cc, 2:3], scalar2=conv[:, cc, 5:6],
                        op0=ALU.mult, op1=ALU.add), rd=[f, conv], wr=[ac])
                    for j in (0, 1, 3, 4):
                        sh = j - 2
                        o0, o1 = max(0, -sh), R - max(0, sh)
                        i0, i1 = max(0, sh), R - max(0, -sh)
                        cx.op("dve", lambda v, j=j, o0=o0, o1=o1, i0=i0, i1=i1: v.scalar_tensor_tensor(
                            out=v3(ac[:, 0:n])[:, :, o0:o1], in0=v3(f[:, 0:n])[:, :, i0:i1],
                            scalar=conv[:, cc, j:j + 1], in1=v3(ac[:, 0:n])[:, :, o0:o1],
                            op0=ALU.mult, op1=ALU.add), rd=[f, ac, conv], wr=[ac])
                    dt_, dap = dst_of(cc)
                    cx.op("act", lambda a: a.activation(out=dap, in_=ac[:, 0:n], func=AF.Silu), rd=[ac], wr=[dt_])

                def stageB(cc):
                    if cc >= 24:
                        return
                    st_, sap = dst_of(cc)
                    tp = self.ps[3 + cc % 2]
                    tpv = tp[:, :].bitcast(BF16)
                    for tb in range(ntb):
                        cx.op("pe", lambda p, tb=tb: p.transpose(
                            tpv[:, tb * 128:(tb + 1) * 128], sap[:, tb * 128:(tb + 1) * 128], self.identb[:, :]),
                            rd=[st_, self.identb], wr=[tp])
                    if cc < 16:
                        ot, oap = xtmo, xtmo[:, 0:ntb, cc * 128:(cc + 1) * 128]
                    else:
                        ot, oap = Btmo, Btmo[:, 0:ntb, (cc - 16) * 128:(cc - 15) * 128]
                    cx.op("dve", lambda v: v.tensor_copy(
                        out=oap, in_=tpv[:, 0:ntb * 128].rearrange("p (a b) -> p a b", b=128)), rd=[tp], wr=[ot])

                for step in range(33):
                    if step < 32:
                        stageA(step)
                    if step >= 1:
                        stageB(step - 1)
                tmv = lambda t: t.ap()[s:s + n, :].rearrange("(tb p) ch -> p tb ch", p=128)
                cx.dma("sp", tmv(self.xtm), xtmo[:, 0:ntb, :], rd=[xtmo], owner=xtmo)
                cx.dma("sp", tmv(self.Btm), Btmo[:, 0:ntb, :], rd=[Btmo], owner=Btmo)
                fm = lambda t: t.ap().rearrange("(g p) t -> p g t", p=128)[:, :, s:s + n]
                cx.dma("sp", fm(self.BT), BTo[:, :, 0:n], rd=[BTo], owner=BTo)
                cx.dma("sp", fm(self.CT), CTo[:, :, 0:n], rd=[CTo], owner=CTo)

    def ssd_scan(self, d, need_ctx):
        cx = self.cx
        T, NCH = self.T, self.NCH
        order = self.chunk_order(d)
        tri = self.maskU if d == 0 else self.maskL
        neg = self.negU if d == 0 else self.negL
        nlat = self.TL // 128
        cx.barrier()
        with ExitStack() as es:
            mkt = lambda nm: cx.sb(es, nm, [128, NCH, 32], F32)
            dt_t, la_t, ncs_t, ecs_t, w2_t, dec_t = mkt("dt_t"), mkt("la_t"), mkt("ncs_t"), mkt("ecs_t"), mkt("w2_t"), mkt("dec_t")
            fl = lambda t: t[:, :, :].rearrange("p c h -> p (c h)")
            sm = self.sd_small
            cx.op("dve", lambda v: v.tensor_tensor(
                out=dt_t[:, :, :], in0=self.dtr_sb[:, :, d * 32:(d + 1) * 32],
                in1=sm[:, d * 32:(d + 1) * 32].unsqueeze(1).to_broadcast([128, NCH, 32]), op=ALU.add),
                rd=[self.dtr_sb, sm], wr=[dt_t])
            cx.op("act", lambda a: a.activation(out=fl(dt_t), in_=fl(dt_t), func=AF.Exp), rd=[dt_t], wr=[dt_t])
            cx.op("act", lambda a: a.activation(out=fl(dt_t), in_=fl(dt_t), func=AF.Ln, bias=self.one_col[:, 0:1],
                                                scale=1.0), rd=[dt_t, self.one_col], wr=[dt_t])
            cx.op("dve", lambda v: v.tensor_tensor(
                out=la_t[:, :, :], in0=dt_t[:, :, :],
                in1=sm[:, 64 + d * 32:64 + (d + 1) * 32].unsqueeze(1).to_broadcast([128, NCH, 32]), op=ALU.mult),
                rd=[dt_t, sm], wr=[la_t])
            NF = NCH * 32
            for c0 in range(0, NF, 512):
                w = min(512, NF - c0)
                csp, cep = self.ps[1], self.ps[2]
                cx.op("pe", lambda p, c0=c0, w=w: p.matmul(csp[:, 0:w], lhsT=tri[:, :], rhs=fl(la_t)[:, c0:c0 + w],
                                                           start=True, stop=True), rd=[tri, la_t], wr=[csp])
                cx.op("pe", lambda p, c0=c0, w=w: p.matmul(cep[:, 0:w], lhsT=self.ones_f[:, :], rhs=fl(la_t)[:, c0:c0 + w],
                                                           start=True, stop=True), rd=[self.ones_f, la_t], wr=[cep])
                cx.op("dve", lambda v, c0=c0, w=w: v.tensor_scalar(
                    out=fl(ncs_t)[:, c0:c0 + w], in0=csp[:, 0:w], scalar1=-1.0, scalar2=None, op0=ALU.mult),
                    rd=[csp], wr=[ncs_t])
                cx.op("act", lambda a, c0=c0, w=w: a.activation(out=fl(ecs_t)[:, c0:c0 + w], in_=csp[:, 0:w],
                                                                func=AF.Exp), rd=[csp], wr=[ecs_t])
                cx.op("act", lambda a, c0=c0, w=w: a.activation(out=fl(dec_t)[:, c0:c0 + w], in_=cep[:, 0:w],
                                                                func=AF.Exp), rd=[cep], wr=[dec_t])
                cx.op("dve", lambda v, c0=c0, w=w: v.tensor_tensor(
                    out=fl(w2_t)[:, c0:c0 + w], in0=cep[:, 0:w], in1=fl(ncs_t)[:, c0:c0 + w], op=ALU.add),
                    rd=[cep, ncs_t], wr=[w2_t])
                cx.op("act", lambda a, c0=c0, w=w: a.activation(out=fl(w2_t)[:, c0:c0 + w], in_=fl(w2_t)[:, c0:c0 + w],
                                                                func=AF.Exp), rd=[w2_t], wr=[w2_t])
            if self.debug_stop and self.debug_stop[1] == "ssd_tab":
                return
            neg4 = cx.sb(es, "neg4", [128, 512], BF16)
            cx.op("pool", lambda g: g.tensor_copy(
                out=neg4[:, :].rearrange("p (r t) -> p r t", t=128),
                in_=neg[:, :].unsqueeze(1).to_broadcast([128, 4, 128])), rd=[neg], wr=[neg4])
            hs = cx.sb(es, "hs", [128, 8, 256], F32)
            hsb = cx.sb(es, "hsb", [128, 8, 256], BF16)
            cx.op("pool", lambda g: g.memset(hs[:, :, :], 0.0), wr=[hs])
            cx.op("pool", lambda g: g.memset(hsb[:, :, :], 0.0), wr=[hsb])
            mk = lambda nm, shp, dt, k=2: [cx.sb(es, f"{nm}{i}", shp, dt) for i in range(k)]
            xt, Bt = mk("xt", [128, 2048], BF16), mk("Bt", [128, 1024], BF16)
            BTc, CTc = mk("BTc", [128, 8, 128], BF16), mk("CTc", [128, 8, 128], BF16)
            xdt, xw = mk("xdt", [128, 2048], BF16), mk("xw", [128, 2048], BF16)
            Eb = mk("Eb", [128, 128], BF16, 4)
            mT = mk("mT", [128, 128], BF16, 4)
            tz = mk("tz", [128, 256], F32)
            yb = mk("yb", [128, 2048], F32)
            if d == 1:
                yf = mk("yf", [128, 2048], F32)
                szc = mk("szc", [128, 2048], BF16)
                ngb = cx.sb(es, "ngb", [128, 2048], F32)
                cx.dma("sp", ngb[:, :], self.sd_ng.ap(), wr=[ngb], owner=ngb)
                yg = cx.sb(es, "yg", [128, 2048], F32)
                junk = cx.sb(es, "junk", [128, 2048], BF16)
                yn = cx.sb(es, "yn", [128, 2048], BF16)
                ss = cx.sb(es, "ss", [128, 1], F32)
                yTo = mk("yTo", [128, 16, 128], BF16)
            fmv = lambda t, c: t.ap().rearrange("(g p) t -> p g t", p=128)[:, :, c * 128:(c + 1) * 128]
            rows = lambda t, c: t.ap()[c * 128:(c + 1) * 128, :]

            def loads(ci):
                c = order[ci]
                b = ci % 2
                cx.dma("sp", xt[b][:, :], rows(self.xtm, c), wr=[xt[b]], owner=xt[b])
                cx.dma("sp", Bt[b][:, :], rows(self.Btm, c), wr=[Bt[b]], owner=Bt[b])
                cx.dma("sp", BTc[b][:, :, :], fmv(self.BT, c), wr=[BTc[b]], owner=BTc[b])
                cx.dma("sp", CTc[b][:, :, :], fmv(self.CT, c), wr=[CTc[b]], owner=CTc[b])
                if d == 1 and (need_ctx or c < nlat):
                    cx.dma("sp", yf[b][:, :], rows(self.hfwd, c), wr=[yf[b]], owner=yf[b])
                    cx.dma("sp", szc[b][:, :], rows(self.sztm, c), wr=[szc[b]], owner=szc[b])
            loads(0)
            ntri = self.nmaskU if d == 0 else self.nmaskL
            E4 = mk("E4", [128, 4, 128], BF16)
            mT4 = mk("mT4", [128, 4, 128], BF16)
            bc64 = lambda ap: ap.unsqueeze(2).to_broadcast([128, 32, 64])
            v64 = lambda ap: ap.rearrange("p (h q) -> p h q", q=64)
            v4 = lambda ap: ap.rearrange("p (r q) -> p r q", q=64)
            items = [(ci, c, g) for ci, c in enumerate(order) for g in range(8)]

            def pre(ci):
                c = order[ci]
                b = ci % 2
                cx.op("pool", lambda g: g.tensor_tensor(out=v64(xdt[b][:, :]), in0=v64(xt[b][:, :]),
                                                        in1=bc64(dt_t[:, c, :]), op=ALU.mult),
                      rd=[xt[b], dt_t], wr=[xdt[b]])
                cx.op("dve", lambda v: v.tensor_tensor(out=v64(xw[b][:, :]), in0=v64(xdt[b][:, :]),
                                                       in1=bc64(w2_t[:, c, :]), op=ALU.mult),
                      rd=[xdt[b], w2_t], wr=[xw[b]])

            def stage1(it):
                ci, c, g = items[it]
                b = ci % 2
                cb = self.ps[it % 2]
                ep = self.ps[2 + it % 2]
                E, m_ = E4[it % 2], mT4[it % 2]
                cx.op("pe", lambda p: p.matmul(cb[:, 0:128], lhsT=BTc[b][:, g, :], rhs=CTc[b][:, g, :],
                                               start=True, stop=True), rd=[BTc[b], CTc[b]], wr=[cb])
                for r in range(4):
                    hd = g * 4 + r
                    lac = la_t[:, c, hd:hd + 1].to_broadcast([128, 128])
                    cx.op("pe", lambda p, r=r, lac=lac: p.matmul(
                        ep[:, r * 128:(r + 1) * 128], lhsT=lac, rhs=tri[:, :], start=True, stop=False),
                        rd=[la_t, tri], wr=[ep])
                    cx.op("pe", lambda p, r=r: p.matmul(
                        ep[:, r * 128:(r + 1) * 128], lhsT=self.identb[:, :], rhs=neg[:, :], start=False, stop=True),
                        rd=[self.identb, neg], wr=[ep])
                for r in range(4):
                    hd = g * 4 + r
                    cx.op("act", lambda a, r=r, hd=hd: a.activation(
                        out=E[:, r, :], in_=ep[:, r * 128:(r + 1) * 128], func=AF.Exp,
                        bias=ncs_t[:, c, hd:hd + 1], scale=1.0), rd=[ep, ncs_t], wr=[E])
                cx.op("dve", lambda v: v.tensor_tensor(
                    out=m_[:, :, :], in0=cb[:, 0:128].unsqueeze(1).to_broadcast([128, 4, 128]), in1=E[:, :, :],
                    op=ALU.mult), rd=[cb, E], wr=[m_])

            def stage2(it):
                ci, c, g = items[it]
                b = ci % 2
                m_ = mT4[it % 2]
                Y = self.ps[4 + it % 2]
                Z, U = self.ps[6], self.ps[7]
                zc = (it % 2) * 256
                for r in range(4):
                    hd = g * 4 + r
                    cx.op("pe", lambda p, hd=hd, r=r: p.matmul(
                        Y[:, r * 64:(r + 1) * 64], lhsT=m_[:, r, :], rhs=xdt[b][:, hd * 64:(hd + 1) * 64],
                        start=True, stop=True), rd=[m_, xdt[b]], wr=[Y])
                cx.op("pe", lambda p: p.matmul(Z[:, zc:zc + 256], lhsT=CTc[b][:, g, :], rhs=hsb[:, g, :],
                                               start=True, stop=True), rd=[CTc[b], hsb], wr=[Z])
                cx.op("pe", lambda p: p.matmul(
                    U[:, zc:zc + 256], lhsT=Bt[b][:, g * 128:(g + 1) * 128], rhs=xw[b][:, g * 256:(g + 1) * 256],
                    start=True, stop=True), rd=[Bt[b], xw[b]], wr=[U])
                t_ = tz[it % 2]
                cx.op("dve", lambda v: v.tensor_tensor(
                    out=v4(t_[:, :]), in0=v4(Z[:, zc:zc + 256]),
                    in1=ecs_t[:, c, g * 4:(g + 1) * 4].unsqueeze(2).to_broadcast([128, 4, 64]), op=ALU.mult),
                    rd=[Z, ecs_t], wr=[t_])
                cx.op("dve", lambda v: v.tensor_tensor(
                    out=yb[b][:, g * 256:(g + 1) * 256], in0=Y[:, 0:256], in1=t_[:, :], op=ALU.add),
                    rd=[Y, t_], wr=[yb[b]])
                cx.op("pool", lambda gp: gp.tensor_tensor(
                    out=v4(hs[:, g, :]), in0=v4(hs[:, g, :]),
                    in1=dec_t[:, c, g * 4:(g + 1) * 4].unsqueeze(2).to_broadcast([128, 4, 64]), op=ALU.mult),
                    rd=[hs, dec_t], wr=[hs])
                cx.op("dve", lambda v: v.tensor_tensor(
                    out=hs[:, g, :], in0=U[:, zc:zc + 256], in1=hs[:, g, :], op=ALU.add), rd=[U, hs], wr=[hs])
                cx.op("act", lambda a: a.activation(out=hsb[:, g, :], in_=hs[:, g, :], func=AF.Copy),
                      rd=[hs], wr=[hsb])

            pre(0)
            stage1(0)
            for it, (ci, c, g) in enumerate(items):
                b = ci % 2
                if g == 0 and ci + 1 < len(order):
                    loads(ci + 1)
                if g == 4 and ci + 1 < len(order):
                    pre(ci + 1)
                if it + 1 < len(items):
                    stage1(it + 1)
                stage2(it)
                if g != 7:
                    continue
                if d == 0:
                    cx.dma("sp", rows(self.hfwd, c), yb[b][:, :], rd=[yb[b]], owner=yb[b])
                    continue
                if not (need_ctx or c < nlat):
                    continue
                cx.op("pool", lambda g: g.tensor_tensor(out=yb[b][:, :], in0=yb[b][:, :], in1=yf[b][:, :], op=ALU.add),
                      rd=[yb[b], yf[b]], wr=[yb[b]])
                cx.op("pool", lambda g: g.tensor_tensor(out=v64(yg[:, :]), in0=v64(xt[b][:, :]),
                                                        in1=bc64(sm[:, 128:160]), op=ALU.mult),
                      rd=[xt[b], sm], wr=[yg])
                cx.op("pool", lambda g: g.tensor_tensor(out=yg[:, :], in0=yg[:, :], in1=yb[b][:, :], op=ALU.add),
                      rd=[yg, yb[b]], wr=[yg])
                cx.op("dve", lambda v: v.tensor_tensor(out=yg[:, :], in0=yg[:, :], in1=szc[b][:, :], op=ALU.mult),
                      rd=[yg, szc[b]], wr=[yg])
                cx.op("act", lambda a: a.activation(out=junk[:, :], in_=yg[:, :], func=AF.Square, accum_out=ss[:, 0:1]),
                      rd=[yg], wr=[junk, ss])
                cx.op("act", lambda a: a.activation(out=ss[:, :], in_=ss[:, :], func=AF.Sqrt, bias=self.eps_col[:, 0:1],
                                                    scale=1.0 / 2048), rd=[ss, self.eps_col], wr=[ss])
                cx.op("dve", lambda v: v.reciprocal(out=ss[:, :], in_=ss[:, :]), rd=[ss], wr=[ss])
                cx.op("dve", lambda v: v.scalar_tensor_tensor(
                    out=yn[:, :], in0=yg[:, :], scalar=ss[:, 0:1], in1=ngb[:, :], op0=ALU.mult, op1=ALU.mult),
                    rd=[yg, ss, ngb], wr=[yn])
                for r in range(2):
                    tp = self.ps[r]
                    tpv = tp[:, :].bitcast(BF16)
                    for q in range(8):
                        cc = r * 8 + q
                        cx.op("pe", lambda p, cc=cc, q=q, tpv=tpv: p.transpose(
                            tpv[:, q * 128:(q + 1) * 128], yn[:, cc * 128:(cc + 1) * 128], self.identb[:, :]),
                            rd=[yn, self.identb], wr=[tp])
                    cx.op("act", lambda a, r=r, tpv=tpv: a.activation(
                        out=yTo[b][:, r * 8:(r + 1) * 8, :], in_=tpv.rearrange("p (a b) -> p a b", b=128),
                        func=AF.Copy), rd=[tp], wr=[yTo[b]])
                cx.dma("sp", self.yT.ap().rearrange("(cc p) t -> p cc t", p=128)[:, :, c * 128:(c + 1) * 128],
                       yTo[b][:, :, :], rd=[yTo[b]], owner=yTo[b])


def prep_core_inputs(inp, b):
    f = np.float32
    xin = np.ascontiguousarray(np.concatenate([inp["x"][b].T, inp["ctx"][b].T], axis=1), dtype=f)
    cT = np.stack([inp["c"][b].reshape(KC, 128).T, inp["c_ctx"].reshape(KC, 128).T], axis=-1)
    return {"xin": xin, "cT": np.ascontiguousarray(cT, dtype=f)}


def prep_shared_inputs(inp):
    f = np.float32
    depth = inp["ada_w"].shape[0]
    sh = {}
    sh["ada_w"] = np.ascontiguousarray(inp["ada_w"], dtype=f)
    sh["ada_bT"] = np.ascontiguousarray(inp["ada_b"].reshape(depth, 72, 128).transpose(0, 2, 1), dtype=f)
    sh["gT"] = np.ascontiguousarray(inp["norm_g"].reshape(depth, 6, KC, 128).transpose(0, 3, 1, 2), dtype=f)
    sh["w_gate"] = np.ascontiguousarray(inp["ffn_w_gate"], dtype=f)
    sh["w_up"] = np.ascontiguousarray(inp["ffn_w_up"], dtype=f)
    sh["w_down"] = np.ascontiguousarray(inp["ffn_w_down"], dtype=f)
    sh["ml_w_in"] = np.ascontiguousarray(inp["mlstm_w_in"][0], dtype=f)
    cw = np.concatenate([inp["mlstm_conv_w"][0], inp["mlstm_conv_b"][0][None]], 0)
    sh["ml_conv"] = np.ascontiguousarray(cw.reshape(6, 16, 128).transpose(2, 1, 0), dtype=f)
    bd = np.zeros((3, 16, 128, 128), f)
    for i, nm in enumerate(("mlstm_w_q", "mlstm_w_k", "mlstm_w_v")):
        w = inp[nm][0].reshape(16, 32, 4, 4)
        for n in range(32):
            bd[i, :, n * 4:(n + 1) * 4, n * 4:(n + 1) * 4] = w[:, n]
    sh["ml_bd"] = np.ascontiguousarray(bd.transpose(0, 2, 1, 3))
    perm = np.array([d * 8 + io * 4 + h for io in range(2) for d in range(2) for h in range(4)])
    wg = inp["mlstm_w_gates"][0][:, perm]
    sh["ml_wg"] = np.ascontiguousarray(wg.reshape(48, 128, 16).transpose(1, 0, 2), dtype=f)
    sh["ml_bg"] = np.ascontiguousarray(inp["mlstm_b_gates"][0][perm].reshape(16, 1), dtype=f)
    ns = np.stack([inp["mlstm_norm_g"][0], inp["mlstm_skip"][0]], 0)
    sh["ml_ns"] = np.ascontiguousarray(ns.reshape(2, 16, 128).transpose(2, 0, 1), dtype=f)
    sh["ml_w_out"] = np.ascontiguousarray(inp["mlstm_w_out"][0], dtype=f)
    sh["sd_w_in"] = np.ascontiguousarray(inp["ssd_w_in"][0], dtype=f)
    cw = np.concatenate([inp["ssd_conv_w"][0], inp["ssd_conv_b"][0][None]], 0)
    sh["sd_conv"] = np.ascontiguousarray(cw.reshape(6, 32, 128).transpose(2, 1, 0), dtype=f)
    small = np.concatenate([inp["ssd_dt_bias"][0].reshape(64), inp["ssd_a_log"][0].reshape(64), inp["ssd_d"][0]])
    sh["sd_smallT"] = np.ascontiguousarray(np.broadcast_to(small[None], (128, 160)), dtype=f)
    sh["sd_ng"] = np.ascontiguousarray(np.broadcast_to(inp["ssd_norm_g"][0][None], (128, 2048)), dtype=f)
    sh["sd_w_out"] = np.ascontiguousarray(inp["ssd_w_out"][0], dtype=f)
    return sh


def kernel(**inp):
    inp = {k: np.asarray(v) for k, v in inp.items()}
    B = inp["x"].shape[0]
    cx = Ctx()
    net = Net(cx)
    net.build()
    sh = prep_shared_inputs(inp)
    in_maps = []
    for b in range(B):
        m = dict(sh)
        m.update(prep_core_inputs(inp, b))
        in_maps.append(m)
    res = run_bass_kernel_spmd(cx.nc, in_maps, core_ids=list(range(B)))
    out = np.stack([np.ascontiguousarray(r["out"].T) for r in res.results], axis=0)
    return out.astype(np.float32)
```

```python
from contextlib import ExitStack
import numpy as np
import concourse.bass as bass
import concourse.mybir as mybir
from concourse.bass_utils import run_bass_kernel_spmd

F32 = mybir.dt.float32
BF16 = mybir.dt.bfloat16
ALU = mybir.AluOpType
AF = mybir.ActivationFunctionType
AX = mybir.AxisListType

D = 1024
KC = D // 128
DFF = 2816
FC = DFF // 128
EPS = 1e-6
NCORES = 8


class Buf:
    __slots__ = ("name", "w", "r", "dram", "dsem", "psum")

    def __init__(self, name, dram=False, psum=False):
        self.name = name
        self.w = {}
        self.r = {}
        self.dram = dram
        self.dsem = None
        self.psum = psum


class Tile:
    def __init__(self, h, buf):
        self.h = h
        self.b = buf

    def __getitem__(self, k):
        return self.h[k]


class Ctx:
    ENG = ("pe", "dve", "act", "pool", "sp")

    def __init__(self):
        nc = self.nc = bass.Bass("TRN2", target_bir_lowering=False)
        self.engs = dict(pe=nc.tensor, dve=nc.vector, act=nc.scalar, pool=nc.gpsimd, sp=nc.sync)
        self.semh = {}
        self.owner = {}
        self.cur = {}
        self.cnt = {}
        self.dcnt = {}
        self.seen = {e: {} for e in self.ENG}
        self.gen = 0
        for e in self.ENG:
            self._newsem(e)
        self.n_ins = 0
        self.n_wait = 0
        self.free_dsems = []

    def _newsem(self, e):
        self.gen += 1
        name = f"s_{e}_{self.gen}"
        self.semh[name] = self.nc.alloc_semaphore(name)
        self.owner[name] = e
        self.cur[e] = name
        self.cnt[e] = 0

    def sb(self, es, name, shape, dt):
        self.uid = getattr(self, "uid", 0) + 1
        name = f"{name}_{self.uid}"
        h = es.enter_context(self.nc.sbuf_tensor(name, list(shape), dt))
        t = Tile(h, Buf(name))
        es.callback(self._release, t.b)
        return t

    def _release(self, buf):
        if buf.dsem is not None:
            self.free_dsems.append(buf.dsem)
            buf.dsem = None

    def psum(self, es, name, shape, dt=F32):
        h = es.enter_context(self.nc.psum_tensor(name, list(shape), dt))
        return Tile(h, Buf(name, psum=True))

    def dram(self, name, shape, dt, kind="Internal"):
        h = self.nc.dram_tensor(name, list(shape), dt, kind=kind)
        return h

    def _val(self, name, val):
        return 16 * self.dcnt[name] if val is None else val

    def _gather(self, e, rd, wr):
        toks = {}

        def add(name, val, skip_same):
            if skip_same and self.owner[name] == e:
                return
            v = self._val(name, val)
            if toks.get(name, 0) < v:
                toks[name] = v

        pe = e == "pe"
        for b in rd:
            for n, v in b.w.items():
                add(n, v, pe)
            if b.psum:
                for n, v in b.r.items():
                    if self.owner[n] != e:
                        add(n, v, False)
        for b in wr:
            for n, v in b.w.items():
                add(n, v, pe)
            for n, v in b.r.items():
                add(n, v, pe)
        return toks

    def _wait(self, e, toks):
        for name, v in toks.items():
            if self.seen[e].get(name, 0) < v:
                self.engs[e].wait_ge(self.semh[name], v)
                self.seen[e][name] = v
                self.n_wait += 1

    def op(self, e, fn, rd=(), wr=()):
        rd = [t.b if isinstance(t, Tile) else t for t in rd]
        wr = [t.b if isinstance(t, Tile) else t for t in wr]
        self._wait(e, self._gather(e, rd, wr))
        ins = fn(self.engs[e])
        name = self.cur[e]
        self.cnt[e] += 1
        ins.then_inc(self.semh[name], 1)
        v = self.cnt[e]
        for b in rd:
            b.r[name] = v
        for b in wr:
            b.w = {name: v}
            b.r = {}
        if v >= 30000:
            self._newsem(e)
        self.n_ins += 1
        return ins

    def dma(self, q, out, in_, rd=(), wr=(), owner=None):
        rd = [t.b if isinstance(t, Tile) else t for t in rd]
        wr = [t.b if isinstance(t, Tile) else t for t in wr]
        owner = owner.b if isinstance(owner, Tile) else owner
        self._wait(q, self._gather("dma", rd, wr))
        ins = self.engs[q].dma_start(out=out, in_=in_)
        if owner.dsem is None:
            if self.free_dsems:
                name = self.free_dsems.pop()
            else:
                name = f"d_{owner.name}"
                self.semh[name] = self.nc.alloc_semaphore(name)
                self.owner[name] = None
                self.dcnt[name] = 0
            owner.dsem = name
        name = owner.dsem
        self.dcnt[name] += 1
        ins.then_inc(self.semh[name], 16)
        for b in rd:
            b.r[name] = None
        for b in wr:
            if b.dram:
                b.w[name] = None
            else:
                b.w = {name: None}
                b.r = {}
        self.n_ins += 1
        return ins

    def barrier(self):
        toks = {}
        for e in self.ENG:
            if self.cnt[e] > 0:
                toks[self.cur[e]] = self.cnt[e]
        for n, c in self.dcnt.items():
            if c > 0:
                toks[n] = 16 * c
        for e in self.ENG:
            t = {n: v for n, v in toks.items() if self.owner[n] != e}
            self._wait(e, t)


def load_weight_bf16(cx, q, Wd_ap, Wbf, stg, col0, ncol, kc):
    src = Wd_ap.rearrange("(kc p) m -> p kc m", p=128)[:, :, col0:col0 + ncol]
    cx.dma(q, stg[:, 0:kc, 0:ncol], src, wr=[stg], owner=stg)
    cx.op("pool", lambda g: g.tensor_copy(out=Wbf[:, :, col0:col0 + ncol], in_=stg[:, 0:kc, 0:ncol]),
          rd=[stg], wr=[Wbf.b if isinstance(Wbf, Tile) else Wbf])


class Net:
    def __init__(self, cx, T_lat=4096, T_ctx=256, depth=2, tile_n=512, debug_stop=None):
        self.cx = cx
        self.nc = cx.nc
        self.TL, self.TC = T_lat, T_ctx
        self.T = T_lat + T_ctx
        self.depth = depth
        self.debug_stop = debug_stop
        self.tiles = [(s, tile_n, 0) for s in range(0, T_lat, tile_n)] + [(T_lat, T_ctx, 1)]
        self.declare_io()

    def declare_io(self):
        nc, T = self.nc, self.T
        di = lambda n, s: nc.dram_tensor(n, list(s), F32, kind="ExternalInput")
        self.xin = di("xin", [D, T])
        self.cT = di("cT", [128, KC, 2])
        self.ada_w = di("ada_w", [self.depth, D, 9 * D])
        self.ada_bT = di("ada_bT", [self.depth, 128, 72])
        self.gT = di("gT", [self.depth, 128, 6, KC])
        self.w_gate = di("w_gate", [self.depth, 2, D, DFF])
        self.w_up = di("w_up", [self.depth, 2, D, DFF])
        self.w_down = di("w_down", [self.depth, 2, DFF, D])
        self.ml_w_in = di("ml_w_in", [D, 4096])
        self.ml_conv = di("ml_conv", [128, 16, 6])
        self.ml_bd = di("ml_bd", [3, 128, 16, 128])
        self.ml_wg = di("ml_wg", [128, 48, 16])
        self.ml_bg = di("ml_bg", [16, 1])
        self.ml_ns = di("ml_ns", [128, 2, 16])
        self.ml_w_out = di("ml_w_out", [2048, D])
        self.sd_w_in = di("sd_w_in", [D, 6208])
        self.sd_conv = di("sd_conv", [128, 32, 6])
        self.sd_smallT = di("sd_smallT", [128, 160])
        self.sd_ng = di("sd_ng", [128, 2048])
        self.sd_w_out = di("sd_w_out", [2048, D])
        self.out = nc.dram_tensor("out", [D, self.TL], F32, kind="ExternalOutput")
        NCH = T // 128
        self.NCH = NCH
        dm = lambda n, s, dt: nc.dram_tensor(n, list(s), dt, kind="ExternalOutput" if self.debug_stop else "Internal")
        self.szT = dm("szT", [2048, T], BF16)
        self.xcsT = dm("xcsT", [2048, T], BF16)
        self.qT = dm("qT", [2048, T], BF16)
        self.kT = dm("kT", [2048, T], BF16)
        self.ktm = dm("ktm", [T, 2048], BF16)
        self.vtm = dm("vtm", [T, 2048], BF16)
        self.hfwd = dm("hfwd", [T, 2048], F32)
        self.sztm = dm("sztm", [T, 2048], BF16)
        self.xtm = dm("xtm", [T, 2048], BF16)
        self.Btm = dm("Btm", [T, 1024], BF16)
        self.BT = dm("BT", [1024, T], BF16)
        self.CT = dm("CT", [1024, T], BF16)
        self.yT = nc.dram_tensor("yT", [2048, T], BF16,
                                 kind="ExternalOutput" if self.debug_stop else "Internal")
        self.yT_b = [Buf(f"yT{i}", dram=True) for i in range(len(self.tiles))]
        self.xres = nc.dram_tensor("xres", [D, T], F32,
                                   kind="ExternalOutput" if self.debug_stop else "Internal")
        self.uT = nc.dram_tensor("uT", [DFF, T], BF16, kind="Internal")
        nt = len(self.tiles)
        self.xres_b = [Buf(f"xres{i}", dram=True) for i in range(nt)]
        self.xin_b = [Buf(f"xin{i}", dram=True) for i in range(nt)]
        self.out_b = [Buf(f"out{i}", dram=True) for i in range(nt)]
        self.uT_b = [Buf(f"uT{i}", dram=True) for i in range(nt)]

    def build(self):
        cx = self.cx
        with ExitStack() as es:
            self.es0 = es
            self.ps = [cx.psum(es, f"ps{i}", [128, 512]) for i in range(8)]
            self.ones_bf = cx.sb(es, "ones_bf", [128, 128], BF16)
            cx.op("pool", lambda g: g.memset(self.ones_bf[:], 1.0 / D), wr=[self.ones_bf])
            self.eps_col = cx.sb(es, "eps_col", [128, 1], F32)
            cx.op("pool", lambda g: g.memset(self.eps_col[:], EPS), wr=[self.eps_col])
            self.one_col = cx.sb(es, "one_col", [128, 1], F32)
            cx.op("pool", lambda g: g.memset(self.one_col[:], 1.0), wr=[self.one_col])
            self.ones_f = cx.sb(es, "ones_f", [128, 128], F32)
            cx.op("pool", lambda g: g.memset(self.ones_f[:], 1.0), wr=[self.ones_f])
            self.ident = cx.sb(es, "ident", [128, 128], F32)
            self.maskU = cx.sb(es, "maskU", [128, 128], F32)
            self.maskL = cx.sb(es, "maskL", [128, 128], F32)
            self.identb = cx.sb(es, "identb", [128, 128], BF16)
            for t_, pat, cm, cmp_ in ((self.ident, [[1, 128]], -1, ALU.is_equal), (self.maskU, [[1, 128]], -1, ALU.is_ge),
                                      (self.maskL, [[-1, 128]], 1, ALU.is_ge)):
                cx.op("pool", lambda g, t_=t_, pat=pat, cm=cm, cmp_=cmp_: g.affine_select(
                    out=t_[:], in_=self.ones_f[:], pattern=pat, compare_op=cmp_, fill=0.0, base=0,
                    channel_multiplier=cm), rd=[self.ones_f], wr=[t_])
            cx.op("pool", lambda g: g.tensor_copy(out=self.identb[:], in_=self.ident[:]), rd=[self.ident],
                  wr=[self.identb])
            self.nmaskU = cx.sb(es, "nmaskU", [128, 128], F32)
            self.nmaskL = cx.sb(es, "nmaskL", [128, 128], F32)
            for nm_, mk_ in ((self.nmaskU, self.maskU), (self.nmaskL, self.maskL)):
                cx.op("pool", lambda g, nm_=nm_, mk_=mk_: g.tensor_scalar(
                    out=nm_[:], in0=mk_[:], scalar1=-1.0, scalar2=None, op0=ALU.mult), rd=[mk_], wr=[nm_])
            self.negU = cx.sb(es, "negU", [128, 128], BF16)
            self.negL = cx.sb(es, "negL", [128, 128], BF16)
            for ng_, mk_ in ((self.negU, self.maskU), (self.negL, self.maskL)):
                cx.op("pool", lambda g, ng_=ng_, mk_=mk_: g.tensor_scalar(
                    out=ng_[:], in0=mk_[:], scalar1=-1.0, scalar2=30000.0, op0=ALU.add, op1=ALU.mult),
                    rd=[mk_], wr=[ng_])
            self.mod = cx.sb(es, "mod", [128, 72, 2], F32)
            self.par = cx.sb(es, "par", [128, 9, KC, 2], F32)
            self.sc2 = cx.sb(es, "sc2", [128, KC, 2], F32)
            self.gsb = cx.sb(es, "gsb", [128, 6, KC], F32)
            self.adab = cx.sb(es, "adab", [128, 72], F32)
            cx.dma("sp", self.sc2[:], self.cT[:], wr=[self.sc2], owner=self.sc2)
            cx.op("act", lambda a: a.activation(out=self.sc2[:], in_=self.sc2[:], func=AF.Silu),
                  rd=[self.sc2], wr=[self.sc2])
            for li in range(self.depth):
                if self.layer(li):
                    break
            cx.barrier()

    def layer(self, li):
        last = li == self.depth - 1
        ds = self.debug_stop
        self.adaln(li)
        src = (self.xin, self.xin_b) if li == 0 else (self.xres, self.xres_b)
        self.ffn(li, 0, 0, src, (self.xres, self.xres_b), ctx_needed=True)
        if ds == (li, "ffn1"):
            return True
        if li % 2 == 0:
            self.mlstm(li, not last)
        else:
            self.ssd(li, not last)
        if ds and ds[0] == li and ds[1] in ("ml_in", "mixer", "ssd_tab", "ssd_d0"):
            return True
        dst = (self.out, self.out_b) if (last and not ds) else (self.xres, self.xres_b)
        self.ffn(li, 1, 2, (self.xres, self.xres_b), dst, ctx_needed=not last)
        return ds == (li, "layer")

    def adaln(self, li):
        cx = self.cx
        cx.barrier()
        with ExitStack() as es:
            NG = 1152
            stg = [cx.sb(es, f"adastg{i}", [128, KC, NG], F32) for i in range(2)]
            cx.dma("sp", self.adab[:], self.ada_bT[li], wr=[self.adab], owner=self.adab)
            cx.dma("sp", self.gsb[:], self.gT[li], wr=[self.gsb], owner=self.gsb)
            mps = self.ps[0]
            for gi in range(9 * D // NG):
                st = stg[gi % 2]
                src = self.ada_w[li].rearrange("(kc p) m -> p kc m", p=128)[:, :, gi * NG:(gi + 1) * NG]
                cx.dma("sp", st[:], src, wr=[st], owner=st)
                for j in range(NG // 128):
                    mc = gi * (NG // 128) + j
                    for k in range(KC):
                        cx.op("pe", lambda p, k=k, j=j, mc=mc, st=st: p.matmul(
                            mps[:, 2 * mc:2 * mc + 2], lhsT=st[:, k, j * 128:(j + 1) * 128],
                            rhs=self.sc2[:, k, :], start=(k == 0), stop=(k == KC - 1)),
                            rd=[st, self.sc2], wr=[mps])
            cx.op("dve", lambda v: v.tensor_tensor(
                out=self.mod[:], in0=mps[:, 0:144].rearrange("p (m c) -> p m c", c=2),
                in1=self.adab[:].unsqueeze(2).to_broadcast([128, 72, 2]), op=ALU.add),
                rd=[mps, self.adab], wr=[self.mod])
            md = lambda j: self.mod[:, j * KC:(j + 1) * KC, :]
            gb = lambda j: self.gsb[:, j, :].unsqueeze(2).to_broadcast([128, KC, 2])
            P = self.par
            spec = [
                (0, "A", 1, 0, 1.0), (1, "B", 0, None, 1.0), (2, "G", 2, 1, 0.5),
                (3, "A", 4, 2, 1.0), (4, "B", 3, None, 1.0), (5, "G", 5, 3, 1.0),
                (6, "A", 7, 4, 1.0), (7, "B", 6, None, 1.0), (8, "G", 8, 5, 0.5)]
            for idx, kind, mj, gj, fac in spec:
                if kind == "A":
                    cx.op("dve", lambda v, idx=idx, mj=mj, gj=gj: v.scalar_tensor_tensor(
                        out=P[:, idx], in0=md(mj), scalar=1.0, in1=gb(gj), op0=ALU.add, op1=ALU.mult),
                        rd=[self.mod, self.gsb], wr=[P])
                elif kind == "B":
                    cx.op("dve", lambda v, idx=idx, mj=mj: v.tensor_copy(out=P[:, idx], in_=md(mj)),
                          rd=[self.mod], wr=[P])
                else:
                    cx.op("dve", lambda v, idx=idx, mj=mj, gj=gj, fac=fac: v.scalar_tensor_tensor(
                        out=P[:, idx], in0=md(mj), scalar=fac, in1=gb(gj), op0=ALU.mult, op1=ALU.mult),
                        rd=[self.mod, self.gsb], wr=[P])

    def rsqrt_eps(self, rstd, ms_ps, n):
        cx = self.cx
        cx.op("act", lambda a: a.activation(out=rstd[:, 0:n], in_=ms_ps[:, 0:n], func=AF.Sqrt,
                                            bias=self.eps_col[:, 0:1], scale=1.0), rd=[ms_ps, self.eps_col],
              wr=[rstd])
        cx.op("dve", lambda v: v.reciprocal(out=rstd[:, 0:n], in_=rstd[:, 0:n]), rd=[rstd], wr=[rstd])

    def norm_mod(self, es_bufs, x, sq, hT, n, c, pidx, ssq_ps, rstd):
        cx = self.cx
        cx.op("act", lambda a: a.activation(out=sq[:, :, 0:n], in_=x[:, :, 0:n], func=AF.Square),
              rd=[x], wr=[sq])
        for k in range(KC):
            cx.op("pe", lambda p, k=k: p.matmul(ssq_ps[:, 0:n], lhsT=self.ones_bf[:], rhs=sq[:, k, 0:n],
                                                start=(k == 0), stop=(k == KC - 1)),
                  rd=[self.ones_bf, sq], wr=[ssq_ps])
        self.rsqrt_eps(rstd, ssq_ps, n)
        cx.op("dve", lambda v: v.tensor_tensor(
            out=x[:, :, 0:n], in0=x[:, :, 0:n],
            in1=rstd[:, 0:n].unsqueeze(1).to_broadcast([128, KC, n]), op=ALU.mult), rd=[x, rstd], wr=[x])
        for k in range(KC):
            cx.op("act", lambda a, k=k: a.activation(
                out=hT[:, k, 0:n], in_=x[:, k, 0:n], func=AF.Identity,
                scale=self.par[:, pidx, k, c:c + 1], bias=self.par[:, pidx + 1, k, c:c + 1]),
                rd=[x, self.par], wr=[hT])

    def ffn(self, li, half, pbase, src, dst, ctx_needed):
        cx = self.cx
        tiles = self.tiles if ctx_needed else self.tiles[:-1]
        pidx = pbase * 3
        srcT, srcB = src
        dstT, dstB = dst
        xv = lambda t: t.ap().rearrange("(kc p) t -> p kc t", p=128)
        cx.barrier()
        with ExitStack() as es:
            Wg = cx.sb(es, "Wg", [128, KC, DFF], BF16)
            Wu = cx.sb(es, "Wu", [128, KC, DFF], BF16)
            NGC = 352
            ngrp = DFF // NGC
            Wgb = [Buf(f"Wg{i}") for i in range(ngrp)]
            Wub = [Buf(f"Wu{i}") for i in range(ngrp)]
            stg = [cx.sb(es, f"wstg{i}", [128, KC, NGC], F32) for i in range(2)]
            xt = [cx.sb(es, f"xt{i}", [128, KC, 512], F32) for i in range(2)]
            hT = [cx.sb(es, f"hT{i}", [128, KC, 512], BF16) for i in range(2)]
            sq = cx.sb(es, "sq", [128, KC, 512], BF16)
            rstd = cx.sb(es, "rstd", [128, 512], F32)
            sg = [cx.sb(es, f"sg{i}", [128, 512], F32) for i in range(2)]
            uo = [cx.sb(es, f"uo{i}", [128, FC // 2, 512], BF16) for i in range(2)]
            ssq_ps = self.ps[0]
            gps = [self.ps[1], self.ps[2]]
            ups = [self.ps[3], self.ps[4]]
            def load_x(i):
                s, n, c = tiles[i]
                cx.dma("sp", xt[i % 2][:, :, 0:n], xv(srcT)[:, :, s:s + n], rd=[srcB[i]], wr=[xt[i % 2]],
                       owner=xt[i % 2])
            load_x(0)
            si = 0
            for g in range(ngrp):
                for (Wdram, Wsb, Wb) in ((self.w_gate, Wg, Wgb), (self.w_up, Wu, Wub)):
                    st = stg[si % 2]
                    si += 1
                    srcw = Wdram[li, half].rearrange("(kc p) m -> p kc m", p=128)[:, :, g * NGC:(g + 1) * NGC]
                    cx.dma("sp", st[:], srcw, wr=[st], owner=st)
                    cx.op("pool", lambda gp, Wsb=Wsb, st=st, g=g: gp.tensor_copy(
                        out=Wsb[:, :, g * NGC:(g + 1) * NGC], in_=st[:]), rd=[st], wr=[Wb[g]])
            def prep(i):
                s_, n_, c_ = tiles[i]
                self.norm_mod(None, xt[i % 2], sq, hT[i % 2], n_, c_, pidx, ssq_ps, rstd)
                if i + 2 < len(tiles):
                    load_x(i + 2)
            if len(tiles) > 1:
                load_x(1)
            prep(0)
            for i, (s, n, c) in enumerate(tiles):
                h = hT[i % 2]
                for f in range(FC):
                    if f == FC // 2 and i + 1 < len(tiles):
                        prep(i + 1)
                    gp_, up_ = gps[f % 2], ups[f % 2]
                    wb = [Wgb[(f * 128) // NGC], Wgb[(f * 128 + 127) // NGC]]
                    wb2 = [Wub[(f * 128) // NGC], Wub[(f * 128 + 127) // NGC]]
                    for k in range(KC):
                        cx.op("pe", lambda p, k=k, f=f, gp_=gp_: p.matmul(
                            gp_[:, 0:n], lhsT=Wg[:, k, f * 128:(f + 1) * 128], rhs=h[:, k, 0:n],
                            start=(k == 0), stop=(k == KC - 1)), rd=[h] + wb, wr=[gp_])
                    for k in range(KC):
                        cx.op("pe", lambda p, k=k, f=f, up_=up_: p.matmul(
                            up_[:, 0:n], lhsT=Wu[:, k, f * 128:(f + 1) * 128], rhs=h[:, k, 0:n],
                            start=(k == 0), stop=(k == KC - 1)), rd=[h] + wb2, wr=[up_])
                    sgt = sg[f % 2]
                    cx.op("act", lambda a, gp_=gp_, sgt=sgt: a.activation(out=sgt[:, 0:n], in_=gp_[:, 0:n],
                                                                          func=AF.Silu), rd=[gp_], wr=[sgt])
                    uh = uo[f // (FC // 2)]
                    fo = f % (FC // 2)
                    cx.op("dve", lambda v, up_=up_, sgt=sgt, uh=uh, fo=fo: v.tensor_tensor(
                        out=uh[:, fo, 0:n], in0=up_[:, 0:n], in1=sgt[:, 0:n], op=ALU.mult),
                        rd=[up_, sgt], wr=[uh])
                    if fo == FC // 2 - 1:
                        hh = f // (FC // 2)
                        dv = self.uT.ap().rearrange("(fc p) t -> p fc t", p=128)[
                            :, hh * (FC // 2):(hh + 1) * (FC // 2), s:s + n]
                        cx.dma("sp", dv, uh[:, :, 0:n], rd=[uh], wr=[self.uT_b[i]], owner=uh)
        self.proj_res(self.w_down[li, half], FC, self.uT, self.uT_b, pidx + 2, src, dst, tiles)

    def proj_res(self, W_ap, kc, inT, inB, gidx, src, dst, tiles):
        cx = self.cx
        srcT, srcB = src
        dstT, dstB = dst
        xv = lambda t: t.ap().rearrange("(kc p) t -> p kc t", p=128)
        cx.barrier()
        with ExitStack() as es:
            Wd = cx.sb(es, "Wd", [128, kc, D], BF16)
            NGC = 256
            ngrp = D // NGC
            Wdb = [Buf(f"Wd{i}") for i in range(ngrp)]
            stg = [cx.sb(es, f"wstg{i}", [128, kc, NGC], F32) for i in range(2)]
            ut = [cx.sb(es, f"ut{i}", [128, kc, 512], BF16) for i in range(2)]
            xt = [cx.sb(es, f"xt{i}", [128, KC, 512], F32) for i in range(2)]
            ys = [cx.sb(es, f"y{i}", [128, KC, 512], F32) for i in range(2 if kc <= 16 else 1)]
            sq = cx.sb(es, "sq", [128, KC, 512], BF16)
            rstd = cx.sb(es, "rstd", [128, 512], F32)
            ssq_ps = self.ps[0]
            yps = [self.ps[1], self.ps[2], self.ps[3], self.ps[4]]

            def load_t(i):
                s, n, c = tiles[i]
                uv = inT.ap().rearrange("(fc p) t -> p fc t", p=128)[:, :, s:s + n]
                cx.dma("sp", ut[i % 2][:, :, 0:n], uv, rd=[inB[i]], wr=[ut[i % 2]], owner=ut[i % 2])
                cx.dma("sp", xt[i % 2][:, :, 0:n], xv(srcT)[:, :, s:s + n], rd=[srcB[i]], wr=[xt[i % 2]],
                       owner=xt[i % 2])
            load_t(0)
            for g in range(ngrp):
                st = stg[g % 2]
                srcw = W_ap.rearrange("(fc p) m -> p fc m", p=128)[:, :, g * NGC:(g + 1) * NGC]
                cx.dma("sp", st[:], srcw, wr=[st], owner=st)
                cx.op("pool", lambda gp, st=st, g=g: gp.tensor_copy(
                    out=Wd[:, :, g * NGC:(g + 1) * NGC], in_=st[:]), rd=[st], wr=[Wdb[g]])
            for i, (s, n, c) in enumerate(tiles):
                if i + 1 < len(tiles):
                    load_t(i + 1)
                u, x = ut[i % 2], xt[i % 2]
                y = ys[i % len(ys)]
                for d in range(KC):
                    yp = yps[d % 4]
                    for f in range(kc):
                        cx.op("pe", lambda p, d=d, f=f, yp=yp: p.matmul(
                            yp[:, 0:n], lhsT=Wd[:, f, d * 128:(d + 1) * 128], rhs=u[:, f, 0:n],
                            start=(f == 0), stop=(f == kc - 1)), rd=[u, Wdb[(d * 128) // NGC]], wr=[yp])
                    cx.op("act", lambda a, d=d, yp=yp: a.activation(out=y[:, d, 0:n], in_=yp[:, 0:n],
                                                                    func=AF.Copy), rd=[yp], wr=[y])
                self.post_norm_res(y, sq, rstd, ssq_ps, x, n, c, gidx)
                sdst = dstT.ap().rearrange("(kc p) t -> p kc t", p=128)[:, :, s:s + n]
                cx.dma("sp", sdst, x[:, :, 0:n], rd=[x], wr=[dstB[i]], owner=x)

    def post_norm_res(self, y, sq, rstd, ssq_ps, x, n, c, gidx):
        cx = self.cx
        cx.op("act", lambda a: a.activation(out=sq[:, :, 0:n], in_=y[:, :, 0:n], func=AF.Square),
              rd=[y], wr=[sq])
        for k in range(KC):
            cx.op("pe", lambda p, k=k: p.matmul(ssq_ps[:, 0:n], lhsT=self.ones_bf[:], rhs=sq[:, k, 0:n],
                                                start=(k == 0), stop=(k == KC - 1)),
                  rd=[self.ones_bf, sq], wr=[ssq_ps])
        self.rsqrt_eps(rstd, ssq_ps, n)
        cx.op("dve", lambda v: v.tensor_tensor(
            out=y[:, :, 0:n], in0=y[:, :, 0:n],
            in1=rstd[:, 0:n].unsqueeze(1).to_broadcast([128, KC, n]), op=ALU.mult), rd=[y, rstd], wr=[y])
        for k in range(KC):
            cx.op("dve", lambda v, k=k: v.scalar_tensor_tensor(
                out=x[:, k, 0:n], in0=y[:, k, 0:n], scalar=self.par[:, gidx, k, c:c + 1], in1=x[:, k, 0:n],
                op0=ALU.mult, op1=ALU.add), rd=[y, x, self.par], wr=[x])


    def chunk_order(self, d):
        nl = self.TL // 128
        nc_ = self.TC // 128
        ctx = list(range(nl, nl + nc_))
        lat = list(range(nl))
        return ctx + lat if d == 0 else ctx[::-1] + lat[::-1]

    def mlstm(self, li, need_ctx):
        cx = self.cx
        T, NCH = self.T, self.NCH
        cx.barrier()
        with ExitStack() as es:
            self.gT16 = cx.sb(es, "gT16", [16, T], F32)
            self.ml_ns_sb = cx.sb(es, "ml_ns_sb", [128, 2, 16], F32)
            cx.dma("sp", self.ml_ns_sb[:], self.ml_ns.ap(), wr=[self.ml_ns_sb], owner=self.ml_ns_sb)
            self.Wtm = [cx.sb(es, f"Wtm{d}", [128, NCH, 4], F32) for d in range(2)]
            self.Wtmb = [cx.sb(es, f"Wtmb{d}", [128, NCH, 4], BF16) for d in range(2)]
            self.Ftm = [cx.sb(es, f"Ftm{d}", [128, NCH, 4], F32) for d in range(2)]
            self.decbc = [cx.sb(es, f"decbc{d}", [128, NCH, 4], F32) for d in range(2)]
            self.mlstm_in(li)
            if self.debug_stop == (li, "ml_in"):
                return
            self.mlstm_gates()
            for d in range(2):
                self.mlstm_scan(d)
        tiles = self.tiles if need_ctx else self.tiles[:-1]
        self.proj_res(self.ml_w_out.ap(), 16, self.yT, self.yT_b, 5, (self.xres, self.xres_b),
                      (self.xres, self.xres_b), tiles)

    def mlstm_in(self, li):
        cx = self.cx
        tiles = self.tiles
        cx.barrier()
        with ExitStack() as es:
            Win = cx.sb(es, "Win", [128, KC, 4096], BF16)
            Winb = [Buf(f"Win{g}") for g in range(8)]
            bd = cx.sb(es, "bd", [128, 3, 16, 128], BF16)
            wg = cx.sb(es, "wg", [128, 48, 16], BF16)
            conv = cx.sb(es, "conv", [128, 16, 6], F32)
            bg = cx.sb(es, "bg", [16, 1], F32)
            stg = [cx.sb(es, f"wstg{i}", [128, KC, 128], F32) for i in range(2)]
            sstg = Tile(stg[0][:, :, :].rearrange("p a b -> p (a b)"), stg[0].b)
            x = cx.sb(es, "x", [128, KC, 512], F32)
            hT = cx.sb(es, "hT", [128, KC, 512], BF16)
            sq = cx.sb(es, "sq", [128, KC, 512], BF16)
            rstd = cx.sb(es, "rstd", [128, 512], F32)
            xmf = [cx.sb(es, f"xmf{i}", [128, 512], F32) for i in range(2)]
            acc = [cx.sb(es, f"acc{i}", [128, 512], F32) for i in range(2)]
            xmb = [cx.sb(es, f"xmb{i}", [128, 512], BF16) for i in range(2)]
            vTs = [cx.sb(es, f"vTs{i}", [128, 512], BF16) for i in range(3)]
            xcb = [cx.sb(es, f"xcb{i}", [128, 4, 512], BF16) for i in range(2)]
            mk = lambda nm, k=2: [cx.sb(es, f"{nm}{i}", [128, 4, 512], BF16) for i in range(k)] * (2 // k)
            szo, xcso, qTo, kTo, ktmo, vtmo = mk("szo", 1), mk("xcso", 1), mk("qTo"), mk("kTo"), mk("ktmo"), mk("vtmo")
            cx.dma("sp", conv[:], self.ml_conv.ap(), wr=[conv], owner=conv)
            cx.dma("sp", bg[:], self.ml_bg.ap(), wr=[bg], owner=bg)
            for i in range(3):
                for hf_ in range(2):
                    cx.dma("sp", sstg[:, :].rearrange("p (a b) -> p a b", b=128),
                           self.ml_bd[i][:, hf_ * 8:(hf_ + 1) * 8, :], wr=[sstg], owner=sstg)
                    cx.op("pool", lambda g, i=i, hf_=hf_: g.tensor_copy(
                        out=bd[:, i, hf_ * 8:(hf_ + 1) * 8, :],
                        in_=sstg[:, :].rearrange("p (a b) -> p a b", b=128)), rd=[sstg], wr=[bd])
            cx.dma("sp", sstg[:, 0:768].rearrange("p (a b) -> p a b", b=16), self.ml_wg.ap(), wr=[sstg], owner=sstg)
            cx.op("pool", lambda g: g.tensor_copy(out=wg[:], in_=sstg[:, 0:768].rearrange(
                "p (a b) -> p a b", b=16)), rd=[sstg], wr=[wg])

            def load_x(i):
                s, n, c = tiles[i]
                cx.dma("sp", x[:, :, 0:n], self.xres.ap().rearrange("(kc p) t -> p kc t", p=128)[:, :, s:s + n],
                       rd=[self.xres_b[i]], wr=[x], owner=x)
            load_x(0)
            gorder = [g for q in range(16) for g in (q, 16 + q)]
            for gi, g in enumerate(gorder):
                st = stg[gi % 2]
                srcw = self.ml_w_in.ap().rearrange("(kc p) m -> p kc m", p=128)[:, :, g * 128:(g + 1) * 128]
                cx.dma("sp", st[:], srcw, wr=[st], owner=st)
                cx.op("pool", lambda gp, st=st, g=g: gp.tensor_copy(
                    out=Win[:, :, g * 128:(g + 1) * 128], in_=st[:]), rd=[st], wr=[Winb[g // 4]])
            g_ps = self.ps[0]
            prod = [self.ps[5], self.ps[6], self.ps[7]]
            pcount = [0]

            def nextp():
                pcount[0] += 1
                return prod[pcount[0] % 3]

            for i, (s, n, c) in enumerate(tiles):
                ntb = n // 128
                self.norm_mod(None, x, sq, hT, n, c, 3, self.ps[0], rstd)
                if i + 1 < len(tiles):
                    load_x(i + 1)
                R = n if c else 64
                v3 = lambda ap: ap.rearrange("p (r w) -> p r w", w=R)

                def stageA(cc):
                    gq, jj = cc // 4, cc % 4
                    ob = gq % 2
                    xm_ps = self.ps[1 + cc % 2]
                    z_ps = self.ps[3 + cc % 2]
                    for k in range(KC):
                        cx.op("pe", lambda p, k=k: p.matmul(
                            xm_ps[:, 0:n], lhsT=Win[:, k, cc * 128:(cc + 1) * 128], rhs=hT[:, k, 0:n],
                            start=(k == 0), stop=(k == KC - 1)), rd=[hT, Winb[cc // 4]], wr=[xm_ps])
                    for k in range(KC):
                        cx.op("pe", lambda p, k=k: p.matmul(
                            z_ps[:, 0:n], lhsT=Win[:, k, 2048 + cc * 128:2048 + (cc + 1) * 128], rhs=hT[:, k, 0:n],
                            start=(k == 0), stop=(k == KC - 1)), rd=[hT, Winb[4 + cc // 4]], wr=[z_ps])

                def stageA_ew(cc):
                    gq, jj = cc // 4, cc % 4
                    ob = gq % 2
                    xm_ps = self.ps[1 + cc % 2]
                    z_ps = self.ps[3 + cc % 2]
                    xf, ac, xb = xmf[cc % 2], acc[cc % 2], xmb[cc % 2]
                    cx.op("act", lambda a: a.activation(out=xf[:, 0:n], in_=xm_ps[:, 0:n], func=AF.Copy),
                          rd=[xm_ps], wr=[xf])
                    cx.op("act", lambda a: a.activation(out=xb[:, 0:n], in_=xm_ps[:, 0:n], func=AF.Copy),
                          rd=[xm_ps], wr=[xb])
                    cx.op("act", lambda a: a.activation(out=szo[ob][:, jj, 0:n], in_=z_ps[:, 0:n], func=AF.Silu),
                          rd=[z_ps], wr=[szo[ob]])
                    cx.op("act", lambda a: a.activation(
                        out=ac[:, 0:n], in_=xm_ps[:, 0:n], func=AF.Identity, scale=conv[:, cc, 2:3],
                        bias=conv[:, cc, 5:6]), rd=[xm_ps, conv], wr=[ac])
                    for j in (0, 1, 3, 4):
                        sh = j - 2
                        o0, o1 = max(0, -sh), R - max(0, sh)
                        i0, i1 = max(0, sh), R - max(0, -sh)
                        cx.op("dve", lambda v, j=j, o0=o0, o1=o1, i0=i0, i1=i1: v.scalar_tensor_tensor(
                            out=v3(ac[:, 0:n])[:, :, o0:o1], in0=v3(xf[:, 0:n])[:, :, i0:i1],
                            scalar=conv[:, cc, j:j + 1], in1=v3(ac[:, 0:n])[:, :, o0:o1],
                            op0=ALU.mult, op1=ALU.add), rd=[xf, ac, conv], wr=[ac])

                def stageA_tail(cc):
                    gq, jj = cc // 4, cc % 4
                    ob = gq % 2
                    ac = acc[cc % 2]
                    cx.op("act", lambda a: a.activation(out=xcb[ob][:, jj, 0:n], in_=ac[:, 0:n], func=AF.Silu),
                          rd=[ac], wr=[xcb[ob]])
                    cx.op("pool", lambda g: g.tensor_scalar(
                        out=xcso[ob][:, jj, 0:n], in0=xcb[ob][:, jj, 0:n], scalar1=self.ml_ns_sb[:, 1, cc:cc + 1],
                        scalar2=None, op0=ALU.mult), rd=[xcb[ob], self.ml_ns_sb], wr=[xcso[ob]])
                    if jj == 3:
                        fm = lambda t: t.ap().rearrange("(cc p) t -> p cc t", p=128)[:, gq * 4:(gq + 1) * 4, s:s + n]
                        for dt_, ot in ((self.szT, szo[ob]), (self.xcsT, xcso[ob])):
                            cx.dma("sp", fm(dt_), ot[:, :, 0:n], rd=[ot], owner=ot)

                def stageB(cc):
                    gq, jj = cc // 4, cc % 4
                    ob = gq % 2
                    xb = xmb[cc % 2]
                    vt = vTs[cc % 3]
                    for wi, (rhs_t, rhs_ap, dst_t, dst_ap, eng) in enumerate((
                            (xcb[ob], xcb[ob][:, jj, 0:n], qTo[ob], qTo[ob][:, jj, 0:n], "act"),
                            (xcb[ob], xcb[ob][:, jj, 0:n], kTo[ob], kTo[ob][:, jj, 0:n], "dve"),
                            (xb, xb[:, 0:n], vt, vt[:, 0:n], "act"))):
                        pp = nextp()
                        cx.op("pe", lambda p, wi=wi, rhs_ap=rhs_ap, pp=pp: p.matmul(
                            pp[:, 0:n], lhsT=bd[:, wi, cc, :], rhs=rhs_ap, start=True, stop=True),
                            rd=[bd, rhs_t], wr=[pp])
                        if eng == "act":
                            cx.op("act", lambda a, pp=pp, dst_ap=dst_ap: a.activation(
                                out=dst_ap, in_=pp[:, 0:n], func=AF.Copy), rd=[pp], wr=[dst_t])
                        else:
                            cx.op("dve", lambda v, pp=pp, dst_ap=dst_ap: v.tensor_copy(out=dst_ap, in_=pp[:, 0:n]),
                                  rd=[pp], wr=[dst_t])
                    for wi, lt, lap, dst_t in ((1, xcb[ob], lambda tb: xcb[ob][:, jj, tb * 128:(tb + 1) * 128], ktmo[ob]),
                                               (2, xb, lambda tb: xb[:, tb * 128:(tb + 1) * 128], vtmo[ob])):
                        pp = nextp()
                        for tb in range(ntb):
                            cx.op("pe", lambda p, wi=wi, lap=lap, tb=tb, pp=pp: p.matmul(
                                pp[:, tb * 128:(tb + 1) * 128], lhsT=lap(tb), rhs=bd[:, wi, cc, :],
                                start=True, stop=True), rd=[bd, lt], wr=[pp])
                        cx.op("act", lambda a, pp=pp, dst_t=dst_t: a.activation(
                            out=dst_t[:, 0:ntb, jj * 128:(jj + 1) * 128],
                            in_=pp[:, 0:ntb * 128].rearrange("p (a b) -> p a b", b=128), func=AF.Copy),
                            rd=[pp], wr=[dst_t])

                def stageC(cc):
                    gq, jj = cc // 4, cc % 4
                    ob = gq % 2
                    vt = vTs[cc % 3]
                    for wi, (rt, rap) in enumerate(((qTo[ob], qTo[ob][:, jj, 0:n]), (kTo[ob], kTo[ob][:, jj, 0:n]),
                                                    (vt, vt[:, 0:n]))):
                        cx.op("pe", lambda p, wi=wi, rap=rap: p.matmul(
                            g_ps[0:16, 0:n], lhsT=wg[:, wi * 16 + cc, :], rhs=rap,
                            start=(cc == 0 and wi == 0), stop=(cc == 15 and wi == 2)), rd=[wg, rt], wr=[g_ps])
                    if jj == 3:
                        fm = lambda t: t.ap().rearrange("(cc p) t -> p cc t", p=128)[:, gq * 4:(gq + 1) * 4, s:s + n]
                        for dt_, ot in ((self.qT, qTo[ob]), (self.kT, kTo[ob])):
                            cx.dma("sp", fm(dt_), ot[:, :, 0:n], rd=[ot], owner=ot)
                        tmv = lambda t: t.ap()[s:s + n, gq * 512:(gq + 1) * 512].rearrange("(tb p) ch -> p tb ch", p=128)
                        for dt_, ot in ((self.ktm, ktmo[ob]), (self.vtm, vtmo[ob])):
                            cx.dma("sp", tmv(dt_), ot[:, 0:ntb, :], rd=[ot], owner=ot)

                for step in range(16 + 2):
                    if step < 16:
                        stageA(step)
                    if 0 <= step - 1 < 16:
                        stageB(step - 1)
                    if step < 16:
                        stageA_ew(step)
                    if 0 <= step - 2 < 16:
                        stageC(step - 2)
                    if step < 16:
                        stageA_tail(step)
                cx.op("act", lambda a: a.activation(out=self.gT16[:, s:s + n], in_=g_ps[0:16, 0:n], func=AF.Identity,
                                                    bias=bg[:, 0:1], scale=1.0), rd=[g_ps, bg], wr=[self.gT16])

    def mlstm_gates(self):
        cx = self.cx
        T, NCH = self.T, self.NCH
        cx.barrier()
        with ExitStack() as es:
            mk = lambda nm: cx.sb(es, nm, [4, T], F32)
            IG, FG, CS, A, P, TM = mk("IG"), mk("FG"), mk("CS"), mk("A"), mk("P"), mk("TM")
            mk2 = lambda nm: cx.sb(es, nm, [4, NCH], F32)
            Mb, Mend, dec = mk2("Mb"), mk2("Mend"), mk2("dec")
            ones4 = cx.sb(es, "ones4", [4, 128], F32)
            R = cx.sb(es, "R", [4, NCH, 4], F32)
            cx.op("pool", lambda g: g.memset(ones4[:], 1.0), wr=[ones4])
            c3 = lambda t: t[:, :].rearrange("p (c t) -> p c t", t=128)
            for d in range(2):
                order = self.chunk_order(d)
                rv = (lambda ap: ap[:, ::-1]) if d == 1 else (lambda ap: ap)
                for s0 in range(0, T, 512):
                    n = min(512, T - s0)
                    for dst, c0 in ((IG, d * 4), (FG, 8 + d * 4)):
                        pp = self.ps[1 + (s0 // 512) % 2] if dst is IG else self.ps[3 + (s0 // 512) % 2]
                        cx.op("pe", lambda p, pp=pp, c0=c0, s0=s0, n=n: p.matmul(
                            pp[0:4, 0:n], lhsT=self.ident[0:16, c0:c0 + 4], rhs=self.gT16[:, s0:s0 + n],
                            start=True, stop=True), rd=[self.ident, self.gT16], wr=[pp])
                        cx.op("act", lambda a, pp=pp, dst=dst, s0=s0, n=n: a.activation(
                            out=dst[:, s0:s0 + n], in_=pp[0:4, 0:n], func=AF.Copy), rd=[pp], wr=[dst])
                cx.op("act", lambda a: a.activation(out=FG[:, :], in_=FG[:, :], func=AF.Exp, scale=-1.0),
                      rd=[FG], wr=[FG])
                cx.op("act", lambda a: a.activation(out=FG[:, :], in_=FG[:, :], func=AF.Ln, bias=self.one_col[0:4, 0:1],
                                                    scale=1.0), rd=[FG, self.one_col], wr=[FG])
                for c in range(NCH):
                    sl = slice(c * 128, (c + 1) * 128)
                    cx.op("dve", lambda v, sl=sl: v.tensor_tensor_scan(
                        out=rv(CS[:, sl]), data0=ones4[:, :], data1=rv(FG[:, sl]), initial=0.0,
                        op0=ALU.mult, op1=ALU.add), rd=[FG, ones4], wr=[CS])
                cx.op("dve", lambda v: v.tensor_tensor(out=A[:, :], in0=IG[:, :], in1=CS[:, :], op=ALU.add),
                      rd=[IG, CS], wr=[A])
                for c in range(NCH):
                    sl = slice(c * 128, (c + 1) * 128)
                    cx.op("dve", lambda v, sl=sl: v.tensor_tensor_scan(
                        out=rv(P[:, sl]), data0=rv(A[:, sl]), data1=rv(A[:, sl]), initial=-1e30,
                        op0=ALU.max, op1=ALU.max), rd=[A], wr=[P])
                last = 127 if d == 0 else 0
                csl = c3(CS)[:, :, last]
                pend = c3(P)[:, :, last]
                cx.op("pool", lambda g: g.memset(Mb[:, :], 0.0), wr=[Mb])
                for i, c in enumerate(order):
                    cx.op("dve", lambda v, c=c: v.tensor_tensor(out=Mend[:, c:c + 1], in0=Mb[:, c:c + 1],
                                                                in1=pend[:, c:c + 1], op=ALU.max),
                          rd=[Mb, P], wr=[Mend])
                    if i + 1 < len(order):
                        cn = order[i + 1]
                        cx.op("dve", lambda v, c=c, cn=cn: v.tensor_tensor(
                            out=Mb[:, cn:cn + 1], in0=Mend[:, c:c + 1], in1=csl[:, c:c + 1], op=ALU.subtract),
                            rd=[Mend, CS], wr=[Mb])
                cx.op("dve", lambda v: v.tensor_tensor(out=dec[:, :], in0=Mb[:, :], in1=Mend[:, :], op=ALU.subtract),
                      rd=[Mb, Mend], wr=[dec])
                cx.op("act", lambda a: a.activation(out=dec[:, :], in_=dec[:, :], func=AF.Exp), rd=[dec], wr=[dec])
                mbc = Mend[:, :].unsqueeze(2).to_broadcast([4, NCH, 128])
                cx.op("dve", lambda v: v.tensor_tensor(out=c3(TM), in0=c3(A), in1=mbc, op=ALU.subtract),
                      rd=[A, Mend], wr=[TM])
                cx.op("act", lambda a: a.activation(out=TM[:, :], in_=TM[:, :], func=AF.Exp), rd=[TM], wr=[TM])
                cx.op("dve", lambda v: v.scalar_tensor_tensor(
                    out=c3(A), in0=c3(CS), scalar=float(0.5 * np.log(512.0)), in1=mbc, op0=ALU.add, op1=ALU.subtract),
                    rd=[CS, Mend], wr=[A])
                cx.op("act", lambda a: a.activation(out=A[:, :], in_=A[:, :], func=AF.Exp), rd=[A], wr=[A])
                for src, pp, dsts in ((TM, self.ps[5], (self.Wtm[d], self.Wtmb[d])), (A, self.ps[6], (self.Ftm[d],))):
                    for c in range(NCH):
                        cx.op("pe", lambda p, c=c, src=src, pp=pp: p.transpose(
                            pp[:, c * 4:(c + 1) * 4], src[:, c * 128:(c + 1) * 128], self.ident[0:4, 0:4]),
                            rd=[src, self.ident], wr=[pp])
                    for dst in dsts:
                        cx.op("dve", lambda v, pp=pp, dst=dst: v.tensor_copy(
                            out=dst[:, :, :], in_=pp[:, 0:NCH * 4].rearrange("p (c h) -> p c h", h=4)),
                            rd=[pp], wr=[dst])
                cx.op("dve", lambda v: v.tensor_tensor(
                    out=R[:, :, :], in0=dec[:, :].unsqueeze(2).to_broadcast([4, NCH, 4]),
                    in1=self.ident[0:4, 0:4].unsqueeze(1).to_broadcast([4, NCH, 4]), op=ALU.mult),
                    rd=[dec, self.ident], wr=[R])
                pp = self.ps[7]
                cx.op("pe", lambda p, pp=pp: p.matmul(pp[:, 0:NCH * 4], lhsT=self.ones_f[0:4, :],
                                                      rhs=R[:, :, :].rearrange("p c h -> p (c h)"),
                                                      start=True, stop=True), rd=[self.ones_f, R], wr=[pp])
                cx.op("dve", lambda v, pp=pp: v.tensor_copy(
                    out=self.decbc[d][:, :, :], in_=pp[:, 0:NCH * 4].rearrange("p (c h) -> p c h", h=4)),
                    rd=[pp], wr=[self.decbc[d]])

    def mlstm_scan(self, d):
        cx = self.cx
        T, NCH = self.T, self.NCH
        order = self.chunk_order(d)
        mask = self.maskU if d == 0 else self.maskL
        Wtm, Wtmb, Ftm, decbc = self.Wtm[d], self.Wtmb[d], self.Ftm[d], self.decbc[d]
        cx.barrier()
        with ExitStack() as es:
            C = [cx.sb(es, f"C{h}", [128, 4, 512], F32) for h in range(4)]
            Cd = [cx.sb(es, f"Cd{h}", [128, 4, 512], BF16) for h in range(4)]
            nv = cx.sb(es, "nv", [128, 4, 4], F32)
            nd = cx.sb(es, "nd", [128, 4, 4], BF16)
            for h in range(4):
                cx.op("pool", lambda g, h=h: g.memset(C[h][:, :, :], 0.0), wr=[C[h]])
                cx.op("pool", lambda g, h=h: g.memset(Cd[h][:, :, :], 0.0), wr=[Cd[h]])
            cx.op("pool", lambda g: g.memset(nv[:, :, :], 0.0), wr=[nv])
            cx.op("pool", lambda g: g.memset(nd[:, :, :], 0.0), wr=[nd])
            mk = lambda nm, shp, dt, k=2: [cx.sb(es, f"{nm}{i}", shp, dt) for i in range(k)]
            qTc, kTc = mk("qTc", [128, 16, 128], BF16), mk("kTc", [128, 16, 128], BF16)
            ktc, vtc = mk("ktc", [128, 2048], BF16), mk("vtc", [128, 2048], BF16)
            SmT = mk("SmT", [128, 128], BF16)
            vw = mk("vw", [128, 512], BF16)
            den = mk("den", [128, 1], F32, 4)
            hb = mk("hb", [128, 2048], F32)
            if d == 1:
                hf = mk("hf", [128, 2048], F32)
                hn = cx.sb(es, "hn", [128, 2048], BF16)
                st = cx.sb(es, "st", [128, 4, 6], F32)
                mv = cx.sb(es, "mv", [128, 4, 2], F32)
                rs = cx.sb(es, "rs", [128, 4], F32)
                xcsc, szc = mk("xcsc", [128, 16, 128], BF16), mk("szc", [128, 16, 128], BF16)
                t1 = mk("t1", [128, 8, 128], F32)
                yTo = mk("yTo", [128, 16, 128], BF16)
            fmv = lambda t, c: t.ap().rearrange("(cc p) t -> p cc t", p=128)[:, :, c * 128:(c + 1) * 128]

            def loads(ci):
                c = order[ci]
                b = ci % 2
                tb = self.yT_b[0]
                cx.dma("sp", qTc[b][:, :, :], fmv(self.qT, c), wr=[qTc[b]], owner=qTc[b])
                cx.dma("sp", kTc[b][:, :, :], fmv(self.kT, c), wr=[kTc[b]], owner=kTc[b])
                cx.dma("sp", ktc[b][:, :], self.ktm.ap()[c * 128:(c + 1) * 128, :], wr=[ktc[b]], owner=ktc[b])
                cx.dma("sp", vtc[b][:, :], self.vtm.ap()[c * 128:(c + 1) * 128, :], wr=[vtc[b]], owner=vtc[b])
                if d == 1:
                    cx.dma("sp", hf[b][:, :], self.hfwd.ap()[c * 128:(c + 1) * 128, :], wr=[hf[b]], owner=hf[b])
                    cx.dma("sp", xcsc[b][:, :, :], fmv(self.xcsT, c), wr=[xcsc[b]], owner=xcsc[b])
                    cx.dma("sp", szc[b][:, :, :], fmv(self.szT, c), wr=[szc[b]], owner=szc[b])
            loads(0)
            items = [(ci, c, h) for ci, c in enumerate(order) for h in range(4)]

            def stage1(it):
                ci, c, h = items[it]
                b = ci % 2
                S_ps = self.ps[it % 2]
                sm, vwt = SmT[it % 2], vw[it % 2]
                for j in range(4):
                    cx.op("pe", lambda p, j=j: p.matmul(
                        S_ps[:, 0:128], lhsT=kTc[b][:, h * 4 + j, :], rhs=qTc[b][:, h * 4 + j, :],
                        start=(j == 0), stop=(j == 3)), rd=[kTc[b], qTc[b]], wr=[S_ps])
                cx.op("act", lambda a: a.activation(
                    out=vwt[:, :], in_=vtc[b][:, h * 512:(h + 1) * 512], func=AF.Copy, scale=Wtm[:, c, h:h + 1]),
                    rd=[vtc[b], Wtm], wr=[vwt])
                cx.op("dve", lambda v: v.tensor_tensor(out=sm[:, :], in0=S_ps[:, 0:128], in1=mask[:, :],
                                                       op=ALU.mult), rd=[S_ps, mask], wr=[sm])

            def stage2(it):
                ci, c, h = items[it]
                b = ci % 2
                cn = order[ci + 1] if ci + 1 < len(order) else None
                N_ps = self.ps[2 + it % 2]
                D_ps = self.ps[4] if it % 2 == 0 else self.ps[7]
                dcol = ((it // 2) % 8) * 8
                sm, vwt, dn = SmT[it % 2], vw[it % 2], den[it % 4]
                cx.op("pe", lambda p: p.matmul(N_ps[:, :], lhsT=sm[:, :], rhs=vwt[:, :], start=True, stop=False),
                      rd=[sm, vwt], wr=[N_ps])
                for j in range(4):
                    cx.op("pe", lambda p, j=j: p.matmul(
                        N_ps[:, :], lhsT=qTc[b][:, h * 4 + j, :], rhs=Cd[h][:, j, :], start=False, stop=(j == 3)),
                        rd=[qTc[b], Cd[h]], wr=[N_ps])
                cx.op("pe", lambda p: p.matmul(D_ps[:, dcol:dcol + 1], lhsT=sm[:, :], rhs=Wtmb[:, c, h:h + 1],
                                               start=True, stop=False), rd=[sm, Wtmb], wr=[D_ps])
                for j in range(4):
                    cx.op("pe", lambda p, j=j: p.matmul(
                        D_ps[:, dcol:dcol + 1], lhsT=qTc[b][:, h * 4 + j, :], rhs=nd[:, h, j:j + 1],
                        start=False, stop=(j == 3)), rd=[qTc[b], nd], wr=[D_ps])
                ucol = dcol + 4
                for j in range(4):
                    cx.op("pe", lambda p, j=j: p.matmul(
                        D_ps[:, ucol + j:ucol + j + 1],
                        lhsT=ktc[b][:, h * 512 + j * 128:h * 512 + (j + 1) * 128], rhs=Wtmb[:, c, h:h + 1],
                        start=True, stop=True), rd=[ktc[b], Wtmb], wr=[D_ps])
                cx.op("act", lambda a: a.activation(out=dn[:, :], in_=D_ps[:, dcol:dcol + 1], func=AF.Abs),
                      rd=[D_ps], wr=[dn])
                cx.op("dve", lambda v: v.tensor_scalar(
                    out=dn[:, :], in0=dn[:, :], scalar1=Ftm[:, c, h:h + 1], scalar2=None,
                    op0=ALU.max), rd=[dn, Ftm], wr=[dn])
                cx.op("dve", lambda v: v.reciprocal(out=dn[:, :], in_=dn[:, :]), rd=[dn], wr=[dn])
                cx.op("act", lambda a: a.activation(out=hb[b][:, h * 512:(h + 1) * 512], in_=N_ps[:, :],
                                                    func=AF.Copy, scale=dn[:, 0:1]), rd=[N_ps, dn], wr=[hb[b]])
                cx.op("dve", lambda v: v.scalar_tensor_tensor(
                    out=nv[:, h, :], in0=nv[:, h, :], scalar=decbc[:, c, h:h + 1], in1=D_ps[:, ucol:ucol + 4],
                    op0=ALU.mult, op1=ALU.add), rd=[nv, decbc, D_ps], wr=[nv])
                if cn is not None:
                    cx.op("dve", lambda v: v.tensor_scalar(
                        out=nd[:, h, :], in0=nv[:, h, :], scalar1=decbc[:, cn, h:h + 1], scalar2=None,
                        op0=ALU.mult), rd=[nv, decbc], wr=[nd])
                for j in range(4):
                    U_ps = self.ps[5 + j % 2]
                    cx.op("pe", lambda p, j=j, U_ps=U_ps: p.matmul(
                        U_ps[:, :], lhsT=ktc[b][:, h * 512 + j * 128:h * 512 + (j + 1) * 128], rhs=vwt[:, :],
                        start=True, stop=True), rd=[ktc[b], vwt], wr=[U_ps])
                    cx.op("dve", lambda v, j=j, U_ps=U_ps: v.scalar_tensor_tensor(
                        out=C[h][:, j, :], in0=C[h][:, j, :], scalar=decbc[:, c, h:h + 1], in1=U_ps[:, :],
                        op0=ALU.mult, op1=ALU.add), rd=[C[h], decbc, U_ps], wr=[C[h]])
                    if cn is not None:
                        cx.op("act", lambda a, j=j: a.activation(
                            out=Cd[h][:, j, :], in_=C[h][:, j, :], func=AF.Copy, scale=decbc[:, cn, h:h + 1]),
                            rd=[C[h], decbc], wr=[Cd[h]])

            stage1(0)
            for it, (ci, c, h) in enumerate(items):
                b = ci % 2
                if h == 0 and ci + 1 < len(order):
                    loads(ci + 1)
                if it + 1 < len(items):
                    stage1(it + 1)
                stage2(it)
                if h != 3:
                    continue
                hrow = self.hfwd.ap()[c * 128:(c + 1) * 128, :]
                if d == 0:
                    cx.dma("sp", hrow, hb[b][:, :], rd=[hb[b]], owner=hb[b])
                    continue
                cx.op("dve", lambda g: g.tensor_tensor(out=hb[b][:, :], in0=hb[b][:, :], in1=hf[b][:, :], op=ALU.add),
                      rd=[hb[b], hf[b]], wr=[hb[b]])
                for h in range(4):
                    cx.op("dve", lambda v, h=h: v.bn_stats(out=st[:, h, :], in_=hb[b][:, h * 512:(h + 1) * 512]),
                          rd=[hb[b]], wr=[st])
                    cx.op("dve", lambda v, h=h: v.bn_aggr(out=mv[:, h, :], in_=st[:, h, :]), rd=[st], wr=[mv])
                cx.op("act", lambda a: a.activation(out=rs[:, :], in_=mv[:, :, 1], func=AF.Sqrt,
                                                    bias=self.eps_col[:, 0:1], scale=1.0), rd=[mv, self.eps_col], wr=[rs])
                cx.op("dve", lambda v: v.reciprocal(out=rs[:, :], in_=rs[:, :]), rd=[rs], wr=[rs])
                for h in range(4):
                    cx.op("dve", lambda v, h=h: v.tensor_scalar(
                        out=hn[:, h * 512:(h + 1) * 512], in0=hb[b][:, h * 512:(h + 1) * 512],
                        scalar1=mv[:, h, 0:1], scalar2=rs[:, h:h + 1], op0=ALU.subtract, op1=ALU.mult),
                        rd=[hb[b], mv, rs], wr=[hn])
                for r in range(2):
                    tp = self.ps[5 + r]
                    tpv = tp[:, :].bitcast(BF16)
                    for q in range(8):
                        cc = r * 8 + q
                        cx.op("pe", lambda p, cc=cc, q=q, tpv=tpv: p.transpose(
                            tpv[:, q * 128:(q + 1) * 128], hn[:, cc * 128:(cc + 1) * 128], self.identb[:, :]),
                            rd=[hn, self.identb], wr=[tp])
                    tt = t1[r]
                    cx.op("dve", lambda v, r=r, tt=tt, tpv=tpv: v.tensor_tensor(
                        out=tt[:, :, :], in0=tpv.rearrange("p (a b) -> p a b", b=128),
                        in1=self.ml_ns_sb[:, 0, r * 8:(r + 1) * 8].unsqueeze(2).to_broadcast([128, 8, 128]),
                        op=ALU.mult), rd=[tp, self.ml_ns_sb], wr=[tt])
                    cx.op("pool", lambda g, r=r, tt=tt: g.tensor_tensor(
                        out=tt[:, :, :], in0=tt[:, :, :], in1=xcsc[b][:, r * 8:(r + 1) * 8, :], op=ALU.add),
                        rd=[tt, xcsc[b]], wr=[tt])
                    cx.op("pool", lambda g, r=r, tt=tt: g.tensor_tensor(
                        out=yTo[b][:, r * 8:(r + 1) * 8, :], in0=tt[:, :, :], in1=szc[b][:, r * 8:(r + 1) * 8, :],
                        op=ALU.mult), rd=[tt, szc[b]], wr=[yTo[b]])
                cx.dma("sp", fmv(self.yT, c), yTo[b][:, :, :], rd=[yTo[b]], owner=yTo[b])


    def ssd(self, li, need_ctx):
        cx = self.cx
        NCH = self.NCH
        cx.barrier()
        with ExitStack() as es:
            self.dtr_sb = cx.sb(es, "dtr_sb", [128, NCH, 64], F32)
            self.sd_small = cx.sb(es, "sd_small", [128, 160], F32)
            cx.dma("sp", self.sd_small[:, :], self.sd_smallT.ap(), wr=[self.sd_small], owner=self.sd_small)
            cx.op("act", lambda a: a.activation(out=self.sd_small[:, 64:128], in_=self.sd_small[:, 64:128],
                                                func=AF.Exp), rd=[self.sd_small], wr=[self.sd_small])
            cx.op("dve", lambda v: v.tensor_scalar(out=self.sd_small[:, 64:128], in0=self.sd_small[:, 64:128],
                                                   scalar1=-1.0, scalar2=None, op0=ALU.mult),
                  rd=[self.sd_small], wr=[self.sd_small])
            self.ssd_in_z(li)
            self.ssd_in_x(li)
            if self.debug_stop == (li, "ml_in"):
                return
            for d in range(2):
                self.ssd_scan(d, need_ctx)
                if self.debug_stop and self.debug_stop[1] in ("ssd_tab", "ssd_d0"):
                    return
        tiles = self.tiles if need_ctx else self.tiles[:-1]
        self.proj_res(self.sd_w_out.ap(), 16, self.yT, self.yT_b, 5, (self.xres, self.xres_b),
                      (self.xres, self.xres_b), tiles)

    def _load_x(self, x, i):
        s, n, c = self.tiles[i]
        self.cx.dma("sp", x[:, :, 0:n], self.xres.ap().rearrange("(kc p) t -> p kc t", p=128)[:, :, s:s + n],
                    rd=[self.xres_b[i]], wr=[x], owner=x)

    def ssd_in_z(self, li):
        cx = self.cx
        tiles = self.tiles
        cx.barrier()
        with ExitStack() as es:
            NZ = 2048
            Wz = cx.sb(es, "Wz", [128, KC, NZ + 64], BF16)
            Wzb = [Buf(f"Wz{g}") for g in range(9)]
            stg = [cx.sb(es, f"wstg{i}", [128, KC, 256], F32) for i in range(2)]
            x = cx.sb(es, "x", [128, KC, 512], F32)
            hT = cx.sb(es, "hT", [128, KC, 512], BF16)
            sq = cx.sb(es, "sq", [128, KC, 512], BF16)
            rstd = cx.sb(es, "rstd", [128, 512], F32)
            szo = [cx.sb(es, f"szo{i}", [128, 4, NZ], BF16) for i in range(2)]
            self._load_x(x, 0)
            wv = self.sd_w_in.ap().rearrange("(kc p) m -> p kc m", p=128)
            for g in range(9):
                st = stg[g % 2]
                if g < 8:
                    cx.dma("sp", st[:], wv[:, :, g * 256:(g + 1) * 256], wr=[st], owner=st)
                    cx.op("pool", lambda gp, st=st, g=g: gp.tensor_copy(
                        out=Wz[:, :, g * 256:(g + 1) * 256], in_=st[:]), rd=[st], wr=[Wzb[g]])
                else:
                    cx.dma("sp", st[:, :, 0:64], wv[:, :, 6144:6208], wr=[st], owner=st)
                    cx.op("pool", lambda gp, st=st: gp.tensor_copy(
                        out=Wz[:, :, NZ:NZ + 64], in_=st[:, :, 0:64]), rd=[st], wr=[Wzb[8]])
            cnt = 0
            hTs = [hT, cx.sb(es, "hT1", [128, KC, 512], BF16)]

            def prep(i):
                s_, n_, c_ = tiles[i]
                self.norm_mod(None, x, sq, hTs[i % 2], n_, c_, 3, self.ps[0], rstd)
                if i + 1 < len(tiles):
                    self._load_x(x, i + 1)
            prep(0)
            for i, (s, n, c) in enumerate(tiles):
                ntb = n // 128
                hT = hTs[i % 2]
                so = szo[i % 2]
                for tb in range(ntb):
                    if tb == ntb // 2 and i + 1 < len(tiles):
                        prep(i + 1)
                    for zb in range(4):
                        cnt += 1
                        zp = self.ps[1 + cnt % 4]
                        for k in range(KC):
                            cx.op("pe", lambda p, k=k, zp=zp: p.matmul(
                                zp[:, :], lhsT=hT[:, k, tb * 128:(tb + 1) * 128], rhs=Wz[:, k, zb * 512:(zb + 1) * 512],
                                start=(k == 0), stop=(k == KC - 1)), rd=[hT, Wzb[2 * zb], Wzb[2 * zb + 1]], wr=[zp])
                        cx.op("act", lambda a, zp=zp: a.activation(out=so[:, tb, zb * 512:(zb + 1) * 512], in_=zp[:, :],
                                                                   func=AF.Silu), rd=[zp], wr=[so])
                    dp = self.ps[5 + tb % 2]
                    for k in range(KC):
                        cx.op("pe", lambda p, k=k, dp=dp: p.matmul(
                            dp[:, 0:64], lhsT=hT[:, k, tb * 128:(tb + 1) * 128], rhs=Wz[:, k, NZ:NZ + 64],
                            start=(k == 0), stop=(k == KC - 1)), rd=[hT, Wzb[8]], wr=[dp])
                    ch = s // 128 + tb
                    cx.op("dve", lambda v, dp=dp, ch=ch: v.tensor_copy(out=self.dtr_sb[:, ch, :], in_=dp[:, 0:64]),
                          rd=[dp], wr=[self.dtr_sb])
                cx.dma("sp", self.sztm.ap()[s:s + n, :].rearrange("(tb p) ch -> p tb ch", p=128), so[:, 0:ntb, :],
                       rd=[so], owner=so)

    def ssd_in_x(self, li):
        cx = self.cx
        tiles = self.tiles
        cx.barrier()
        with ExitStack() as es:
            Wx = cx.sb(es, "Wx", [128, KC, 4096], BF16)
            Wxb = [Buf(f"Wx{g}") for g in range(16)]
            conv = cx.sb(es, "conv", [128, 32, 6], F32)
            stg = [cx.sb(es, f"wstg{i}", [128, KC, 256], F32) for i in range(2)]
            x = cx.sb(es, "x", [128, KC, 512], F32)
            hT = cx.sb(es, "hT", [128, KC, 512], BF16)
            sq = cx.sb(es, "sq", [128, KC, 512], BF16)
            rstd = cx.sb(es, "rstd", [128, 512], F32)
            xf = [cx.sb(es, f"xf{i}", [128, 512], F32) for i in range(4)]
            acc = [cx.sb(es, f"acc{i}", [128, 512], F32) for i in range(4)]
            xc = [cx.sb(es, f"xc{i}", [128, 512], BF16) for i in range(4)]
            xtmo = cx.sb(es, "xtmo", [128, 4, 2048], BF16)
            Btmo = cx.sb(es, "Btmo", [128, 4, 1024], BF16)
            BTo = cx.sb(es, "BTo", [128, 8, 512], BF16)
            CTo = cx.sb(es, "CTo", [128, 8, 512], BF16)
            cx.dma("sp", conv[:], self.sd_conv.ap(), wr=[conv], owner=conv)
            self._load_x(x, 0)
            wv = self.sd_w_in.ap().rearrange("(kc p) m -> p kc m", p=128)
            for g in range(16):
                st = stg[g % 2]
                cx.dma("sp", st[:], wv[:, :, 2048 + g * 256:2048 + (g + 1) * 256], wr=[st], owner=st)
                cx.op("pool", lambda gp, st=st, g=g: gp.tensor_copy(
                    out=Wx[:, :, g * 256:(g + 1) * 256], in_=st[:]), rd=[st], wr=[Wxb[g]])
            hTs = [hT, cx.sb(es, "hT1", [128, KC, 512], BF16)]

            def prep(i):
                s_, n_, c_ = tiles[i]
                self.norm_mod(None, x, sq, hTs[i % 2], n_, c_, 3, self.ps[0], rstd)
                if i + 1 < len(tiles):
                    self._load_x(x, i + 1)
            prep(0)
            for i, (s, n, c) in enumerate(tiles):
                ntb = n // 128
                hT = hTs[i % 2]
                R = n if c else 64
                v3 = lambda ap: ap.rearrange("p (r w) -> p r w", w=R)

                def dst_of(cc):
                    if cc < 16:
                        return xc[cc % 4], xc[cc % 4][:, 0:n]
                    if cc < 24:
                        return BTo, BTo[:, cc - 16, 0:n]
                    return CTo, CTo[:, cc - 24, 0:n]

                def stageA(pair):
                    ccs = (2 * pair, 2 * pair + 1)
                    xps = {cc: self.ps[1 + cc % 4] for cc in ccs}
                    for cc in ccs:
                        for k in range(KC):
                            cx.op("pe", lambda p, k=k, cc=cc: p.matmul(
                                xps[cc][:, 0:n], lhsT=Wx[:, k, cc * 128:(cc + 1) * 128], rhs=hT[:, k, 0:n],
                                start=(k == 0), stop=(k == KC - 1)), rd=[hT, Wxb[cc // 2]], wr=[xps[cc]])
                    for cc in ccs:
                        f, ac = xf[cc % 4], acc[cc % 4]
                        cx.op("act", lambda a, cc=cc, f=f: a.activation(out=f[:, 0:n], in_=xps[cc][:, 0:n],
                                                                        func=AF.Copy), rd=[xps[cc]], wr=[f])
                        cx.op("act", lambda a, cc=cc, ac=ac: a.activation(
                            out=ac[:, 0:n], in_=xps[cc][:, 0:n], func=AF.Identity, scale=conv[:, cc, 2:3],
                            bias=conv[:, cc, 5:6]), rd=[xps[cc], conv], wr=[ac])
                    for j in (0, 1, 3, 4):
                        sh = j - 2
                        o0, o1 = max(0, -sh), R - max(0, sh)
                        i0, i1 = max(0, sh), R - max(0, -sh)
                        for cc in ccs:
                            f, ac = xf[cc % 4], acc[cc % 4]
                            cx.op("dve", lambda v, j=j, cc=cc, f=f, ac=ac, o0=o0, o1=o1, i0=i0, i1=i1:
                                  v.scalar_tensor_tensor(
                                      out=v3(ac[:, 0:n])[:, :, o0:o1], in0=v3(f[:, 0:n])[:, :, i0:i1],
                                      scalar=conv[:, cc, j:j + 1], in1=v3(ac[:, 0:n])[:, :, o0:o1],
                                      op0=ALU.mult, op1=ALU.add), rd=[f, ac, conv], wr=[ac])
                    for cc in ccs:
                        dt_, dap = dst_of(cc)
                        ac = acc[cc % 4]
                        cx.op("act", lambda a, dap=dap, ac=ac: a.activation(out=dap, in_=ac[:, 0:n], func=AF.Silu),
                              rd=[ac], wr=[dt_])

                def stageB(pair):
                    for cc in (2 * pair, 2 * pair + 1):
                        if cc >= 24:
                            continue
                        st_, sap = dst_of(cc)
                        tp = self.ps[5 + cc % 2]
                        tpv = tp[:, :].bitcast(BF16)
                        for tb in range(ntb):
                            cx.op("pe", lambda p, tb=tb, sap=sap, tpv=tpv: p.transpose(
                                tpv[:, tb * 128:(tb + 1) * 128], sap[:, tb * 128:(tb + 1) * 128], self.identb[:, :]),
                                rd=[st_, self.identb], wr=[tp])
                        if cc < 16:
                            ot, oap = xtmo, xtmo[:, 0:ntb, cc * 128:(cc + 1) * 128]
                        else:
                            ot, oap = Btmo, Btmo[:, 0:ntb, (cc - 16) * 128:(cc - 15) * 128]
                        cx.op("act", lambda a, oap=oap, tpv=tpv: a.activation(
                            out=oap, in_=tpv[:, 0:ntb * 128].rearrange("p (a b) -> p a b", b=128), func=AF.Copy),
                            rd=[tp], wr=[ot])

                for step in range(17):
                    if step == 8 and i + 1 < len(tiles):
                        prep(i + 1)
                    if step < 16:
                        stageA(step)
                    if step >= 1:
                        stageB(step - 1)
                tmv = lambda t: t.ap()[s:s + n, :].rearrange("(tb p) ch -> p tb ch", p=128)
                cx.dma("sp", tmv(self.xtm), xtmo[:, 0:ntb, :], rd=[xtmo], owner=xtmo)
                cx.dma("sp", tmv(self.Btm), Btmo[:, 0:ntb, :], rd=[Btmo], owner=Btmo)
                fm = lambda t: t.ap().rearrange("(g p) t -> p g t", p=128)[:, :, s:s + n]
                cx.dma("sp", fm(self.BT), BTo[:, :, 0:n], rd=[BTo], owner=BTo)
                cx.dma("sp", fm(self.CT), CTo[:, :, 0:n], rd=[CTo], owner=CTo)

    def ssd_scan(self, d, need_ctx):
        cx = self.cx
        T, NCH = self.T, self.NCH
        order = self.chunk_order(d)
        tri = self.maskU if d == 0 else self.maskL
        neg = self.negU if d == 0 else self.negL
        nlat = self.TL // 128
        cx.barrier()
        with ExitStack() as es:
            mkt = lambda nm: cx.sb(es, nm, [128, NCH, 32], F32)
            dt_t, la_t, ncs_t, ecs_t, w2_t, dec_t = mkt("dt_t"), mkt("la_t"), mkt("ncs_t"), mkt("ecs_t"), mkt("w2_t"), mkt("dec_t")
            fl = lambda t: t[:, :, :].rearrange("p c h -> p (c h)")
            sm = self.sd_small
            cx.op("dve", lambda v: v.tensor_tensor(
                out=dt_t[:, :, :], in0=self.dtr_sb[:, :, d * 32:(d + 1) * 32],
                in1=sm[:, d * 32:(d + 1) * 32].unsqueeze(1).to_broadcast([128, NCH, 32]), op=ALU.add),
                rd=[self.dtr_sb, sm], wr=[dt_t])
            cx.op("act", lambda a: a.activation(out=fl(dt_t), in_=fl(dt_t), func=AF.Exp), rd=[dt_t], wr=[dt_t])
            cx.op("act", lambda a: a.activation(out=fl(dt_t), in_=fl(dt_t), func=AF.Ln, bias=self.one_col[:, 0:1],
                                                scale=1.0), rd=[dt_t, self.one_col], wr=[dt_t])
            cx.op("dve", lambda v: v.tensor_tensor(
                out=la_t[:, :, :], in0=dt_t[:, :, :],
                in1=sm[:, 64 + d * 32:64 + (d + 1) * 32].unsqueeze(1).to_broadcast([128, NCH, 32]), op=ALU.mult),
                rd=[dt_t, sm], wr=[la_t])
            NF = NCH * 32
            for c0 in range(0, NF, 512):
                w = min(512, NF - c0)
                csp, cep = self.ps[1], self.ps[2]
                cx.op("pe", lambda p, c0=c0, w=w: p.matmul(csp[:, 0:w], lhsT=tri[:, :], rhs=fl(la_t)[:, c0:c0 + w],
                                                           start=True, stop=True), rd=[tri, la_t], wr=[csp])
                cx.op("pe", lambda p, c0=c0, w=w: p.matmul(cep[:, 0:w], lhsT=self.ones_f[:, :], rhs=fl(la_t)[:, c0:c0 + w],
                                                           start=True, stop=True), rd=[self.ones_f, la_t], wr=[cep])
                cx.op("dve", lambda v, c0=c0, w=w: v.tensor_scalar(
                    out=fl(ncs_t)[:, c0:c0 + w], in0=csp[:, 0:w], scalar1=-1.0, scalar2=None, op0=ALU.mult),
                    rd=[csp], wr=[ncs_t])
                cx.op("act", lambda a, c0=c0, w=w: a.activation(out=fl(ecs_t)[:, c0:c0 + w], in_=csp[:, 0:w],
                                                                func=AF.Exp), rd=[csp], wr=[ecs_t])
                cx.op("act", lambda a, c0=c0, w=w: a.activation(out=fl(dec_t)[:, c0:c0 + w], in_=cep[:, 0:w],
                                                                func=AF.Exp), rd=[cep], wr=[dec_t])
                cx.op("dve", lambda v, c0=c0, w=w: v.tensor_tensor(
                    out=fl(w2_t)[:, c0:c0 + w], in0=cep[:, 0:w], in1=fl(ncs_t)[:, c0:c0 + w], op=ALU.add),
                    rd=[cep, ncs_t], wr=[w2_t])
                cx.op("act", lambda a, c0=c0, w=w: a.activation(out=fl(w2_t)[:, c0:c0 + w], in_=fl(w2_t)[:, c0:c0 + w],
                                                                func=AF.Exp), rd=[w2_t], wr=[w2_t])
            if self.debug_stop and self.debug_stop[1] == "ssd_tab":
                return
            neg4 = cx.sb(es, "neg4", [128, 512], BF16)
            cx.op("pool", lambda g: g.tensor_copy(
                out=neg4[:, :].rearrange("p (r t) -> p r t", t=128),
                in_=neg[:, :].unsqueeze(1).to_broadcast([128, 4, 128])), rd=[neg], wr=[neg4])
            hs = cx.sb(es, "hs", [128, 8, 256], F32)
            hsb = cx.sb(es, "hsb", [128, 8, 256], BF16)
            cx.op("pool", lambda g: g.memset(hs[:, :, :], 0.0), wr=[hs])
            cx.op("pool", lambda g: g.memset(hsb[:, :, :], 0.0), wr=[hsb])
            mk = lambda nm, shp, dt, k=2: [cx.sb(es, f"{nm}{i}", shp, dt) for i in range(k)]
            xt, Bt = mk("xt", [128, 2048], BF16), mk("Bt", [128, 1024], BF16)
            BTc, CTc = mk("BTc", [128, 8, 128], BF16), mk("CTc", [128, 8, 128], BF16)
            xdt, xw = mk("xdt", [128, 2048], BF16), mk("xw", [128, 2048], BF16)
            Eb = mk("Eb", [128, 128], BF16, 4)
            mT = mk("mT", [128, 128], BF16, 4)
            tz = mk("tz", [128, 256], F32)
            yb = mk("yb", [128, 2048], F32)
            if d == 1:
                yf = mk("yf", [128, 2048], F32)
                szc = mk("szc", [128, 2048], BF16)
                ngb = cx.sb(es, "ngb", [128, 2048], F32)
                cx.dma("sp", ngb[:, :], self.sd_ng.ap(), wr=[ngb], owner=ngb)
                yg = cx.sb(es, "yg", [128, 2048], F32)
                junk = cx.sb(es, "junk", [128, 2048], BF16)
                yn = cx.sb(es, "yn", [128, 2048], BF16)
                ss = cx.sb(es, "ss", [128, 1], F32)
                yTo = mk("yTo", [128, 16, 128], BF16)
            fmv = lambda t, c: t.ap().rearrange("(g p) t -> p g t", p=128)[:, :, c * 128:(c + 1) * 128]
            rows = lambda t, c: t.ap()[c * 128:(c + 1) * 128, :]

            def loads(ci):
                c = order[ci]
                b = ci % 2
                cx.dma("sp", xt[b][:, :], rows(self.xtm, c), wr=[xt[b]], owner=xt[b])
                cx.dma("sp", Bt[b][:, :], rows(self.Btm, c), wr=[Bt[b]], owner=Bt[b])
                cx.dma("sp", BTc[b][:, :, :], fmv(self.BT, c), wr=[BTc[b]], owner=BTc[b])
                cx.dma("sp", CTc[b][:, :, :], fmv(self.CT, c), wr=[CTc[b]], owner=CTc[b])
                if d == 1 and (need_ctx or c < nlat):
                    cx.dma("sp", yf[b][:, :], rows(self.hfwd, c), wr=[yf[b]], owner=yf[b])
                    cx.dma("sp", szc[b][:, :], rows(self.sztm, c), wr=[szc[b]], owner=szc[b])
            loads(0)
            ntri = self.nmaskU if d == 0 else self.nmaskL
            E4 = mk("E4", [128, 4, 128], BF16)
            mT4 = mk("mT4", [128, 4, 128], BF16)
            bc64 = lambda ap: ap.unsqueeze(2).to_broadcast([128, 32, 64])
            v64 = lambda ap: ap.rearrange("p (h q) -> p h q", q=64)
            v4 = lambda ap: ap.rearrange("p (r q) -> p r q", q=64)
            items = [(ci, c, g) for ci, c in enumerate(order) for g in range(8)]

            def pre(ci):
                c = order[ci]
                b = ci % 2
                cx.op("pool", lambda g: g.tensor_tensor(out=v64(xdt[b][:, :]), in0=v64(xt[b][:, :]),
                                                        in1=bc64(dt_t[:, c, :]), op=ALU.mult),
                      rd=[xt[b], dt_t], wr=[xdt[b]])
                cx.op("dve", lambda v: v.tensor_tensor(out=v64(xw[b][:, :]), in0=v64(xdt[b][:, :]),
                                                       in1=bc64(w2_t[:, c, :]), op=ALU.mult),
                      rd=[xdt[b], w2_t], wr=[xw[b]])

            def stage1(it):
                ci, c, g = items[it]
                b = ci % 2
                cb = self.ps[it % 2]
                ep = self.ps[2 + it % 2]
                E, m_ = E4[it % 2], mT4[it % 2]
                cx.op("pe", lambda p: p.matmul(cb[:, 0:128], lhsT=BTc[b][:, g, :], rhs=CTc[b][:, g, :],
                                               start=True, stop=True), rd=[BTc[b], CTc[b]], wr=[cb])
                for r in range(4):
                    hd = g * 4 + r
                    lac = la_t[:, c, hd:hd + 1].to_broadcast([128, 128])
                    cx.op("pe", lambda p, r=r, lac=lac: p.matmul(
                        ep[:, r * 128:(r + 1) * 128], lhsT=lac, rhs=tri[:, :], start=True, stop=False),
                        rd=[la_t, tri], wr=[ep])
                    cx.op("pe", lambda p, r=r: p.matmul(
                        ep[:, r * 128:(r + 1) * 128], lhsT=self.identb[:, :], rhs=neg[:, :], start=False, stop=True),
                        rd=[self.identb, neg], wr=[ep])
                for r in range(4):
                    hd = g * 4 + r
                    cx.op("act", lambda a, r=r, hd=hd: a.activation(
                        out=E[:, r, :], in_=ep[:, r * 128:(r + 1) * 128], func=AF.Exp,
                        bias=ncs_t[:, c, hd:hd + 1], scale=1.0), rd=[ep, ncs_t], wr=[E])
                cx.op("dve", lambda v: v.tensor_tensor(
                    out=m_[:, :, :], in0=cb[:, 0:128].unsqueeze(1).to_broadcast([128, 4, 128]), in1=E[:, :, :],
                    op=ALU.mult), rd=[cb, E], wr=[m_])

            def stage2(it):
                ci, c, g = items[it]
                b = ci % 2
                m_ = mT4[it % 2]
                Y = self.ps[4 + it % 2]
                Z = U = self.ps[6 + it % 2]
                zc, uc = 0, 256
                for r in range(4):
                    hd = g * 4 + r
                    cx.op("pe", lambda p, hd=hd, r=r: p.matmul(
                        Y[:, r * 64:(r + 1) * 64], lhsT=m_[:, r, :], rhs=xdt[b][:, hd * 64:(hd + 1) * 64],
                        start=True, stop=True), rd=[m_, xdt[b]], wr=[Y])
                cx.op("pe", lambda p: p.matmul(Z[:, zc:zc + 256], lhsT=CTc[b][:, g, :], rhs=hsb[:, g, :],
                                               start=True, stop=True), rd=[CTc[b], hsb], wr=[Z])
                cx.op("pe", lambda p: p.matmul(
                    U[:, uc:uc + 256], lhsT=Bt[b][:, g * 128:(g + 1) * 128], rhs=xw[b][:, g * 256:(g + 1) * 256],
                    start=True, stop=True), rd=[Bt[b], xw[b]], wr=[U])
                t_ = tz[it % 2]
                cx.op("dve", lambda v: v.tensor_tensor(
                    out=v4(t_[:, :]), in0=v4(Z[:, zc:zc + 256]),
                    in1=ecs_t[:, c, g * 4:(g + 1) * 4].unsqueeze(2).to_broadcast([128, 4, 64]), op=ALU.mult),
                    rd=[Z, ecs_t], wr=[t_])
                cx.op("dve", lambda v: v.tensor_tensor(
                    out=yb[b][:, g * 256:(g + 1) * 256], in0=Y[:, 0:256], in1=t_[:, :], op=ALU.add),
                    rd=[Y, t_], wr=[yb[b]])
                cx.op("pool", lambda gp: gp.tensor_tensor(
                    out=v4(hs[:, g, :]), in0=v4(hs[:, g, :]),
                    in1=dec_t[:, c, g * 4:(g + 1) * 4].unsqueeze(2).to_broadcast([128, 4, 64]), op=ALU.mult),
                    rd=[hs, dec_t], wr=[hs])
                cx.op("dve", lambda v: v.tensor_tensor(
                    out=hs[:, g, :], in0=U[:, uc:uc + 256], in1=hs[:, g, :], op=ALU.add), rd=[U, hs], wr=[hs])
                cx.op("act", lambda a: a.activation(out=hsb[:, g, :], in_=hs[:, g, :], func=AF.Copy),
                      rd=[hs], wr=[hsb])

            pre(0)
            stage1(0)
            for it, (ci, c, g) in enumerate(items):
                b = ci % 2
                if g == 0 and ci + 1 < len(order):
                    loads(ci + 1)
                if g == 4 and ci + 1 < len(order):
                    pre(ci + 1)
                if it + 1 < len(items):
                    stage1(it + 1)
                stage2(it)
                if g != 7:
                    continue
                if d == 0:
                    cx.dma("sp", rows(self.hfwd, c), yb[b][:, :], rd=[yb[b]], owner=yb[b])
                    continue
                if not (need_ctx or c < nlat):
                    continue
                cx.op("dve", lambda g: g.tensor_tensor(out=yb[b][:, :], in0=yb[b][:, :], in1=yf[b][:, :], op=ALU.add),
                      rd=[yb[b], yf[b]], wr=[yb[b]])
                cx.op("pool", lambda g: g.tensor_tensor(out=v64(yg[:, :]), in0=v64(xt[b][:, :]),
                                                        in1=bc64(sm[:, 128:160]), op=ALU.mult),
                      rd=[xt[b], sm], wr=[yg])
                cx.op("dve", lambda g: g.tensor_tensor(out=yg[:, :], in0=yg[:, :], in1=yb[b][:, :], op=ALU.add),
                      rd=[yg, yb[b]], wr=[yg])
                cx.op("dve", lambda v: v.tensor_tensor(out=yg[:, :], in0=yg[:, :], in1=szc[b][:, :], op=ALU.mult),
                      rd=[yg, szc[b]], wr=[yg])
                cx.op("act", lambda a: a.activation(out=junk[:, :], in_=yg[:, :], func=AF.Square, accum_out=ss[:, 0:1]),
                      rd=[yg], wr=[junk, ss])
                cx.op("act", lambda a: a.activation(out=ss[:, :], in_=ss[:, :], func=AF.Sqrt, bias=self.eps_col[:, 0:1],
                                                    scale=1.0 / 2048), rd=[ss, self.eps_col], wr=[ss])
                cx.op("dve", lambda v: v.reciprocal(out=ss[:, :], in_=ss[:, :]), rd=[ss], wr=[ss])
                cx.op("dve", lambda v: v.scalar_tensor_tensor(
                    out=yn[:, :], in0=yg[:, :], scalar=ss[:, 0:1], in1=ngb[:, :], op0=ALU.mult, op1=ALU.mult),
                    rd=[yg, ss, ngb], wr=[yn])
                for r in range(2):
                    tp = self.ps[r]
                    tpv = tp[:, :].bitcast(BF16)
                    for q in range(8):
                        cc = r * 8 + q
                        cx.op("pe", lambda p, cc=cc, q=q, tpv=tpv: p.transpose(
                            tpv[:, q * 128:(q + 1) * 128], yn[:, cc * 128:(cc + 1) * 128], self.identb[:, :]),
                            rd=[yn, self.identb], wr=[tp])
                    cx.op("act", lambda a, r=r, tpv=tpv: a.activation(
                        out=yTo[b][:, r * 8:(r + 1) * 8, :], in_=tpv.rearrange("p (a b) -> p a b", b=128),
                        func=AF.Copy), rd=[tp], wr=[yTo[b]])
                cx.dma("sp", self.yT.ap().rearrange("(cc p) t -> p cc t", p=128)[:, :, c * 128:(c + 1) * 128],
                       yTo[b][:, :, :], rd=[yTo[b]], owner=yTo[b])


def prep_core_inputs(inp, b):
    f = np.float32
    xin = np.ascontiguousarray(np.concatenate([inp["x"][b].T, inp["ctx"][b].T], axis=1), dtype=f)
    cT = np.stack([inp["c"][b].reshape(KC, 128).T, inp["c_ctx"].reshape(KC, 128).T], axis=-1)
    return {"xin": xin, "cT": np.ascontiguousarray(cT, dtype=f)}


def prep_shared_inputs(inp):
    f = np.float32
    depth = inp["ada_w"].shape[0]
    sh = {}
    sh["ada_w"] = np.ascontiguousarray(inp["ada_w"], dtype=f)
    sh["ada_bT"] = np.ascontiguousarray(inp["ada_b"].reshape(depth, 72, 128).transpose(0, 2, 1), dtype=f)
    sh["gT"] = np.ascontiguousarray(inp["norm_g"].reshape(depth, 6, KC, 128).transpose(0, 3, 1, 2), dtype=f)
    sh["w_gate"] = np.ascontiguousarray(inp["ffn_w_gate"], dtype=f)
    sh["w_up"] = np.ascontiguousarray(inp["ffn_w_up"], dtype=f)
    sh["w_down"] = np.ascontiguousarray(inp["ffn_w_down"], dtype=f)
    sh["ml_w_in"] = np.ascontiguousarray(inp["mlstm_w_in"][0], dtype=f)
    cw = np.concatenate([inp["mlstm_conv_w"][0], inp["mlstm_conv_b"][0][None]], 0)
    sh["ml_conv"] = np.ascontiguousarray(cw.reshape(6, 16, 128).transpose(2, 1, 0), dtype=f)
    bd = np.zeros((3, 16, 128, 128), f)
    for i, nm in enumerate(("mlstm_w_q", "mlstm_w_k", "mlstm_w_v")):
        w = inp[nm][0].reshape(16, 32, 4, 4)
        for n in range(32):
            bd[i, :, n * 4:(n + 1) * 4, n * 4:(n + 1) * 4] = w[:, n]
    sh["ml_bd"] = np.ascontiguousarray(bd.transpose(0, 2, 1, 3))
    perm = np.array([d * 8 + io * 4 + h for io in range(2) for d in range(2) for h in range(4)])
    wg = inp["mlstm_w_gates"][0][:, perm]
    sh["ml_wg"] = np.ascontiguousarray(wg.reshape(48, 128, 16).transpose(1, 0, 2), dtype=f)
    sh["ml_bg"] = np.ascontiguousarray(inp["mlstm_b_gates"][0][perm].reshape(16, 1), dtype=f)
    ns = np.stack([inp["mlstm_norm_g"][0], inp["mlstm_skip"][0]], 0)
    sh["ml_ns"] = np.ascontiguousarray(ns.reshape(2, 16, 128).transpose(2, 0, 1), dtype=f)
    sh["ml_w_out"] = np.ascontiguousarray(inp["mlstm_w_out"][0], dtype=f)
    sh["sd_w_in"] = np.ascontiguousarray(inp["ssd_w_in"][0], dtype=f)
    cw = np.concatenate([inp["ssd_conv_w"][0], inp["ssd_conv_b"][0][None]], 0)
    sh["sd_conv"] = np.ascontiguousarray(cw.reshape(6, 32, 128).transpose(2, 1, 0), dtype=f)
    small = np.concatenate([inp["ssd_dt_bias"][0].reshape(64), inp["ssd_a_log"][0].reshape(64), inp["ssd_d"][0]])
    sh["sd_smallT"] = np.ascontiguousarray(np.broadcast_to(small[None], (128, 160)), dtype=f)
    sh["sd_ng"] = np.ascontiguousarray(np.broadcast_to(inp["ssd_norm_g"][0][None], (128, 2048)), dtype=f)
    sh["sd_w_out"] = np.ascontiguousarray(inp["ssd_w_out"][0], dtype=f)
    return sh


def kernel(**inp):
    inp = {k: np.asarray(v) for k, v in inp.items()}
    B = inp["x"].shape[0]
    cx = Ctx()
    net = Net(cx)
    net.build()
    sh = prep_shared_inputs(inp)
    in_maps = []
    for b in range(B):
        m = dict(sh)
        m.update(prep_core_inputs(inp, b))
        in_maps.append(m)
    res = run_bass_kernel_spmd(cx.nc, in_maps, core_ids=list(range(B)))
    out = np.stack([np.ascontiguousarray(r["out"].T) for r in res.results], axis=0)
    return out.astype(np.float32)
```
